# Optimizing a Trainium2 kernel written in Bass

```python
import math
import jax
import jax.numpy as jnp
from jax import lax
import numpy as np

D_MODEL = 2048
BATCH = 16
SEQ = 256
DEPTH = 4
DEC_BATCH = 4
DEC_SEQ = 1024
PAST_LEN = 256

GRID_W = 64
N_AB_LAYERS = (DEPTH + 1) // 2
N_C_LAYERS = DEPTH // 2

A_HEADS = 4
A_DK = 128
A_DV = 256
A_GATE_RANK = 16
A_TAU = 16.0
A_CHUNK = 32
B_HEADS = 8
B_DH = 64
B_DV = 2 * B_DH
C_HEADS = 16
C_Q_RANK = 512
C_KV_RANK = 256
C_NOPE = 128
C_ROPE = 64
C_DV = 128
N_GROUPS = 4
EXPERTS_PER_GROUP = 4
N_EXPERTS = N_GROUPS * EXPERTS_PER_GROUP
EXPERT_TOP_K = 2
D_EXPERT = 512

ROPE_THETA = 10000.0
Q_BLOCK = 128
LN_EPS = 1e-5
RMS_EPS = 1e-6
DEEPNORM_ALPHA = (2.0 * DEPTH) ** 0.25
DEEPNORM_BETA = (8.0 * DEPTH) ** -0.25

AB_SIZES = (A_HEADS * A_DK, A_HEADS * A_DK, A_HEADS * A_DV, A_HEADS * A_DV, 2 * A_GATE_RANK,
            B_HEADS * 2 * B_DH, B_HEADS * 2 * B_DH, B_HEADS * B_DV)
AB_IN = sum(AB_SIZES)
AB_OUT = A_HEADS * A_DV + B_HEADS * B_DV
C_IN = C_Q_RANK + C_KV_RANK + C_ROPE

kernel_name = 'hybrid_gla_diffattn_mla_hmoe_diffusion_step'


def _split(x, sizes):
    out, start = [], 0
    for s in sizes:
        out.append(x[..., start:start + s])
        start += s
    return out


def layer_norm(x, g, b):
    xf = x.astype(jnp.float32)
    mu = jnp.mean(xf, axis=-1, keepdims=True)
    var = jnp.mean(jnp.square(xf - mu), axis=-1, keepdims=True)
    return ((xf - mu) * lax.rsqrt(var + LN_EPS) * g + b).astype(x.dtype)


def rms_norm(x, g):
    xf = x.astype(jnp.float32)
    return (xf * lax.rsqrt(jnp.mean(xf * xf, axis=-1, keepdims=True) + RMS_EPS) * g).astype(x.dtype)


def rope_tables(n_rows, dim):
    rows = jnp.repeat(jnp.arange(n_rows, dtype=jnp.float32), GRID_W)
    cols = jnp.tile(jnp.arange(GRID_W, dtype=jnp.float32), n_rows)
    quarter = dim // 4
    freqs = ROPE_THETA ** (-jnp.arange(quarter, dtype=jnp.float32) / quarter)
    ang = jnp.concatenate([rows[:, None] * freqs, cols[:, None] * freqs], axis=-1)
    return jnp.cos(ang), jnp.sin(ang)


def apply_rope(x, cos, sin):
    half = x.shape[-1] // 2
    shape = (cos.shape[0],) + (1,) * (x.ndim - 3) + (half,)
    cos, sin = cos.reshape(shape), sin.reshape(shape)
    xf = x.astype(jnp.float32)
    x1, x2 = xf[..., :half], xf[..., half:]
    return jnp.concatenate([x1 * cos - x2 * sin, x1 * sin + x2 * cos], axis=-1).astype(x.dtype)


def gla_chunked(q, k, v, log_a, s0):
    bsz, t, h, dk = q.shape
    dv = v.shape[-1]
    n = t // A_CHUNK
    f32 = jnp.float32
    qc = q.astype(f32).reshape(bsz, n, A_CHUNK, h, dk)
    kc = k.astype(f32).reshape(bsz, n, A_CHUNK, h, dk)
    vc = v.astype(f32).reshape(bsz, n, A_CHUNK, h, dv)
    b = jnp.cumsum(log_a.astype(f32).reshape(bsz, n, A_CHUNK, h, dk), axis=2)
    causal = jnp.tril(jnp.ones((A_CHUNK, A_CHUNK), dtype=bool))[:, :, None, None]
    rel = b[:, :, :, None] - b[:, :, None, :]
    decay = jnp.where(causal, jnp.exp(jnp.where(causal, rel, 0.0)), 0.0)
    scores = jnp.einsum('bnthk,bnshk,bntshk->bnhts', qc, kc, decay)
    o_intra = jnp.einsum('bnhts,bnshv->bnthv', scores, vc)
    b_last = b[:, :, -1]
    k_tail = kc * jnp.exp(b_last[:, :, None] - b)
    delta = jnp.einsum('bnshk,bnshv->bnhkv', k_tail, vc)
    chunk_decay = jnp.exp(b_last)

    def step(s, inp):
        dec, dlt = inp
        return dec[..., None] * s + dlt, s

    s_final, s_prev = lax.scan(step, s0.astype(f32),
                               (jnp.moveaxis(chunk_decay, 1, 0), jnp.moveaxis(delta, 1, 0)))
    s_prev = jnp.moveaxis(s_prev, 0, 1)
    o_inter = jnp.einsum('bnthk,bnhkv->bnthv', qc * jnp.exp(b), s_prev)
    o = (o_intra + o_inter).reshape(bsz, t, h, dv)
    return o.astype(v.dtype), s_final.astype(s0.dtype)


def diff_attention(q, k, v, lam):
    bsz, tq, nh = q.shape[:3]
    nb = tq // Q_BLOCK
    qb = jnp.moveaxis(q.reshape((bsz, nb, Q_BLOCK) + q.shape[2:]), 1, 0)
    scale = B_DH ** -0.5

    def block(qi):
        s = jnp.einsum('bqhjd,bkhjd->bhjqk', qi, k).astype(jnp.float32) * scale
        p = jax.nn.softmax(s, axis=-1)
        wgt = p[:, :, 0] - lam * p[:, :, 1]
        return jnp.einsum('bhqk,bkhv->bqhv', wgt.astype(v.dtype), v)

    o = lax.map(block, qb)
    return jnp.moveaxis(o, 0, 1).reshape(bsz, tq, nh, v.shape[-1])


def softmax_attention(q, k, v, scale):
    bsz, tq, nh = q.shape[:3]
    nb = tq // Q_BLOCK
    qb = jnp.moveaxis(q.reshape((bsz, nb, Q_BLOCK) + q.shape[2:]), 1, 0)

    def block(qi):
        s = jnp.einsum('bqhd,bkhd->bhqk', qi, k).astype(jnp.float32) * scale
        p = jax.nn.softmax(s, axis=-1)
        return jnp.einsum('bhqk,bkhv->bqhv', p.astype(v.dtype), v)

    o = lax.map(block, qb)
    return jnp.moveaxis(o, 0, 1).reshape(bsz, tq, nh, v.shape[-1])


def mixer_ab(h, w_in, w_g2, b_g2, gla_g, lam_vec, diff_g, w_out, lam_init, rope, ctx):
    bsz, t, _ = h.shape
    gq, gk, gv, gr, gg, dq, dk, dv = _split(h @ w_in, AB_SIZES)
    q = gq.reshape(bsz, t, A_HEADS, A_DK) * (A_DK ** -0.5)
    k = gk.reshape(bsz, t, A_HEADS, A_DK)
    v = gv.reshape(bsz, t, A_HEADS, A_DV)
    gate_logit = jnp.einsum('btjr,jrk->btjk', gg.reshape(bsz, t, 2, A_GATE_RANK), w_g2) + b_g2
    log_a = (jax.nn.log_sigmoid(gate_logit.astype(jnp.float32)) / A_TAU).reshape(bsz, t, 2, A_HEADS, A_DK)
    if ctx is None:
        s0 = jnp.zeros((bsz, 2, A_HEADS, A_DK, A_DV), h.dtype)
    else:
        s0 = ctx[0]
    o_f, s_f = gla_chunked(q, k, v, log_a[:, :, 0], s0[:, 0])
    o_b, s_b = gla_chunked(q[:, ::-1], k[:, ::-1], v[:, ::-1], log_a[:, ::-1, 1], s0[:, 1])
    o_gla = rms_norm(o_f + o_b[:, ::-1], gla_g).reshape(bsz, t, A_HEADS * A_DV) * jax.nn.silu(gr)
    q2 = dq.reshape(bsz, t, B_HEADS, 2, B_DH)
    k2 = dk.reshape(bsz, t, B_HEADS, 2, B_DH)
    v2 = dv.reshape(bsz, t, B_HEADS, B_DV)
    if ctx is None:
        keys, vals = k2, v2
    else:
        q2 = apply_rope(q2, rope[0], rope[1])
        keys = jnp.concatenate([ctx[1], apply_rope(k2, rope[0], rope[1])], axis=1)
        vals = jnp.concatenate([ctx[2], v2], axis=1)
    lv = lam_vec.astype(jnp.float32)
    lam = jnp.exp(jnp.sum(lv[0] * lv[1])) - jnp.exp(jnp.sum(lv[2] * lv[3])) + lam_init
    o_diff = rms_norm(diff_attention(q2, keys, vals, lam), diff_g) * (1.0 - lam_init)
    out = jnp.concatenate([o_gla, o_diff.reshape(bsz, t, B_HEADS * B_DV)], axis=-1) @ w_out
    return out, (jnp.stack([s_f, s_b], axis=1), k2, v2)


def mixer_mla(h, w_in, q_g, w_uq, kv_g, w_ukv, w_out, rope, ctx):
    bsz, t, _ = h.shape
    cq, ckv, krope = _split(h @ w_in, (C_Q_RANK, C_KV_RANK, C_ROPE))
    q = (rms_norm(cq, q_g) @ w_uq).reshape(bsz, t, C_HEADS, C_NOPE + C_ROPE)
    ckv = rms_norm(ckv, kv_g)
    if ctx is None:
        ckv_all, krope_all = ckv, krope
    else:
        q = jnp.concatenate([q[..., :C_NOPE], apply_rope(q[..., C_NOPE:], rope[0], rope[1])], axis=-1)
        krope_lat = apply_rope(krope[:, :, None, :], rope[0], rope[1])[:, :, 0]
        ckv_all = jnp.concatenate([ctx[0], ckv], axis=1)
        krope_all = jnp.concatenate([ctx[1], krope_lat], axis=1)
    tk = ckv_all.shape[1]
    kv = (ckv_all @ w_ukv).reshape(bsz, tk, C_HEADS, C_NOPE + C_DV)
    k = jnp.concatenate([kv[..., :C_NOPE],
                         jnp.broadcast_to(krope_all[:, :, None, :], (bsz, tk, C_HEADS, C_ROPE))], axis=-1)
    o = softmax_attention(q, k, kv[..., C_NOPE:], (C_NOPE + C_ROPE) ** -0.5)
    return o.reshape(bsz, t, C_HEADS * C_DV) @ w_out, (ckv, krope)


def hier_moe(h, w_rg, b_rg, w_re, b_re, w_gate, w_up, w_down):
    bsz, t, d = h.shape
    xt = h.reshape(bsz * t, d)
    g_logits = (xt @ w_rg + b_rg).astype(jnp.float32)
    g_prob = jax.nn.softmax(g_logits, axis=-1)
    g_sel = jnp.argmax(g_logits, axis=-1)
    g_w = jnp.take_along_axis(g_prob, g_sel[:, None], axis=-1)
    e_logits = (xt @ w_re + b_re).astype(jnp.float32).reshape(-1, N_GROUPS, EXPERTS_PER_GROUP)
    e_in_group = jnp.take_along_axis(e_logits, g_sel[:, None, None], axis=1)[:, 0]
    top_p, top_i = lax.top_k(jax.nn.softmax(e_in_group, axis=-1), EXPERT_TOP_K)
    top_w = g_w * top_p / jnp.sum(top_p, axis=-1, keepdims=True)
    expert_id = g_sel[:, None] * EXPERTS_PER_GROUP + top_i
    combine = jnp.einsum('tk,tke->te', top_w, jax.nn.one_hot(expert_id, N_EXPERTS, dtype=jnp.float32))
    gate = jnp.einsum('td,edf->tef', xt, w_gate)
    up = jnp.einsum('td,edf->tef', xt, w_up)
    act = jax.nn.silu(gate) * up * combine[:, :, None].astype(h.dtype)
    return jnp.einsum('tef,efd->td', act, w_down).reshape(bsz, t, d)


def run_trunk(x, cond, rope, caches, w):
    gla_states, diff_ks, diff_vs, mla_ckvs, mla_kropes = [], [], [], [], []
    for layer in range(DEPTH):
        mod = jax.nn.silu(cond) @ w['ada_w'][layer] + w['ada_b'][layer]
        shift1, scale1, gate1, shift2, scale2, gate2 = jnp.split(mod[:, None, :], 6, axis=-1)
        h = x * (1.0 + scale1) + shift1
        i = layer // 2
        if layer % 2 == 0:
            ctx = None if caches is None else (caches[0][:, i], caches[1][:, i], caches[2][:, i])
            y, (st, kk, vv) = mixer_ab(h, w['ab_w_in'][i], w['gla_w_gate2'][i], w['gla_b_gate2'][i],
                                       w['gla_norm_g'][i], w['diff_lambda'][i], w['diff_norm_g'][i],
                                       w['ab_w_out'][i], 0.8 - 0.6 * math.exp(-0.3 * layer),
                                       None if rope is None else rope[0], ctx)
            gla_states.append(st)
            diff_ks.append(kk)
            diff_vs.append(vv)
        else:
            ctx = None if caches is None else (caches[3][:, i], caches[4][:, i])
            y, (ckv, kr) = mixer_mla(h, w['mla_w_in'][i], w['mla_q_norm_g'][i], w['mla_w_uq'][i],
                                     w['mla_kv_norm_g'][i], w['mla_w_ukv'][i], w['mla_w_out'][i],
                                     None if rope is None else rope[1], ctx)
            mla_ckvs.append(ckv)
            mla_kropes.append(kr)
        x = layer_norm(DEEPNORM_ALPHA * x + gate1 * y, w['ln_g'][layer, 0], w['ln_b'][layer, 0])
        h = x * (1.0 + scale2) + shift2
        y = hier_moe(h, w['moe_w_rg'][layer], w['moe_b_rg'][layer], w['moe_w_re'][layer],
                     w['moe_b_re'][layer], w['moe_w_gate'][layer], w['moe_w_up'][layer], w['moe_w_down'][layer])
        x = layer_norm(DEEPNORM_ALPHA * x + gate2 * y, w['ln_g'][layer, 1], w['ln_b'][layer, 1])
    states = (jnp.stack(gla_states, axis=1), jnp.stack(diff_ks, axis=1), jnp.stack(diff_vs, axis=1),
              jnp.stack(mla_ckvs, axis=1), jnp.stack(mla_kropes, axis=1))
    return x, states


def setup_inputs(seed: int = 0) -> dict:
    key = jax.random.key(seed)
    keys = iter(jax.random.split(key, 64))
    d = D_MODEL

    def nrm(shape, scale):
        return jax.random.normal(next(keys), shape, jnp.float32) * scale

    return {
        'x_prompt': nrm((BATCH, SEQ, d), 1.0),
        'x_sample': nrm((DEC_BATCH, DEC_SEQ, d), 1.0),
        'state_gla': nrm((DEC_BATCH, N_AB_LAYERS, 2, A_HEADS, A_DK, A_DV), 1.0),
        'cache_diff_k': nrm((DEC_BATCH, N_AB_LAYERS, PAST_LEN, B_HEADS, 2, B_DH), 1.0),
        'cache_diff_v': nrm((DEC_BATCH, N_AB_LAYERS, PAST_LEN, B_HEADS, B_DV), 1.0),
        'cache_mla_ckv': nrm((DEC_BATCH, N_C_LAYERS, PAST_LEN, C_KV_RANK), 1.0),
        'cache_mla_krope': nrm((DEC_BATCH, N_C_LAYERS, PAST_LEN, C_ROPE), 1.0),
        'c': nrm((DEC_BATCH, d), 1.0),
        'c_ctx': nrm((d,), 1.0),
        'ada_w': nrm((DEPTH, d, 6 * d), 0.5 * d ** -0.5),
        'ada_b': nrm((DEPTH, 6 * d), 0.02),
        'ln_g': 1.0 + nrm((DEPTH, 2, d), 0.02),
        'ln_b': nrm((DEPTH, 2, d), 0.02),
        'ab_w_in': nrm((N_AB_LAYERS, d, AB_IN), d ** -0.5),
        'gla_w_gate2': nrm((N_AB_LAYERS, 2, A_GATE_RANK, A_HEADS * A_DK), A_GATE_RANK ** -0.5),
        'gla_b_gate2': nrm((N_AB_LAYERS, 2, A_HEADS * A_DK), 0.5),
        'gla_norm_g': 1.0 + nrm((N_AB_LAYERS, A_DV), 0.02),
        'diff_lambda': nrm((N_AB_LAYERS, 4, B_DH), 0.1),
        'diff_norm_g': 1.0 + nrm((N_AB_LAYERS, B_DV), 0.02),
        'ab_w_out': nrm((N_AB_LAYERS, AB_OUT, d), DEEPNORM_BETA * AB_OUT ** -0.5),
        'mla_w_in': nrm((N_C_LAYERS, d, C_IN), d ** -0.5),
        'mla_q_norm_g': 1.0 + nrm((N_C_LAYERS, C_Q_RANK), 0.02),
        'mla_w_uq': nrm((N_C_LAYERS, C_Q_RANK, C_HEADS * (C_NOPE + C_ROPE)), C_Q_RANK ** -0.5),
        'mla_kv_norm_g': 1.0 + nrm((N_C_LAYERS, C_KV_RANK), 0.02),
        'mla_w_ukv': nrm((N_C_LAYERS, C_KV_RANK, C_HEADS * (C_NOPE + C_DV)), C_KV_RANK ** -0.5),
        'mla_w_out': nrm((N_C_LAYERS, C_HEADS * C_DV, d), DEEPNORM_BETA * (C_HEADS * C_DV) ** -0.5),
        'moe_w_rg': nrm((DEPTH, d, N_GROUPS), d ** -0.5),
        'moe_b_rg': nrm((DEPTH, N_GROUPS), 0.01),
        'moe_w_re': nrm((DEPTH, d, N_EXPERTS), d ** -0.5),
        'moe_b_re': nrm((DEPTH, N_EXPERTS), 0.01),
        'moe_w_gate': nrm((DEPTH, N_EXPERTS, d, D_EXPERT), d ** -0.5),
        'moe_w_up': nrm((DEPTH, N_EXPERTS, d, D_EXPERT), d ** -0.5),
        'moe_w_down': nrm((DEPTH, N_EXPERTS, D_EXPERT, d), DEEPNORM_BETA * D_EXPERT ** -0.5),
    }


def reference(x_prompt, x_sample, state_gla, cache_diff_k, cache_diff_v, cache_mla_ckv, cache_mla_krope,
              c, c_ctx, ada_w, ada_b, ln_g, ln_b, ab_w_in, gla_w_gate2, gla_b_gate2, gla_norm_g,
              diff_lambda, diff_norm_g, ab_w_out, mla_w_in, mla_q_norm_g, mla_w_uq, mla_kv_norm_g,
              mla_w_ukv, mla_w_out, moe_w_rg, moe_b_rg, moe_w_re, moe_b_re, moe_w_gate, moe_w_up,
              moe_w_down):
    w = {
        'ada_w': ada_w, 'ada_b': ada_b, 'ln_g': ln_g, 'ln_b': ln_b,
        'ab_w_in': ab_w_in, 'gla_w_gate2': gla_w_gate2, 'gla_b_gate2': gla_b_gate2,
        'gla_norm_g': gla_norm_g, 'diff_lambda': diff_lambda, 'diff_norm_g': diff_norm_g,
        'ab_w_out': ab_w_out, 'mla_w_in': mla_w_in, 'mla_q_norm_g': mla_q_norm_g, 'mla_w_uq': mla_w_uq,
        'mla_kv_norm_g': mla_kv_norm_g, 'mla_w_ukv': mla_w_ukv, 'mla_w_out': mla_w_out,
        'moe_w_rg': moe_w_rg, 'moe_b_rg': moe_b_rg, 'moe_w_re': moe_w_re, 'moe_b_re': moe_b_re,
        'moe_w_gate': moe_w_gate, 'moe_w_up': moe_w_up, 'moe_w_down': moe_w_down,
    }
    y_prompt, (new_gla, new_diff_k, new_diff_v, new_mla_ckv, new_mla_krope) = run_trunk(
        x_prompt, c_ctx[None, :], None, None, w)
    n_rows = x_sample.shape[1] // GRID_W
    rope = (rope_tables(n_rows, B_DH), rope_tables(n_rows, C_ROPE))
    caches = (state_gla, cache_diff_k, cache_diff_v, cache_mla_ckv, cache_mla_krope)
    y_sample, _ = run_trunk(x_sample, c, rope, caches, w)
    return (y_prompt, y_sample, new_gla, new_diff_k, new_diff_v, new_mla_ckv, new_mla_krope)
```

```python
import contextlib
import math
import numpy as np
import concourse.bass as bass
import concourse.mybir as mybir
from concourse.bass_utils import run_bass_kernel_spmd

F32 = mybir.dt.float32
BF16 = mybir.dt.bfloat16
AF = mybir.ActivationFunctionType
ALU = mybir.AluOpType
AX = mybir.AxisListType

D = 2048
KC = 16
T = 1024
NT = 8
DEPTH = 4
PAST = 256
NKEY = PAST + T
NKT = NKEY // 128
N_EXP = 16
D_EXP = 512
AB_IN = 6176
C_IN = 832
ALPHA = (2.0 * DEPTH) ** 0.25
LN_EPS = 1e-5
RMS_EPS = 1e-6
NEG = -30000.0


class Dep:
    __slots__ = ("w", "r", "excl")

    def __init__(self, excl=False):
        self.w = None
        self.r = {}
        self.excl = excl


class Eng:
    def __init__(self, K, e, name, own_wait=True):
        self.e = e
        self.name = name
        self.sem = K.es.enter_context(K.nc.semaphore("s_" + name))
        self.cnt = 0
        self.waited = {}
        self.own_wait = own_wait

    def wait(self, ev):
        if ev is None:
            return
        sem, val = ev
        if sem is self.sem and not self.own_wait:
            return
        key = id(sem)
        if self.waited.get(key, 0) >= val:
            return
        self.e.wait_ge(sem, val)
        self.waited[key] = val


class DmaQ:
    def __init__(self, K, eng, nsem, name):
        self.eng = eng
        self.sems = [K.es.enter_context(K.nc.semaphore(f"d_{name}{i}")) for i in range(nsem)]
        self.vals = [0] * nsem
        self.i = 0

    def next_sem(self):
        i = self.i
        self.i = (self.i + 1) % len(self.sems)
        if self.vals[i] > 0:
            self.eng.wait((self.sems[i], self.vals[i]))
        return i


class Kern:
    def __init__(self, nc):
        self.nc = nc
        self.es = contextlib.ExitStack()
        self.pe = Eng(self, nc.tensor, "pe", own_wait=False)
        self.act = Eng(self, nc.scalar, "act")
        self.dve = Eng(self, nc.vector, "dve")
        self.pool = Eng(self, nc.gpsimd, "pool")
        self.sp = Eng(self, nc.sync, "sp")
        self.engs = [self.pe, self.act, self.dve, self.pool, self.sp]
        self.q_sync = DmaQ(self, self.sp, 28, "sy")
        self.q_pool = DmaQ(self, self.pool, 28, "po")

    def _pre(self, eng, reads, writes):
        for d in reads:
            eng.wait(d.w)
            if d.excl:
                for ev in list(d.r.values()):
                    if ev[0] is not eng.sem:
                        eng.wait(ev)
        for d in writes:
            eng.wait(d.w)
            for ev in list(d.r.values()):
                eng.wait(ev)

    def _post(self, ev, reads, writes):
        sem, v = ev
        for d in reads:
            d.r[id(sem)] = ev
        for d in writes:
            d.w = ev
            d.r = {}

    def op(self, eng, fn, reads=(), writes=()):
        self._pre(eng, reads, writes)
        ins = fn(eng.e)
        eng.cnt += 1
        ins.then_inc(eng.sem, 1)
        ev = (eng.sem, eng.cnt)
        self._post(ev, reads, writes)
        return ev

    def dma(self, q, out, in_, reads=(), writes=()):
        eng = q.eng
        self._pre(eng, reads, writes)
        i = q.next_sem()
        ins = eng.e.dma_start(out=out, in_=in_)
        ins.then_inc(q.sems[i], 16)
        q.vals[i] += 16
        ev = (q.sems[i], q.vals[i])
        self._post(ev, reads, writes)
        return ev

    def all_events(self):
        evs = [(e.sem, e.cnt) for e in self.engs if e.cnt > 0]
        for q in (self.q_sync, self.q_pool):
            for s, v in zip(q.sems, q.vals):
                if v > 0:
                    evs.append((s, v))
        return evs

    def barrier(self):
        evs = self.all_events()
        for e in self.engs:
            for ev in evs:
                e.wait(ev)

    def finish(self):
        for ev in self.all_events():
            self.sp.wait(ev)


class Mem:
    def __init__(self, big, nbytes):
        self.big = big
        self.n = nbytes
        self.top = 0
        self.peak = 0

    def alloc(self, shape, dtype=F32):
        esz = 4 if dtype == F32 else 2
        nfree = 1
        for s in shape[1:]:
            nfree *= s
        nb = nfree * esz
        off = (self.top + 63) // 64 * 64
        self.top = off + nb
        self.peak = max(self.peak, self.top)
        assert self.top <= self.n, f"SBUF overflow {self.top} > {self.n}"
        ap = self.big[:, off // 2:(off + nb) // 2]
        if dtype == F32:
            ap = ap.bitcast(F32)
        if len(shape) > 2:
            names = [f"d{i}" for i in range(len(shape) - 1)]
            kw = {n: s for n, s in zip(names[:-1], shape[1:-1])}
            ap = ap.rearrange(f"p ({' '.join(names)}) -> p {' '.join(names)}", **kw)
        if shape[0] < 128:
            ap = ap[0:shape[0]]
        return ap


class Ring:
    def __init__(self, K, mem, nslot, slot_elems):
        self.K = K
        self.slots = [mem.alloc([128, slot_elems], BF16) for _ in range(nslot)]
        self.deps = [Dep() for _ in range(nslot)]
        self.i = 0

    def load(self, srcs):
        i = self.i
        self.i = (self.i + 1) % len(self.slots)
        views = []
        off = 0
        for src, a, b in srcs:
            v = self.slots[i][:, off:off + a * b].rearrange("p (a b) -> p a b", a=a)
            self.K.dma(self.K.q_pool, v, src, writes=[self.deps[i]])
            views.append(v)
            off += a * b
        return views, self.deps[i]


def build(cfg):
    depth = cfg.get("depth", DEPTH)
    n_exp = cfg.get("n_exp", N_EXP)
    do_mixer = cfg.get("mixer", True)
    nc = bass.Bass("TRN2", target_bir_lowering=False)
    K = Kern(nc)
    es = K.es

    def din(name, shape):
        return nc.dram_tensor(name, list(shape), F32, kind="ExternalInput").ap()

    def dout(name, shape):
        return nc.dram_tensor(name, list(shape), F32, kind="ExternalOutput").ap()

    x_in = din("x_in", [T, D])
    cond = din("cond", [D])
    st_gla = din("st_gla", [2, 2, 4, 128, 256])
    c_dk = din("c_dk", [2, PAST, 1024])
    c_dv = din("c_dv", [2, PAST, 1024])
    c_ckv = din("c_ckv", [2, PAST, 256])
    c_kr = din("c_kr", [2, PAST, 64])
    rope_cos = din("rope_cos", [T, 32])
    rope_sin = din("rope_sin", [T, 32])
    ropeT_c = din("ropeT_c", [64, T])
    ropeT_s = din("ropeT_s", [64, T])
    seqsel = din("seqsel", [4, T])
    kmask = din("kmask", [4, NKEY])
    keep = din("keep", [128, 1])
    identF_d = din("identF", [128, 128])
    glam_d = din("glam", [6, 128, 128])
    csel_d = din("csel", [128, 4])
    dl = max(depth, 1)
    nab = max((depth + 1) // 2, 1)
    ncl = max(depth // 2, 1)
    ada_w = din("ada_w", [dl, D, 6 * D])
    ada_b = din("ada_b", [dl, 6 * D])
    ln_g = din("ln_g", [dl, 2, D])
    ln_b = din("ln_b", [dl, 2, D])
    ab_w_in = din("ab_w_in", [nab, D, AB_IN])
    gla_w_gate2 = din("gla_w_gate2", [nab, 2, 16, 512])
    gla_b_gate2 = din("gla_b_gate2", [nab, 2, 512])
    gla_norm_g = din("gla_norm_g", [nab, 256])
    diff_lambda = din("diff_lambda", [nab, 4, 64])
    diff_norm_g = din("diff_norm_g", [nab, 128])
    ab_w_out = din("ab_w_out", [nab, D, D])
    mla_w_in = din("mla_w_in", [ncl, D, C_IN])
    mla_q_norm_g = din("mla_q_norm_g", [ncl, 512])
    mla_w_uq = din("mla_w_uq", [ncl, 512, 3072])
    mla_kv_norm_g = din("mla_kv_norm_g", [ncl, 256])
    mla_w_ukv = din("mla_w_ukv", [ncl, 256, 4096])
    mla_w_out = din("mla_w_out", [ncl, D, D])
    moe_w_rg = din("moe_w_rg", [dl, D, 4])
    moe_b_rg = din("moe_b_rg", [dl, 4])
    moe_w_re = din("moe_w_re", [dl, D, 16])
    moe_b_re = din("moe_b_re", [dl, 16])
    ne = max(n_exp, 1)
    moe_w_gate = din("moe_w_gate", [dl, ne, D, D_EXP])
    moe_w_up = din("moe_w_up", [dl, ne, D, D_EXP])
    moe_w_down = din("moe_w_down", [dl, ne, D_EXP, D])

    y_out = dout("y", [T, D])
    o_gla = dout("o_gla", [2, 4, 2, 4, 128, 256])
    o_dk = dout("o_dk", [2, T, 1024])
    o_dv = dout("o_dv", [2, T, 1024])
    o_ckv = dout("o_ckv", [2, T, 256])
    o_kr = dout("o_kr", [2, T, 64])
    x_spill = nc.dram_tensor("x_spill", [T, D], F32, kind="Internal").ap()

    SB_BYTES = 207 * 1024
    big = es.enter_context(nc.sbuf_tensor("big", [128, SB_BYTES // 2], BF16))
    mem = Mem(big, SB_BYTES)
    ps = es.enter_context(nc.psum_tensor("ps", [128, 4096], F32))
    dps = [Dep(excl=True) for _ in range(8)]

    def bank(i, n=512, off=0):
        return ps[:, i * 512 + off:i * 512 + off + n]

    def mm_group(out_ap, pairs, reads, writes):
        def fn(e):
            n = len(pairs)
            ins = None
            for i, (l, r) in enumerate(pairs):
                ins = e.matmul(out_ap, lhsT=l, rhs=r, start=(i == 0), stop=(i == n - 1))
            return ins
        return K.op(K.pe, fn, reads, writes)

    identF = mem.alloc([128, 128], F32)
    identB = mem.alloc([128, 128], BF16)
    S_rep = mem.alloc([128, KC, 128], BF16)
    modcol = mem.alloc([128, 4, KC], F32)
    adab_col = mem.alloc([128, 96], F32)
    gate_bc = mem.alloc([128, D], F32)
    smallc = mem.alloc([128, 64], F32)
    d_identF, d_identB, d_Srep, d_modcol, d_adab, d_gate, d_small = (Dep() for _ in range(7))

    X_OFF = (mem.top + 63) // 64 * 64
    X = mem.alloc([128, NT, D], F32)
    dX = [Dep() for _ in range(NT)]
    PH_X = mem.top
    PH_NOX = X_OFF

    K.dma(K.q_sync, identF, identF_d, writes=[d_identF])
    K.op(K.dve, lambda e: e.tensor_copy(out=identB, in_=identF), reads=[d_identF], writes=[d_identB])
    xv = x_in.rearrange("(t p) d -> p t d", p=128)
    for t in range(NT):
        K.dma(K.q_sync, X[:, t, :], xv[:, t, :], writes=[dX[t]])

    mem.top = PH_X
    c16 = mem.alloc([16, 128], F32)
    scol = mem.alloc([128, KC], F32)
    d_c16, d_scol = Dep(), Dep()
    K.dma(K.q_sync, c16, cond.rearrange("(c p) -> c p", p=128), writes=[d_c16])
    K.op(K.pe, lambda e: e.transpose(out=bank(0, 16), in_=c16, identity=identF[0:16, 0:16]),
         reads=[d_c16, d_identF], writes=[dps[0]])
    K.op(K.act, lambda e: e.activation(out=scol, in_=bank(0, 16), func=AF.Silu), reads=[dps[0]], writes=[d_scol])
    K.op(K.dve, lambda e: e.tensor_copy(out=S_rep, in_=scol.unsqueeze(2).to_broadcast([128, KC, 128])),
         reads=[d_scol], writes=[d_Srep])
    K.barrier()
    mem.top = PH_X

    def emit_mod(l, half, ring):
        adaw_v = ada_w[l].rearrange("(c p) n -> p c n", p=128)
        if half == 0:
            ab96 = mem.alloc([96, 128], F32)
            d96 = Dep()
            K.dma(K.q_sync, ab96, ada_b[l].rearrange("(c p) -> c p", p=128), writes=[d96])
            K.op(K.pe, lambda e: e.transpose(out=bank(0, 96), in_=ab96, identity=identF[0:96, 0:96]),
                 reads=[d96, d_identF], writes=[dps[0]])
            K.op(K.act, lambda e: e.copy(out=adab_col, in_=bank(0, 96)), reads=[dps[0]], writes=[d_adab])
        tmp = mem.alloc([128, 256], F32)
        d_tmp = Dep()
        for si in range(3):
            seg = half * 3 + si
            if si == 2:
                K.dma(K.q_sync, gate_bc, ada_b[l, seg * D:(seg + 1) * D].partition_broadcast(128),
                      writes=[d_gate])
            for blk in range(8):
                c0 = seg * D + blk * 256
                (w,), dw = ring.load([(adaw_v[:, :, c0:c0 + 256], KC, 256)])
                pb = (blk + si * 8) % 2
                po = bank(pb, 256)
                mm_group(po, [(S_rep[:, kc, :], w[:, kc, :]) for kc in range(KC)],
                         reads=[d_Srep, dw], writes=[dps[pb]])
                if si < 2:
                    mc = half * 2 + si
                    K.op(K.dve, lambda e: e.tensor_tensor(
                        out=tmp.rearrange("p (c j) -> p c j", c=2), in0=po.rearrange("p (c j) -> p c j", c=2),
                        in1=identF.unsqueeze(1).to_broadcast([128, 2, 128]), op=ALU.mult),
                        reads=[dps[pb], d_identF], writes=[d_tmp])
                    K.op(K.dve, lambda e: e.tensor_reduce(
                        out=modcol[:, mc, blk * 2:blk * 2 + 2], in_=tmp.rearrange("p (c j) -> p c j", c=2),
                        axis=AX.X, op=ALU.add), reads=[d_tmp], writes=[d_modcol])
                else:
                    K.op(K.dve, lambda e: e.tensor_tensor(
                        out=gate_bc[:, blk * 256:(blk + 1) * 256], in0=gate_bc[:, blk * 256:(blk + 1) * 256],
                        in1=po, op=ALU.add), reads=[dps[pb], d_gate], writes=[d_gate])
            if si < 2:
                mc = half * 2 + si
                K.op(K.dve, lambda e: e.tensor_tensor(
                    out=modcol[:, mc, :], in0=modcol[:, mc, :], in1=adab_col[:, seg * KC:(seg + 1) * KC],
                    op=ALU.add), reads=[d_modcol, d_adab], writes=[d_modcol])
                if si == 1:
                    K.op(K.dve, lambda e: e.tensor_scalar(
                        out=modcol[:, mc, :], in0=modcol[:, mc, :], scalar1=1.0, scalar2=None, op0=ALU.add),
                        reads=[d_modcol], writes=[d_modcol])

    def emit_convert(half, hT, d_hT, router=None):
        sh, sc = half * 2, half * 2 + 1
        for t in range(NT):
            for g in range(4):
                pb = 2 + (t * 4 + g) % 4
                def tr(e, t=t, g=g, pb=pb):
                    ins = None
                    for j in range(4):
                        kc = g * 4 + j
                        ins = e.transpose(out=bank(pb, 128, j * 128), in_=X[:, t, kc * 128:(kc + 1) * 128],
                                          identity=identF)
                    return ins
                K.op(K.pe, tr, reads=[dX[t], d_identF], writes=[dps[pb]])
                for j in range(4):
                    kc = g * 4 + j
                    K.op(K.act, lambda e, kc=kc, j=j, pb=pb, t=t: e.activation(
                        out=hT[:, kc, t * 128:(t + 1) * 128], in_=bank(pb, 128, j * 128), func=AF.Identity,
                        scale=modcol[:, sc, kc:kc + 1], bias=modcol[:, sh, kc:kc + 1]),
                        reads=[dps[pb], d_modcol], writes=[d_hT[t]])
                    if router is not None:
                        hF, d_hF = router["hF"], router["d_hF"]
                        K.op(K.dve, lambda e, kc=kc, j=j, pb=pb: e.tensor_scalar(
                            out=hF[:, kc, :], in0=bank(pb, 128, j * 128), scalar1=modcol[:, sc, kc:kc + 1],
                            scalar2=modcol[:, sh, kc:kc + 1], op0=ALU.mult, op1=ALU.add),
                            reads=[dps[pb], d_modcol], writes=[d_hF])
            if router is not None:
                emit_route(t, router)

    def emit_route(t, R):
        hF, d_hF, wr, d_wr, comb, d_comb, rb, d_rb, rt, d_rt = (R[k] for k in (
            "hF", "d_hF", "wr", "d_wr", "comb", "d_comb", "rb", "d_rb", "rt", "d_rt"))
        mm_group(bank(1, 20), [(hF[:, kc, :], wr[:, kc, :]) for kc in range(KC)],
                 reads=[d_hF, d_wr], writes=[dps[1]])
        V = K.dve
        lg = rt[:, 0:20]
        def dv(fn, reads=(), writes=()):
            return K.op(V, fn, reads=list(reads) + [d_rt], writes=list(writes) + [d_rt])
        dv(lambda e: e.tensor_tensor(out=lg, in0=bank(1, 20), in1=rb, op=ALU.add), reads=[dps[1], d_rb])
        gl = rt[:, 0:4]
        el = rt[:, 4:20].rearrange("p (g j) -> p g j", g=4)
        gmax, gsum, m1, m2, e2, den, w1, w2 = (rt[:, 20 + i:21 + i] for i in range(8))
        ohg = rt[:, 32:36]
        tmp16 = rt[:, 36:52].rearrange("p (g j) -> p g j", g=4)
        esel = rt[:, 52:56]
        oh1 = rt[:, 56:60]
        msk = rt[:, 60:64]
        oh2 = rt[:, 64:68]
        cig = rt[:, 68:72]
        gex = rt[:, 72:76]
        dv(lambda e: e.reduce_max(out=gmax, in_=gl, axis=AX.X))
        dv(lambda e: e.tensor_scalar(out=ohg, in0=gl, scalar1=gmax, scalar2=None, op0=ALU.is_equal))
        dv(lambda e: e.tensor_scalar(out=gex, in0=gl, scalar1=gmax, scalar2=None, op0=ALU.subtract))
        K.op(K.act, lambda e: e.activation(out=gex, in_=gex, func=AF.Exp, accum_out=gsum),
             reads=[d_rt], writes=[d_rt])
        dv(lambda e: e.tensor_tensor(out=tmp16, in0=el, in1=ohg.unsqueeze(2).to_broadcast([128, 4, 4]),
                                     op=ALU.mult))
        dv(lambda e: e.tensor_reduce(out=esel, in_=tmp16.rearrange("p g j -> p j g"), axis=AX.X, op=ALU.add))
        dv(lambda e: e.reduce_max(out=m1, in_=esel, axis=AX.X))
        dv(lambda e: e.tensor_scalar(out=oh1, in0=esel, scalar1=m1, scalar2=None, op0=ALU.is_equal))
        dv(lambda e: e.scalar_tensor_tensor(out=msk, in0=oh1, scalar=-1e30, in1=esel, op0=ALU.mult, op1=ALU.add))
        dv(lambda e: e.reduce_max(out=m2, in_=msk, axis=AX.X))
        dv(lambda e: e.tensor_scalar(out=oh2, in0=msk, scalar1=m2, scalar2=None, op0=ALU.is_equal))
        dv(lambda e: e.tensor_tensor(out=e2, in0=m2, in1=m1, op=ALU.subtract))
        K.op(K.act, lambda e: e.activation(out=e2, in_=e2, func=AF.Exp), reads=[d_rt], writes=[d_rt])
        dv(lambda e: e.scalar_tensor_tensor(out=den, in0=e2, scalar=1.0, in1=gsum, op0=ALU.add, op1=ALU.mult))
        dv(lambda e: e.reciprocal(out=w1, in_=den))
        dv(lambda e: e.tensor_tensor(out=w2, in0=e2, in1=w1, op=ALU.mult))
        dv(lambda e: e.tensor_scalar(out=cig, in0=oh1, scalar1=w1, scalar2=None, op0=ALU.mult))
        dv(lambda e: e.scalar_tensor_tensor(out=cig, in0=oh2, scalar=w2, in1=cig, op0=ALU.mult, op1=ALU.add))
        K.op(V, lambda e: e.tensor_tensor(
            out=comb[:, t, :].rearrange("p (g j) -> p g j", g=4),
            in0=ohg.unsqueeze(2).to_broadcast([128, 4, 4]), in1=cig.unsqueeze(1).to_broadcast([128, 4, 4]),
            op=ALU.mult), reads=[d_rt], writes=[d_comb[t]])

    def emit_ln_tile(t, gb, d_gb, scr, d_scr):
        st, mv, cols = scr
        lnstop = cfg.get("lnstop", 99)
        if lnstop < 1:
            return
        for c in range(4):
            K.op(K.dve, lambda e, c=c: e.bn_stats(out=st[:, c, :], in_=X[:, t, c * 512:(c + 1) * 512]),
                 reads=[dX[t]], writes=[d_scr])
        K.op(K.dve, lambda e: e.bn_aggr(out=mv, in_=st), reads=[d_scr], writes=[d_scr])
        if lnstop < 2:
            return
        K.op(K.dve, lambda e: e.tensor_scalar(out=cols[:, 0:1], in0=mv[:, 1:2], scalar1=LN_EPS, scalar2=None,
                                              op0=ALU.add), reads=[d_scr], writes=[d_scr])
        K.op(K.act, lambda e: e.activation(out=cols[:, 0:1], in_=cols[:, 0:1], func=AF.Ln),
             reads=[d_scr], writes=[d_scr])
        K.op(K.act, lambda e: e.activation(out=cols[:, 0:1], in_=cols[:, 0:1], func=AF.Exp, scale=-0.5),
             reads=[d_scr], writes=[d_scr])
        K.op(K.dve, lambda e: e.scalar_tensor_tensor(out=cols[:, 1:2], in0=mv[:, 0:1], scalar=-1.0,
                                                     in1=cols[:, 0:1], op0=ALU.mult, op1=ALU.mult),
             reads=[d_scr], writes=[d_scr])
        if lnstop < 3:
            return
        K.op(K.act, lambda e: e.activation(out=X[:, t, :], in_=X[:, t, :], func=AF.Identity,
                                           scale=cols[:, 0:1], bias=cols[:, 1:2]),
             reads=[d_scr], writes=[dX[t]])
        if lnstop < 4:
            return
        K.op(K.dve, lambda e: e.tensor_tensor(out=X[:, t, :], in0=X[:, t, :], in1=gb[:, 0, :], op=ALU.mult),
             reads=[d_gb], writes=[dX[t]])
        K.op(K.dve, lambda e: e.tensor_tensor(out=X[:, t, :], in0=X[:, t, :], in1=gb[:, 1, :], op=ALU.add),
             reads=[d_gb], writes=[dX[t]])

    def load_ln(l, which):
        gb = mem.alloc([128, 2, D], F32)
        d_gb = Dep()
        K.dma(K.q_sync, gb[:, 0, :], ln_g[l, which].partition_broadcast(128), writes=[d_gb])
        K.dma(K.q_sync, gb[:, 1, :], ln_b[l, which].partition_broadcast(128), writes=[d_gb])
        st = mem.alloc([128, 4, 6], F32)
        mv = mem.alloc([128, 2], F32)
        cols = mem.alloc([128, 2], F32)
        return gb, d_gb, (st, mv, cols), Dep()

    def emit_moe(l):
        mem.top = PH_X
        ring = Ring(K, mem, 6, 4096)
        emit_mod(l, 1, ring)
        chk("mod1")
        hT = mem.alloc([128, KC, T], BF16)
        d_hT = [Dep() for _ in range(NT)]
        R = {}
        R["hF"] = mem.alloc([128, KC, 128], F32); R["d_hF"] = Dep()
        R["wr"] = mem.alloc([128, KC, 20], F32); R["d_wr"] = Dep()
        R["comb"] = mem.alloc([128, NT, 16], F32); R["d_comb"] = [Dep() for _ in range(NT)]
        R["rb"] = mem.alloc([128, 20], F32); R["d_rb"] = Dep()
        R["rt"] = mem.alloc([128, 80], F32); R["d_rt"] = Dep()
        K.dma(K.q_sync, R["wr"][:, :, 0:4], moe_w_rg[l].rearrange("(c p) n -> p c n", p=128), writes=[R["d_wr"]])
        K.dma(K.q_sync, R["wr"][:, :, 4:20], moe_w_re[l].rearrange("(c p) n -> p c n", p=128), writes=[R["d_wr"]])
        K.dma(K.q_sync, R["rb"][:, 0:4], moe_b_rg[l].partition_broadcast(128), writes=[R["d_rb"]])
        K.dma(K.q_sync, R["rb"][:, 4:20], moe_b_re[l].partition_broadcast(128), writes=[R["d_rb"]])
        emit_convert(1, hT, d_hT, router=R)
        comb, d_comb = R["comb"], R["d_comb"]
        chk("conv1")

        actT = mem.alloc([128, 4, T], BF16)
        d_act = [[Dep() for _ in range(2)] for _ in range(4)]
        sg = [mem.alloc([128, 512], BF16) for _ in range(2)]
        d_sg = [Dep(), Dep()]
        acc = [mem.alloc([128, 512], F32) for _ in range(2)]
        d_acc = [Dep(), Dep()]
        gb, d_gb, scr, d_scr = load_ln(l, 1)
        ui = 0
        di = 0
        for ei in range(n_exp):
            wg = moe_w_gate[l, ei].rearrange("(c p) f -> p c f", p=128)
            wu = moe_w_up[l, ei].rearrange("(c p) f -> p c f", p=128)
            wd = moe_w_down[l, ei].rearrange("(c p) d -> p c d", p=128)
            dsl = []
            for fp in range(2):
                (g_w,), d_g = ring.load([(wg[:, :, fp * 256:(fp + 1) * 256], KC, 256)])
                (u_w,), d_u = ring.load([(wu[:, :, fp * 256:(fp + 1) * 256], KC, 256)])
                for fi in range(2):
                    f = fp * 2 + fi
                    for hf in range(2):
                        pg, pu = 2 + (ui % 2) * 2, 3 + (ui % 2) * 2
                        ui += 1
                        tok = slice(hf * 512, (hf + 1) * 512)
                        rd = [d_hT[t] for t in range(hf * 4, hf * 4 + 4)]
                        mm_group(bank(pg), [(g_w[:, kc, fi * 128:(fi + 1) * 128], hT[:, kc, tok]) for kc in range(KC)],
                                 reads=rd + [d_g], writes=[dps[pg]])
                        mm_group(bank(pu), [(u_w[:, kc, fi * 128:(fi + 1) * 128], hT[:, kc, tok]) for kc in range(KC)],
                                 reads=rd + [d_u], writes=[dps[pu]])
                        s = ui % 2
                        K.op(K.act, lambda e, s=s, pg=pg: e.activation(out=sg[s], in_=bank(pg), func=AF.Silu),
                             reads=[dps[pg]], writes=[d_sg[s]])
                        K.op(K.dve, lambda e, s=s, pu=pu, f=f, tok=tok: e.tensor_tensor(
                            out=actT[:, f, tok], in0=sg[s], in1=bank(pu), op=ALU.mult),
                            reads=[d_sg[s], dps[pu]], writes=[d_act[f][hf]])
            for fp in range(2):
                (d_w,), d_d = ring.load([(wd[:, fp * 2:fp * 2 + 2, :], 2, D)])
                dsl.append((d_w, d_d))
            for t in range(NT):
                hf = t // 4
                for db in range(4):
                    pb = di % 2
                    di += 1
                    pairs = [(actT[:, f, t * 128:(t + 1) * 128], dsl[f // 2][0][:, f % 2, db * 512:(db + 1) * 512])
                             for f in range(4)]
                    mm_group(bank(pb), pairs, reads=[d_act[f][hf] for f in range(4)] + [dsl[0][1], dsl[1][1]],
                             writes=[dps[pb]])
                    a = di % 2
                    K.op(K.dve, lambda e, a=a, pb=pb, t=t, db=db, ei=ei: e.scalar_tensor_tensor(
                        out=acc[a], in0=bank(pb), scalar=comb[:, t, ei:ei + 1], in1=gate_bc[:, db * 512:(db + 1) * 512],
                        op0=ALU.mult, op1=ALU.mult), reads=[dps[pb], d_comb[t], d_gate], writes=[d_acc[a]])
                    xs = X[:, t, db * 512:(db + 1) * 512]
                    if ei == 0:
                        K.op(K.dve, lambda e, a=a, xs=xs: e.scalar_tensor_tensor(
                            out=xs, in0=xs, scalar=ALPHA, in1=acc[a], op0=ALU.mult, op1=ALU.add),
                            reads=[d_acc[a]] + d_hT, writes=[dX[t]])
                    else:
                        K.op(K.dve, lambda e, a=a, xs=xs: e.tensor_tensor(out=xs, in0=xs, in1=acc[a], op=ALU.add),
                             reads=[d_acc[a]], writes=[dX[t]])
        chk("moe")
        for t in range(NT):
            emit_ln_tile(t, gb, d_gb, scr, d_scr)
        K.barrier()
        mem.top = PH_X


    def rstd_from_ss(col, n, dep):
        K.op(K.dve, lambda e: e.tensor_scalar(out=col, in0=col, scalar1=1.0 / n, scalar2=RMS_EPS,
                                              op0=ALU.mult, op1=ALU.add), reads=[dep], writes=[dep])
        K.op(K.act, lambda e: e.activation(out=col, in_=col, func=AF.Ln), reads=[dep], writes=[dep])
        K.op(K.act, lambda e: e.activation(out=col, in_=col, func=AF.Exp, scale=-0.5), reads=[dep], writes=[dep])

    def emit_out_proj(l, w_out_l, oT, d_oT, ring):
        wv = w_out_l.rearrange("(c p) n -> p c n", p=128)
        tmpo = [mem.alloc([128, 256], F32) for _ in range(2)]
        d_tmpo = [Dep(), Dep()]
        gb, d_gb, scr, d_scr = load_ln(l, 0)
        n = 0
        for blk in range(8):
            (w,), dw = ring.load([(wv[:, :, blk * 256:(blk + 1) * 256], KC, 256)])
            for t in range(NT):
                pb = n % 4
                a = n % 2
                n += 1
                mm_group(bank(pb, 256), [(oT[:, kc, t * 128:(t + 1) * 128], w[:, kc, :]) for kc in range(KC)],
                         reads=[d_oT, dw], writes=[dps[pb]])
                K.op(K.dve, lambda e, a=a, pb=pb, blk=blk: e.tensor_tensor(
                    out=tmpo[a], in0=bank(pb, 256), in1=gate_bc[:, blk * 256:(blk + 1) * 256], op=ALU.mult),
                    reads=[dps[pb], d_gate], writes=[d_tmpo[a]])
                xs = X[:, t, blk * 256:(blk + 1) * 256]
                K.op(K.dve, lambda e, a=a, xs=xs: e.scalar_tensor_tensor(
                    out=xs, in0=xs, scalar=ALPHA, in1=tmpo[a], op0=ALU.mult, op1=ALU.add),
                    reads=[d_tmpo[a]], writes=[dX[t]])
        for t in range(NT):
            emit_ln_tile(t, gb, d_gb, scr, d_scr)

    def attn_scores(sb0, qparts, kparts):
        deps_r = []
        for (q, dq), (k, dk_) in zip(qparts, kparts):
            deps_r += [dq, dk_]
        for j, (c0, n) in enumerate(((0, 512), (512, 512), (1024, 256))):
            mm_group(bank(sb0 + j, n), [(q, k[:, c0:c0 + n]) for (q, _), (k, _) in zip(qparts, kparts)],
                     reads=deps_r, writes=[dps[sb0 + j]])

    def softmax_exp(sb0, scale, e_out, d_e, cols, d_cols, ci):
        sc = ps[:, sb0 * 512: sb0 * 512 + NKEY]
        rd = [dps[sb0], dps[sb0 + 1], dps[sb0 + 2]]
        K.op(K.dve, lambda e: e.reduce_max(out=cols[:, ci:ci + 1], in_=sc, axis=AX.X), reads=rd, writes=[d_cols])
        K.op(K.dve, lambda e: e.tensor_scalar(out=cols[:, ci:ci + 1], in0=cols[:, ci:ci + 1], scalar1=-scale,
                                              scalar2=None, op0=ALU.mult), reads=[d_cols], writes=[d_cols])
        K.op(K.act, lambda e: e.activation(out=e_out, in_=sc, func=AF.Exp, scale=scale, bias=cols[:, ci:ci + 1],
                                           accum_out=cols[:, ci + 1:ci + 2]), reads=rd + [d_cols], writes=[d_e, d_cols])

    def attn_pv(e_in, d_e, eT, d_eT, V, d_V, vcol0, out_ps_off):
        pT6 = bank(6).bitcast(BF16)
        pT7 = bank(7, 128).bitcast(BF16)
        def tr(e):
            ins = None
            for kt in range(NKT):
                dst = pT6[:, kt * 128:(kt + 1) * 128] if kt < 8 else pT7[:, (kt - 8) * 128:(kt - 7) * 128]
                ins = e.transpose(out=dst, in_=e_in[:, kt * 128:(kt + 1) * 128], identity=identB)
            return ins
        K.op(K.pe, tr, reads=[d_e, d_identB], writes=[dps[6], dps[7]])
        K.op(K.act, lambda e: e.copy(out=eT[:, 0:8, :], in_=pT6.rearrange("p (a b) -> p a b", a=8)),
             reads=[dps[6]], writes=[d_eT])
        K.op(K.act, lambda e: e.copy(out=eT[:, 8:10, :], in_=pT7.rearrange("p (a b) -> p a b", a=2)),
             reads=[dps[7]], writes=[d_eT])
        mm_group(bank(7, 128, out_ps_off), [(eT[:, kt, :], V[:, kt, vcol0:vcol0 + 128]) for kt in range(NKT)],
                 reads=[d_eT, d_V], writes=[dps[7]])

    def emit_mixer_mla(l):
        i = l // 2
        mem.top = PH_X
        ring = Ring(K, mem, 4, 4096)
        emit_mod(l, 0, ring)
        cqnT = mem.alloc([128, 4, T], BF16); d_cqnT = Dep()
        ckvT = mem.alloc([128, 2, NKEY], BF16); d_ckvT = Dep()
        krT = mem.alloc([68, NKEY], BF16); d_krT = Dep()
        oT = mem.alloc([128, KC, T], BF16); d_oT = Dep()
        cs_tm = mem.alloc([128, NT, 2, 32], F32); d_cs = Dep()
        gq = mem.alloc([128, 512], F32); gkv = mem.alloc([128, 256], F32); d_g = Dep()
        K.dma(K.q_sync, cs_tm[:, :, 0, :], rope_cos.rearrange("(t p) r -> p t r", p=128), writes=[d_cs])
        K.dma(K.q_sync, cs_tm[:, :, 1, :], rope_sin.rearrange("(t p) r -> p t r", p=128), writes=[d_cs])
        K.dma(K.q_sync, gq, mla_q_norm_g[i].partition_broadcast(128), writes=[d_g])
        K.dma(K.q_sync, gkv, mla_kv_norm_g[i].partition_broadcast(128), writes=[d_g])
        K.dma(K.q_pool, krT[64:68, :], kmask, writes=[d_krT])
        mark1 = mem.top
        hT = mem.alloc([128, KC, T], BF16)
        d_hT = [Dep() for _ in range(NT)]
        emit_convert(0, hT, d_hT)
        wv = mla_w_in[i].rearrange("(c p) n -> p c n", p=128)
        wblk = []
        for c0, n in ((0, 256), (256, 256), (512, 256), (768, 64)):
            (w,), dw = ring.load([(wv[:, :, c0:c0 + n], KC, n)])
            wblk.append((w, dw, n))
        cch = mem.alloc([128, 2, 256], F32); d_cch = Dep()
        kch = mem.alloc([128, 2, 64], F32); d_kch = Dep()
        K.dma(K.q_sync, cch, c_ckv[i].rearrange("(t p) f -> p t f", p=128), writes=[d_cch])
        K.dma(K.q_sync, kch, c_kr[i].rearrange("(t p) f -> p t f", p=128), writes=[d_kch])
        cqn = mem.alloc([128, 512], F32); ckvn = mem.alloc([128, 256], F32); krf = mem.alloc([128, 64], F32)
        krr = mem.alloc([128, 64], F32); rt1 = mem.alloc([128, 32], F32); rt2 = mem.alloc([128, 32], F32)
        junk = mem.alloc([128, 512], BF16); c1 = mem.alloc([128, 4], F32)
        d_cqn, d_ckvn, d_krf, d_krr, d_junk, d_c1 = (Dep() for _ in range(6))
        for tt in range(2):
            def trc(e, tt=tt):
                ins = None
                for c in range(2):
                    ins = e.transpose(out=bank(0, 128, c * 128), in_=cch[:, tt, c * 128:(c + 1) * 128], identity=identF)
                ins = e.transpose(out=bank(0, 128, 256)[0:64, :], in_=kch[:, tt, :], identity=identF)
                return ins
            K.op(K.pe, trc, reads=[d_cch, d_kch, d_identF], writes=[dps[0]])
            K.op(K.act, lambda e, tt=tt: e.copy(out=ckvT[:, :, tt * 128:(tt + 1) * 128],
                                                in_=bank(0, 256).rearrange("p (c j) -> p c j", c=2)),
                 reads=[dps[0]], writes=[d_ckvT])
            K.op(K.act, lambda e, tt=tt: e.copy(out=krT[0:64, tt * 128:(tt + 1) * 128], in_=bank(0, 128, 256)[0:64, :]),
                 reads=[dps[0]], writes=[d_krT])
        ov_ckv = o_ckv[i].rearrange("(t p) f -> p t f", p=128)
        ov_kr = o_kr[i].rearrange("(t p) f -> p t f", p=128)
        for t in range(NT):
            tk = slice(t * 128, (t + 1) * 128)
            pa, pb_ = 2 + (t % 2) * 2, 3 + (t % 2) * 2
            for j in range(2):
                mm_group(bank(pa, 256, j * 256), [(hT[:, kc, tk], wblk[j][0][:, kc, :]) for kc in range(KC)],
                         reads=[d_hT[t], wblk[j][1]], writes=[dps[pa]])
            mm_group(bank(pb_, 256), [(hT[:, kc, tk], wblk[2][0][:, kc, :]) for kc in range(KC)],
                     reads=[d_hT[t], wblk[2][1]], writes=[dps[pb_]])
            mm_group(bank(pb_, 64, 256), [(hT[:, kc, tk], wblk[3][0][:, kc, :]) for kc in range(KC)],
                     reads=[d_hT[t], wblk[3][1]], writes=[dps[pb_]])
            K.op(K.act, lambda e, pa=pa: e.activation(out=junk, in_=bank(pa), func=AF.Square, accum_out=c1[:, 0:1]),
                 reads=[dps[pa]], writes=[d_junk, d_c1])
            K.op(K.act, lambda e, pb_=pb_: e.activation(out=junk[:, 0:256], in_=bank(pb_, 256), func=AF.Square,
                                                        accum_out=c1[:, 1:2]), reads=[dps[pb_]], writes=[d_junk, d_c1])
            rstd_from_ss(c1[:, 0:1], 512.0, d_c1)
            rstd_from_ss(c1[:, 1:2], 256.0, d_c1)
            K.op(K.dve, lambda e, pa=pa: e.scalar_tensor_tensor(out=cqn, in0=bank(pa), scalar=c1[:, 0:1], in1=gq,
                                                                op0=ALU.mult, op1=ALU.mult),
                 reads=[dps[pa], d_c1, d_g], writes=[d_cqn])
            K.op(K.dve, lambda e, pb_=pb_: e.scalar_tensor_tensor(out=ckvn, in0=bank(pb_, 256), scalar=c1[:, 1:2], in1=gkv,
                                                                  op0=ALU.mult, op1=ALU.mult),
                 reads=[dps[pb_], d_c1, d_g], writes=[d_ckvn])
            K.op(K.act, lambda e, pb_=pb_: e.copy(out=krf, in_=bank(pb_, 64, 256)), reads=[dps[pb_]], writes=[d_krf])
            K.dma(K.q_sync, ov_ckv[:, t, :], ckvn, reads=[d_ckvn])
            K.dma(K.q_sync, ov_kr[:, t, :], krf, reads=[d_krf])
            cos_t, sin_t = cs_tm[:, t, 0, :], cs_tm[:, t, 1, :]
            x1, x2 = krf[:, 0:32], krf[:, 32:64]
            V_ = K.dve
            K.op(V_, lambda e: e.tensor_tensor(out=rt1, in0=x2, in1=sin_t, op=ALU.mult), reads=[d_krf, d_cs], writes=[d_krr])
            K.op(V_, lambda e: e.tensor_tensor(out=krr[:, 0:32], in0=x1, in1=cos_t, op=ALU.mult), reads=[d_krf, d_cs], writes=[d_krr])
            K.op(V_, lambda e: e.tensor_tensor(out=krr[:, 0:32], in0=krr[:, 0:32], in1=rt1, op=ALU.subtract), reads=[d_krr], writes=[d_krr])
            K.op(V_, lambda e: e.tensor_tensor(out=rt2, in0=x1, in1=sin_t, op=ALU.mult), reads=[d_krf, d_cs], writes=[d_krr])
            K.op(V_, lambda e: e.tensor_tensor(out=krr[:, 32:64], in0=x2, in1=cos_t, op=ALU.mult), reads=[d_krf, d_cs], writes=[d_krr])
            K.op(V_, lambda e: e.tensor_tensor(out=krr[:, 32:64], in0=krr[:, 32:64], in1=rt2, op=ALU.add), reads=[d_krr], writes=[d_krr])
            def tr1(e):
                ins = None
                for c in range(4):
                    ins = e.transpose(out=bank(0, 128, c * 128), in_=cqn[:, c * 128:(c + 1) * 128], identity=identF)
                return ins
            K.op(K.pe, tr1, reads=[d_cqn, d_identF], writes=[dps[0]])
            K.op(K.act, lambda e, tk=tk: e.copy(out=cqnT[:, :, tk], in_=bank(0).rearrange("p (c j) -> p c j", c=4)),
                 reads=[dps[0]], writes=[d_cqnT])
            def tr2(e):
                ins = None
                for c in range(2):
                    ins = e.transpose(out=bank(1, 128, c * 128), in_=ckvn[:, c * 128:(c + 1) * 128], identity=identF)
                ins = e.transpose(out=bank(1, 128, 256)[0:64, :], in_=krr, identity=identF)
                return ins
            K.op(K.pe, tr2, reads=[d_ckvn, d_krr, d_identF], writes=[dps[1]])
            kk = slice(PAST + t * 128, PAST + (t + 1) * 128)
            K.op(K.act, lambda e, kk=kk: e.copy(out=ckvT[:, :, kk], in_=bank(1, 256).rearrange("p (c j) -> p c j", c=2)),
                 reads=[dps[1]], writes=[d_ckvT])
            K.op(K.act, lambda e, kk=kk: e.copy(out=krT[0:64, kk], in_=bank(1, 128, 256)[0:64, :]),
                 reads=[dps[1]], writes=[d_krT])
        K.barrier()
        mem.top = mark1
        rC = mem.alloc([64, T], F32); rS = mem.alloc([64, T], F32); d_rCS = Dep()
        K.dma(K.q_sync, rC, ropeT_c, writes=[d_rCS])
        K.dma(K.q_sync, rS, ropeT_s, writes=[d_rCS])
        qTn = mem.alloc([128, 2, T], BF16); d_qTn = Dep()
        qTr = mem.alloc([68, 2, T], BF16); d_qTr = Dep()
        for hh in range(2):
            K.dma(K.q_pool, qTr[64:68, hh, :], seqsel, writes=[d_qTr])
        kTn = mem.alloc([128, 2, NKEY], BF16); d_kTn = Dep()
        Vh = mem.alloc([128, NKT, 256], BF16); d_Vh = Dep()
        wsw = mem.alloc([128, 4, 2, 64], BF16); d_wsw = Dep()
        tq = mem.alloc([64, 512], F32); d_tq = Dep()
        tq2 = mem.alloc([64, 512], F32); d_tq2 = Dep()
        eb = [mem.alloc([128, NKEY], BF16) for _ in range(2)]; d_eb = [Dep(), Dep()]
        eT = mem.alloc([128, NKT, 128], BF16); d_eT = Dep()
        on = mem.alloc([128, 128], F32); d_on = Dep()
        cols = mem.alloc([128, 8], F32); d_cols = Dep()
        wq_v = mla_w_uq[i].rearrange("(c p) n -> p c n", p=128)
        wkv_v = mla_w_ukv[i].rearrange("(c p) n -> p c n", p=128)
        scale = (128 + 64) ** -0.5
        it = 0
        for hp in range(8):
            (wq,), d_wq = ring.load([(wq_v[:, :, hp * 384:(hp + 1) * 384], 4, 384)])
            (wkv,), d_wkv = ring.load([(wkv_v[:, :, hp * 512:(hp + 1) * 512], 2, 512)])
            for hh in range(2):
                b0 = hh * 192 + 128
                K.op(K.dve, lambda e, hh=hh, b0=b0: e.tensor_copy(out=wsw[:, :, hh, 0:32], in_=wq[:, :, b0 + 32:b0 + 64]),
                     reads=[d_wq], writes=[d_wsw])
                K.op(K.dve, lambda e, hh=hh, b0=b0: e.tensor_copy(out=wsw[:, :, hh, 32:64], in_=wq[:, :, b0:b0 + 32]),
                     reads=[d_wq], writes=[d_wsw])
            for hh in range(2):
                for hf in range(2):
                    tok = slice(hf * 512, (hf + 1) * 512)
                    mm_group(bank(0), [(wq[:, c, hh * 192:hh * 192 + 128], cqnT[:, c, tok]) for c in range(4)],
                             reads=[d_wq, d_cqnT], writes=[dps[0]])
                    K.op(K.act, lambda e, hh=hh, tok=tok: e.copy(out=qTn[:, hh, tok], in_=bank(0)), reads=[dps[0]], writes=[d_qTn])
                    mm_group(bank(1)[0:64, :], [(wq[:, c, hh * 192 + 128:hh * 192 + 192], cqnT[:, c, tok]) for c in range(4)],
                             reads=[d_wq, d_cqnT], writes=[dps[1]])
                    mm_group(bank(2)[0:64, :], [(wsw[:, c, hh, :], cqnT[:, c, tok]) for c in range(4)],
                             reads=[d_wsw, d_cqnT], writes=[dps[2]])
                    K.op(K.dve, lambda e, tok=tok: e.tensor_tensor(out=tq, in0=bank(2)[0:64, :], in1=rS[:, tok], op=ALU.mult),
                         reads=[dps[2], d_rCS], writes=[d_tq])
                    K.op(K.dve, lambda e, tok=tok: e.tensor_tensor(out=tq2, in0=bank(1)[0:64, :], in1=rC[:, tok], op=ALU.mult),
                         reads=[dps[1], d_rCS], writes=[d_tq2])
                    K.op(K.dve, lambda e, hh=hh, tok=tok: e.tensor_tensor(out=qTr[0:64, hh, tok], in0=tq, in1=tq2, op=ALU.add),
                         reads=[d_tq, d_tq2], writes=[d_qTr])
                for j, (c0, n) in enumerate(((0, 512), (512, 512), (1024, 256))):
                    pbk = 3 + j
                    mm_group(bank(pbk, n), [(wkv[:, c, hh * 256:hh * 256 + 128], ckvT[:, c, c0:c0 + n]) for c in range(2)],
                             reads=[d_wkv, d_ckvT], writes=[dps[pbk]])
                    K.op(K.act, lambda e, hh=hh, c0=c0, n=n, pbk=pbk: e.copy(out=kTn[:, hh, c0:c0 + n], in_=bank(pbk, n)),
                         reads=[dps[pbk]], writes=[d_kTn])
            for kt in range(NKT):
                pbv = kt % 2
                mm_group(bank(pbv, 256).rearrange("p (a b) -> p a b", a=2),
                         [(ckvT[:, c, kt * 128:(kt + 1) * 128],
                           wkv[:, c, :].rearrange("p (a b) -> p a b", a=2)[:, :, 128:256]) for c in range(2)],
                         reads=[d_wkv, d_ckvT], writes=[dps[pbv]])
                K.op(K.act, lambda e, kt=kt, pbv=pbv: e.copy(out=Vh[:, kt, :], in_=bank(pbv, 256)), reads=[dps[pbv]], writes=[d_Vh])
            for hh in range(2):
                h = hp * 2 + hh
                for qb in range(NT):
                    qs = slice(qb * 128, (qb + 1) * 128)
                    sb0 = (it % 2) * 3
                    es = it % 2
                    it += 1
                    attn_scores(sb0, [(qTn[:, hh, qs], d_qTn), (qTr[:, hh, qs], d_qTr)],
                                [(kTn[:, hh, :], d_kTn), (krT, d_krT)])
                    softmax_exp(sb0, scale, eb[es], d_eb[es], cols, d_cols, 0)
                    attn_pv(eb[es], d_eb[es], eT, d_eT, Vh, d_Vh, hh * 128, 256)
                    K.op(K.dve, lambda e: e.reciprocal(out=cols[:, 2:3], in_=cols[:, 1:2]), reads=[d_cols], writes=[d_cols])
                    K.op(K.dve, lambda e: e.tensor_scalar(out=on, in0=bank(7, 128, 256), scalar1=cols[:, 2:3], scalar2=None,
                                                          op0=ALU.mult), reads=[dps[7], d_cols], writes=[d_on])
                    K.op(K.pe, lambda e: e.transpose(out=bank(7, 128, 384), in_=on, identity=identF),
                         reads=[d_on, d_identF], writes=[dps[7]])
                    K.op(K.act, lambda e, h=h, qs=qs: e.copy(out=oT[:, h, qs], in_=bank(7, 128, 384)),
                         reads=[dps[7]], writes=[d_oT])
        K.barrier()
        mem.top = mark1
        emit_out_proj(l, mla_w_out[i], oT, d_oT, ring)
        K.barrier()
        mem.top = PH_X


    def emit_mixer_ab(l):
        i = l // 2
        lam_init = 0.8 - 0.6 * math.exp(-0.3 * l)
        mem.top = PH_X
        ring = Ring(K, mem, 3, 4096)
        emit_mod(l, 0, ring)
        oT = mem.alloc([128, KC, T], BF16); d_oT = Dep()
        hT = mem.alloc([128, KC, T], BF16)
        d_hT = [Dep() for _ in range(NT)]
        emit_convert(0, hT, d_hT)
        cs_tm = mem.alloc([128, NT, 2, 32], F32); d_cs = Dep()
        K.dma(K.q_sync, cs_tm[:, :, 0, :], rope_cos.rearrange("(t p) r -> p t r", p=128), writes=[d_cs])
        K.dma(K.q_sync, cs_tm[:, :, 1, :], rope_sin.rearrange("(t p) r -> p t r", p=128), writes=[d_cs])
        wv = ab_w_in[i].rearrange("(c p) n -> p c n", p=128)
        markA = mem.top
        xsp = x_spill.rearrange("(t p) d -> p t d", p=128)
        for t in range(NT):
            K.dma(K.q_sync, xsp[:, t, :], X[:, t, :], reads=[dX[t]])
        K.barrier()
        mem.top = PH_NOX
        if not cfg.get("gla", True):
            K.op(K.dve, lambda e: e.memset(oT[:, 0:8, :], 0.0), writes=[d_oT])
        else:
            emit_gla(l, i, hT, d_hT, oT, d_oT, wv, ring)
            K.barrier()
            mem.top = PH_NOX
        lvb = mem.alloc([128, 4, 64], F32); lcol = mem.alloc([128, 8], F32); d_l = Dep()
        gdb = mem.alloc([128, 128], F32); d_gdb = Dep()
        K.dma(K.q_sync, lvb, diff_lambda[i].partition_broadcast(128), writes=[d_l])
        K.dma(K.q_sync, gdb, diff_norm_g[i].partition_broadcast(128), writes=[d_gdb])
        K.op(K.dve, lambda e: e.tensor_scalar(out=gdb, in0=gdb, scalar1=1.0 - lam_init, scalar2=None, op0=ALU.mult),
             reads=[d_gdb], writes=[d_gdb])
        lv4 = lvb.rearrange("p (a b) d -> p a b d", a=2)
        prod = mem.alloc([128, 2, 64], F32)
        K.op(K.dve, lambda e: e.tensor_tensor(out=prod, in0=lv4[:, :, 0, :], in1=lv4[:, :, 1, :], op=ALU.mult),
             reads=[d_l], writes=[d_l])
        K.op(K.dve, lambda e: e.tensor_reduce(out=lcol[:, 0:2], in_=prod, axis=AX.X, op=ALU.add), reads=[d_l], writes=[d_l])
        K.op(K.act, lambda e: e.activation(out=lcol[:, 0:2], in_=lcol[:, 0:2], func=AF.Exp), reads=[d_l], writes=[d_l])
        K.op(K.dve, lambda e: e.tensor_tensor(out=lcol[:, 2:3], in0=lcol[:, 1:2], in1=lcol[:, 0:1], op=ALU.subtract),
             reads=[d_l], writes=[d_l])
        K.op(K.dve, lambda e: e.tensor_scalar(out=lcol[:, 2:3], in0=lcol[:, 2:3], scalar1=-lam_init, scalar2=None,
                                              op0=ALU.add), reads=[d_l], writes=[d_l])
        neg_lam = lcol[:, 2:3]
        qT = mem.alloc([68, 4, T], BF16); d_qT = Dep()
        kT = mem.alloc([68, 4, NKEY], BF16); d_kT = Dep()
        Vd = mem.alloc([128, NKT, 256], BF16); d_Vd = Dep()
        for s4 in range(4):
            K.dma(K.q_pool, qT[64:68, s4, :], seqsel, writes=[d_qT])
            K.dma(K.q_pool, kT[64:68, s4, :], kmask, writes=[d_kT])
        kc_f = mem.alloc([128, 2, 256], F32); d_kcf = Dep()
        k2f = [mem.alloc([128, 256], F32) for _ in range(2)]; d_k2f = [Dep(), Dep()]
        v2f = [mem.alloc([128, 256], F32) for _ in range(2)]; d_v2f = [Dep(), Dep()]
        qr = mem.alloc([128, 4, 64], F32); kr_ = mem.alloc([128, 4, 64], F32); d_qr = Dep(); d_kr = Dep()
        r1 = mem.alloc([128, 4, 32], F32); r2 = mem.alloc([128, 4, 32], F32); d_r = Dep()
        eb = [mem.alloc([128, NKEY], BF16) for _ in range(2)]; d_eb = [Dep(), Dep()]
        wg = mem.alloc([128, NKEY], BF16); d_wg = Dep()
        eT = mem.alloc([128, NKT, 128], BF16); d_eT = Dep()
        on = mem.alloc([128, 128], F32); d_on = Dep()
        junk = mem.alloc([128, 128], BF16); d_junk = Dep()
        cols = mem.alloc([128, 8], F32); d_cols = Dep()
        odk = o_dk[i].rearrange("(t p) f -> p t f", p=128)
        odv = o_dv[i].rearrange("(t p) f -> p t f", p=128)
        cdk = c_dk[i].rearrange("(t p) f -> p t f", p=128)
        cdv = c_dv[i].rearrange("(t p) f -> p t f", p=128)

        def rope_tm(dst, src, rd, t, d_dst):
            cos_b = cs_tm[:, t, 0, :].unsqueeze(1).to_broadcast([128, 4, 32])
            sin_b = cs_tm[:, t, 1, :].unsqueeze(1).to_broadcast([128, 4, 32])
            x1, x2 = src[:, :, 0:32], src[:, :, 32:64]
            V_ = K.dve
            K.op(V_, lambda e: e.tensor_tensor(out=r1, in0=x2, in1=sin_b, op=ALU.mult), reads=rd + [d_cs], writes=[d_r])
            K.op(V_, lambda e: e.tensor_tensor(out=dst[:, :, 0:32], in0=x1, in1=cos_b, op=ALU.mult), reads=rd + [d_cs], writes=[d_dst])
            K.op(V_, lambda e: e.tensor_tensor(out=dst[:, :, 0:32], in0=dst[:, :, 0:32], in1=r1, op=ALU.subtract), reads=[d_r], writes=[d_dst])
            K.op(V_, lambda e: e.tensor_tensor(out=r2, in0=x1, in1=sin_b, op=ALU.mult), reads=rd + [d_cs], writes=[d_r])
            K.op(V_, lambda e: e.tensor_tensor(out=dst[:, :, 32:64], in0=x2, in1=cos_b, op=ALU.mult), reads=rd + [d_cs], writes=[d_dst])
            K.op(V_, lambda e: e.tensor_tensor(out=dst[:, :, 32:64], in0=dst[:, :, 32:64], in1=r2, op=ALU.add), reads=[d_r], writes=[d_dst])

        def tr4(src, d_src, dstT, d_dstT, col0, pb):
            def f(e):
                ins = None
                for s4 in range(4):
                    ins = e.transpose(out=bank(pb, 128, s4 * 128)[0:64, :], in_=src[:, s4, :], identity=identF)
                return ins
            K.op(K.pe, f, reads=[d_src, d_identF], writes=[dps[pb]])
            K.op(K.act, lambda e: e.copy(out=dstT[0:64, :, col0:col0 + 128],
                                         in_=bank(pb)[0:64, :].rearrange("p (a b) -> p a b", a=4)),
                 reads=[dps[pb]], writes=[d_dstT])

        it = 0
        for hp in range(4):
            (wq,), d_wq = ring.load([(wv[:, :, 3104 + hp * 256:3104 + (hp + 1) * 256], KC, 256)])
            (wk,), d_wk = ring.load([(wv[:, :, 4128 + hp * 256:4128 + (hp + 1) * 256], KC, 256)])
            (wvv,), d_wv = ring.load([(wv[:, :, 5152 + hp * 256:5152 + (hp + 1) * 256], KC, 256)])
            K.dma(K.q_sync, kc_f, cdk[:, :, hp * 256:(hp + 1) * 256], writes=[d_kcf])
            K.dma(K.q_pool, Vd[:, 0:2, :], cdv[:, :, hp * 256:(hp + 1) * 256], writes=[d_Vd])
            for tt in range(2):
                tr4(kc_f[:, tt, :].rearrange("p (a b) -> p a b", a=4), d_kcf, kT, d_kT, tt * 128, 6)
            for t in range(NT):
                tk = slice(t * 128, (t + 1) * 128)
                a = t % 2
                pq = 0 + a * 3
                mm_group(bank(pq, 256), [(hT[:, kc, tk], wq[:, kc, :]) for kc in range(KC)], reads=[d_hT[t], d_wq], writes=[dps[pq]])
                mm_group(bank(pq + 1, 256), [(hT[:, kc, tk], wk[:, kc, :]) for kc in range(KC)], reads=[d_hT[t], d_wk], writes=[dps[pq + 1]])
                mm_group(bank(pq + 2, 256), [(hT[:, kc, tk], wvv[:, kc, :]) for kc in range(KC)], reads=[d_hT[t], d_wv], writes=[dps[pq + 2]])
                K.op(K.act, lambda e, a=a, pq=pq: e.copy(out=k2f[a], in_=bank(pq + 1, 256)), reads=[dps[pq + 1]], writes=[d_k2f[a]])
                K.op(K.act, lambda e, a=a, pq=pq: e.copy(out=v2f[a], in_=bank(pq + 2, 256)), reads=[dps[pq + 2]], writes=[d_v2f[a]])
                K.dma(K.q_sync, odk[:, t, hp * 256:(hp + 1) * 256], k2f[a], reads=[d_k2f[a]])
                K.dma(K.q_sync, odv[:, t, hp * 256:(hp + 1) * 256], v2f[a], reads=[d_v2f[a]])
                K.op(K.dve, lambda e, a=a, t=t: e.tensor_copy(out=Vd[:, 2 + t, :], in_=v2f[a]), reads=[d_v2f[a]], writes=[d_Vd])
                rope_tm(qr, bank(pq, 256).rearrange("p (a b) -> p a b", a=4), [dps[pq]], t, d_qr)
                rope_tm(kr_, k2f[a].rearrange("p (a b) -> p a b", a=4), [d_k2f[a]], t, d_kr)
                tr4(qr, d_qr, qT, d_qT, t * 128, 6)
                tr4(kr_, d_kr, kT, d_kT, PAST + t * 128, 6)
            for hh in range(2):
                h = hp * 2 + hh
                for qb in range(NT):
                    qs = slice(qb * 128, (qb + 1) * 128)
                    for j in range(2):
                        s4 = hh * 2 + j
                        attn_scores(j * 3, [(qT[:, s4, qs], d_qT)], [(kT[:, s4, :], d_kT)])
                        softmax_exp(j * 3, 0.125, eb[j], d_eb[j], cols, d_cols, j * 2)
                    K.op(K.dve, lambda e: e.reciprocal(out=cols[:, 4:5], in_=cols[:, 1:2]), reads=[d_cols], writes=[d_cols])
                    K.op(K.dve, lambda e: e.reciprocal(out=cols[:, 5:6], in_=cols[:, 3:4]), reads=[d_cols], writes=[d_cols])
                    K.op(K.dve, lambda e: e.tensor_tensor(out=cols[:, 5:6], in0=cols[:, 5:6], in1=neg_lam, op=ALU.mult),
                         reads=[d_cols, d_l], writes=[d_cols])
                    K.op(K.dve, lambda e: e.tensor_scalar(out=wg, in0=eb[0], scalar1=cols[:, 4:5], scalar2=None, op0=ALU.mult),
                         reads=[d_eb[0], d_cols], writes=[d_wg])
                    K.op(K.dve, lambda e: e.scalar_tensor_tensor(out=wg, in0=eb[1], scalar=cols[:, 5:6], in1=wg,
                                                                 op0=ALU.mult, op1=ALU.add),
                         reads=[d_eb[1], d_cols], writes=[d_wg])
                    attn_pv(wg, d_wg, eT, d_eT, Vd, d_Vd, hh * 128, 256)
                    K.op(K.act, lambda e: e.activation(out=junk, in_=bank(7, 128, 256), func=AF.Square, accum_out=cols[:, 6:7]),
                         reads=[dps[7]], writes=[d_junk, d_cols])
                    rstd_from_ss(cols[:, 6:7], 128.0, d_cols)
                    K.op(K.dve, lambda e: e.scalar_tensor_tensor(out=on, in0=bank(7, 128, 256), scalar=cols[:, 6:7], in1=gdb,
                                                                 op0=ALU.mult, op1=ALU.mult),
                         reads=[dps[7], d_cols, d_gdb], writes=[d_on])
                    K.op(K.pe, lambda e: e.transpose(out=bank(7, 128, 384), in_=on, identity=identF),
                         reads=[d_on, d_identF], writes=[dps[7]])
                    K.op(K.act, lambda e, h=h, qs=qs: e.copy(out=oT[:, 8 + h, qs], in_=bank(7, 128, 384)),
                         reads=[dps[7]], writes=[d_oT])
        assert mem.top <= PH_X, f"low region overflow {mem.top} > {PH_X}"
        K.barrier()
        mem.top = markA
        for t in range(NT):
            K.dma(K.q_sync, X[:, t, :], xsp[:, t, :], writes=[dX[t]])
        emit_out_proj(l, ab_w_out[i], oT, d_oT, ring)
        K.barrier()
        mem.top = PH_X

    def emit_gla(l, i, hT, d_hT, oT, d_oT, wv, ring):
        glam_sb = mem.alloc([128, 6, 128], F32); d_glam = Dep()
        for m in range(6):
            K.dma(K.q_sync, glam_sb[:, m, :], glam_d[m], writes=[d_glam])
        csel_sb = mem.alloc([128, 4], F32)
        K.dma(K.q_sync, csel_sb, csel_d, writes=[d_glam])
        keepc = mem.alloc([128, 1], F32)
        K.dma(K.q_sync, keepc, keep, writes=[d_glam])
        ggb = mem.alloc([128, 256], F32)
        K.dma(K.q_sync, ggb, gla_norm_g[i].partition_broadcast(128), writes=[d_glam])
        ggT = mem.alloc([17, 2, T], F32); d_ggT = Dep()
        K.op(K.dve, lambda e: e.memset(ggT, 1.0), writes=[d_ggT])
        wg2 = mem.alloc([17, 2, 512], F32); d_wg2 = Dep()
        for dr in range(2):
            K.dma(K.q_sync, wg2[0:16, dr, :], gla_w_gate2[i, dr], writes=[d_wg2])
            K.dma(K.q_sync, wg2[16:17, dr, :], gla_b_gate2[i, dr].rearrange("(o f) -> o f", o=1), writes=[d_wg2])
        (wgg,), d_wgg = ring.load([(wv[:, :, 3072:3104], KC, 32)])
        for dr in range(2):
            for hf in range(2):
                tok = slice(hf * 512, (hf + 1) * 512)
                mm_group(bank(0)[0:16, :], [(wgg[:, kc, dr * 16:(dr + 1) * 16], hT[:, kc, tok]) for kc in range(KC)],
                         reads=d_hT + [d_wgg], writes=[dps[0]])
                K.op(K.act, lambda e, dr=dr, tok=tok: e.copy(out=ggT[0:16, dr, tok], in_=bank(0)[0:16, :]),
                     reads=[dps[0]], writes=[d_ggT])
        q_f = mem.alloc([128, NT, 128], F32); k_f = mem.alloc([128, NT, 128], F32); v_b = mem.alloc([128, NT, 256], BF16)
        d_qkv = Dep()
        khat = mem.alloc([128, NT, 128], BF16); qtT = mem.alloc([128, T], BF16); ktT = mem.alloc([128, T], BF16)
        AT = mem.alloc([128, NT, 128], BF16); dcol = mem.alloc([128, 32], F32)
        d_khat, d_qtT, d_ktT, d_AT, d_dcol = (Dep() for _ in range(5))
        o_f = mem.alloc([128, NT, 256], BF16); d_of = Dep()
        S = mem.alloc([128, 256], F32); S_bf = mem.alloc([128, 256], BF16); d_S = Dep(); d_Sbf = Dep()
        qblk = mem.alloc([128, 4, 128], BF16); khm = mem.alloc([128, 4, 128], BF16); d_qblk = Dep(); d_khm = Dep()
        K.op(K.dve, lambda e: e.memset(qblk, 0.0), writes=[d_qblk])
        e1 = mem.alloc([128, 128], F32); sp = mem.alloc([128, 128], F32); ebt = mem.alloc([128, 3, 128], F32)
        qt = mem.alloc([128, 128], F32); kt = mem.alloc([128, 128], F32)
        osum = mem.alloc([128, 256], F32); sgr = mem.alloc([128, 256], F32); on = mem.alloc([128, 256], F32)
        junk = mem.alloc([128, 256], BF16); cg = mem.alloc([128, 4], F32)
        d_e1, d_sp, d_ebt, d_qt, d_kt, d_osum, d_sgr, d_on, d_junk, d_cg = (Dep() for _ in range(10))
        og = o_gla[i]
        for h in range(4):
            (wq_,), d_wq = ring.load([(wv[:, :, h * 128:(h + 1) * 128], KC, 128)])
            (wk_,), d_wk = ring.load([(wv[:, :, 512 + h * 128:512 + (h + 1) * 128], KC, 128)])
            (wv_,), d_wv_ = ring.load([(wv[:, :, 1024 + h * 256:1024 + (h + 1) * 256], KC, 256)])
            for t in range(NT):
                tk = slice(t * 128, (t + 1) * 128)
                mm_group(bank(0, 128, 0), [(hT[:, kc, tk], wq_[:, kc, :]) for kc in range(KC)], reads=[d_hT[t], d_wq], writes=[dps[0]])
                mm_group(bank(0, 128, 128), [(hT[:, kc, tk], wk_[:, kc, :]) for kc in range(KC)], reads=[d_hT[t], d_wk], writes=[dps[0]])
                mm_group(bank(0, 256, 256), [(hT[:, kc, tk], wv_[:, kc, :]) for kc in range(KC)], reads=[d_hT[t], d_wv_], writes=[dps[0]])
                K.op(K.act, lambda e, t=t: e.mul(out=q_f[:, t, :], in_=bank(0, 128, 0), mul=128.0 ** -0.5), reads=[dps[0]], writes=[d_qkv])
                K.op(K.act, lambda e, t=t: e.copy(out=k_f[:, t, :], in_=bank(0, 128, 128)), reads=[dps[0]], writes=[d_qkv])
                K.op(K.act, lambda e, t=t: e.copy(out=v_b[:, t, :], in_=bank(0, 256, 256)), reads=[dps[0]], writes=[d_qkv])
            (wgr,), d_wgr = ring.load([(wv[:, :, 2048 + h * 256:2048 + (h + 1) * 256], KC, 256)])
            for dr in range(2):
                mi, ms = (0, 1) if dr == 0 else (2, 3)
                K.dma(K.q_sync, S, st_gla[i, dr, h], writes=[d_S])
                K.op(K.act, lambda e: e.copy(out=S_bf, in_=S), reads=[d_S], writes=[d_Sbf])
                for t in range(NT):
                    tk = slice(t * 128, (t + 1) * 128)
                    mm_group(bank(1, 128), [(ggT[0:17, dr, tk], wg2[0:17, dr, h * 128:(h + 1) * 128])],
                             reads=[d_ggT, d_wg2], writes=[dps[1]])
                    K.op(K.act, lambda e: e.activation(out=e1, in_=bank(1, 128), func=AF.Exp, scale=-1.0), reads=[dps[1]], writes=[d_e1])
                    K.op(K.act, lambda e: e.activation(out=sp, in_=e1, func=AF.Ln, bias=1.0), reads=[d_e1], writes=[d_sp])
                    mm_group(bank(2, 128, 0), [(glam_sb[:, mi, :], sp)], reads=[d_glam, d_sp], writes=[dps[2]])
                    mm_group(bank(2, 128, 128), [(glam_sb[:, ms, :], sp)], reads=[d_glam, d_sp], writes=[dps[2]])
                    mm_group(bank(2, 4, 256), [(sp, csel_sb)], reads=[d_glam, d_sp], writes=[dps[2]])
                    K.op(K.act, lambda e: e.activation(out=ebt[:, 0, :], in_=bank(2, 128, 0), func=AF.Exp, scale=-1.0 / 16), reads=[dps[2]], writes=[d_ebt])
                    K.op(K.act, lambda e: e.activation(out=ebt[:, 1, :], in_=bank(2, 128, 0), func=AF.Exp, scale=1.0 / 16), reads=[dps[2]], writes=[d_ebt])
                    K.op(K.act, lambda e: e.activation(out=ebt[:, 2, :], in_=bank(2, 128, 128), func=AF.Exp, scale=-1.0 / 16), reads=[dps[2]], writes=[d_ebt])
                    K.op(K.act, lambda e, t=t: e.activation(out=dcol[:, t * 4:(t + 1) * 4], in_=bank(2, 4, 256), func=AF.Exp, scale=-1.0 / 16),
                         reads=[dps[2]], writes=[d_dcol])
                    K.op(K.dve, lambda e, t=t: e.tensor_tensor(out=qt, in0=q_f[:, t, :], in1=ebt[:, 0, :], op=ALU.mult), reads=[d_qkv, d_ebt], writes=[d_qt])
                    K.op(K.dve, lambda e, t=t: e.tensor_tensor(out=kt, in0=k_f[:, t, :], in1=ebt[:, 1, :], op=ALU.mult), reads=[d_qkv, d_ebt], writes=[d_kt])
                    K.op(K.dve, lambda e, t=t: e.tensor_tensor(out=khat[:, t, :], in0=k_f[:, t, :], in1=ebt[:, 2, :], op=ALU.mult), reads=[d_qkv, d_ebt], writes=[d_khat])
                    def trqk(e):
                        e.transpose(out=bank(3, 128, 0), in_=qt, identity=identF)
                        return e.transpose(out=bank(3, 128, 128), in_=kt, identity=identF)
                    K.op(K.pe, trqk, reads=[d_qt, d_kt, d_identF], writes=[dps[3]])
                    K.op(K.act, lambda e, tk=tk: e.copy(out=qtT[:, tk], in_=bank(3, 128, 0)), reads=[dps[3]], writes=[d_qtT])
                    K.op(K.act, lambda e, tk=tk: e.copy(out=ktT[:, tk], in_=bank(3, 128, 128)), reads=[dps[3]], writes=[d_ktT])
                    mm_group(bank(4, 128), [(ktT[:, tk], qtT[:, tk])], reads=[d_qtT, d_ktT], writes=[dps[4]])
                    K.op(K.dve, lambda e, t=t, dr=dr: e.tensor_tensor(out=AT[:, t, :], in0=bank(4, 128), in1=glam_sb[:, 4 + dr, :], op=ALU.mult),
                         reads=[dps[4], d_glam], writes=[d_AT])
                torder = list(range(NT)) if dr == 0 else list(range(NT - 1, -1, -1))
                corder = [0, 1, 2, 3] if dr == 0 else [3, 2, 1, 0]
                kv_i = 0
                for n_t, t in enumerate(torder):
                    tk = slice(t * 128, (t + 1) * 128)
                    if n_t > 0 and n_t % 2 == 0:
                        K.op(K.dve, lambda e: e.tensor_scalar(out=S, in0=S, scalar1=keepc[:, 0:1], scalar2=None, op0=ALU.mult),
                             reads=[d_glam], writes=[d_S])
                        K.op(K.act, lambda e: e.copy(out=S_bf, in_=S), reads=[d_S], writes=[d_Sbf])
                    for c in range(4):
                        K.op(K.act, lambda e, c=c, t=t: e.copy(out=qblk[:, c, c * 32:(c + 1) * 32],
                                                               in_=qtT[:, t * 128 + c * 32:t * 128 + (c + 1) * 32]),
                             reads=[d_qtT], writes=[d_qblk])
                        K.op(K.dve, lambda e, c=c, t=t: e.tensor_scalar(out=khm[:, c, :], in0=khat[:, t, :], scalar1=csel_sb[:, c:c + 1],
                                                                        scalar2=None, op0=ALU.mult),
                             reads=[d_khat, d_glam], writes=[d_khm])
                    K.op(K.pe, lambda e, t=t: e.matmul(bank(5, 256), lhsT=AT[:, t, :], rhs=v_b[:, t, :], start=True, stop=False),
                         reads=[d_AT, d_qkv], writes=[dps[5]])
                    for ci, c in enumerate(corder):
                        K.op(K.pe, lambda e, c=c, ci=ci: e.matmul(bank(5, 256), lhsT=qblk[:, c, :], rhs=S_bf, start=False, stop=(ci == 3)),
                             reads=[d_qblk, d_Sbf], writes=[dps[5]])
                        kvo = (kv_i % 2) * 256
                        kv_i += 1
                        mm_group(bank(6, 256, kvo), [(khm[:, c, :], v_b[:, t, :])], reads=[d_khm, d_qkv], writes=[dps[6]])
                        K.op(K.dve, lambda e, c=c, t=t, kvo=kvo: e.scalar_tensor_tensor(
                            out=S, in0=S, scalar=dcol[:, t * 4 + c:t * 4 + c + 1], in1=bank(6, 256, kvo), op0=ALU.mult, op1=ALU.add),
                            reads=[dps[6], d_dcol], writes=[d_S])
                        K.op(K.act, lambda e: e.copy(out=S_bf, in_=S), reads=[d_S], writes=[d_Sbf])
                    if n_t % 2 == 1:
                        K.dma(K.q_sync, og[t // 2, dr, h], S, reads=[d_S])
                    if dr == 0:
                        K.op(K.act, lambda e, t=t: e.copy(out=o_f[:, t, :], in_=bank(5, 256)), reads=[dps[5]], writes=[d_of])
                    else:
                        K.op(K.dve, lambda e, t=t: e.tensor_tensor(out=osum, in0=bank(5, 256), in1=o_f[:, t, :], op=ALU.add),
                             reads=[dps[5], d_of], writes=[d_osum])
                        K.op(K.act, lambda e: e.activation(out=junk, in_=osum, func=AF.Square, accum_out=cg[:, 0:1]),
                             reads=[d_osum], writes=[d_junk, d_cg])
                        rstd_from_ss(cg[:, 0:1], 256.0, d_cg)
                        mm_group(bank(1, 256, 128), [(hT[:, kc, tk], wgr[:, kc, :]) for kc in range(KC)], reads=[d_hT[t], d_wgr], writes=[dps[1]])
                        K.op(K.act, lambda e: e.activation(out=sgr, in_=bank(1, 256, 128), func=AF.Silu), reads=[dps[1]], writes=[d_sgr])
                        K.op(K.dve, lambda e: e.scalar_tensor_tensor(out=on, in0=osum, scalar=cg[:, 0:1], in1=ggb, op0=ALU.mult, op1=ALU.mult),
                             reads=[d_osum, d_cg, d_glam], writes=[d_on])
                        K.op(K.dve, lambda e: e.tensor_tensor(out=on, in0=on, in1=sgr, op=ALU.mult), reads=[d_sgr], writes=[d_on])
                        def tro(e):
                            e.transpose(out=bank(7, 128, 0), in_=on[:, 0:128], identity=identF)
                            return e.transpose(out=bank(7, 128, 128), in_=on[:, 128:256], identity=identF)
                        K.op(K.pe, tro, reads=[d_on, d_identF], writes=[dps[7]])
                        K.op(K.act, lambda e, h=h, tk=tk: e.copy(out=oT[:, h * 2:h * 2 + 2, tk],
                                                                 in_=bank(7, 256).rearrange("p (a b) -> p a b", a=2)),
                             reads=[dps[7]], writes=[d_oT])

    def emit_mixer_stub(l):
        mem.top = PH_X
        ring = Ring(K, mem, 4, 4096)
        emit_mod(l, 0, ring)
        chk("mod0")
        if cfg.get("dbg_noln"):
            gb, d_gb, scr, d_scr = None, Dep(), (None, None, None), Dep()
        else:
            gb, d_gb, scr, d_scr = load_ln(l, 0)
        for t in range(NT):
            if not cfg.get("dbg_nomul"):
                K.op(K.act, lambda e, t=t: e.mul(out=X[:, t, :], in_=X[:, t, :], mul=ALPHA), writes=[dX[t]])
            emit_ln_tile(t, gb, d_gb, scr, d_scr)
        K.barrier()
        mem.top = PH_X
        chk("ln0")

    class _Stop(Exception):
        pass
    stop = cfg.get("stop", "")
    def chk(tag):
        if stop == tag:
            K.barrier()
            raise _Stop()
    try:
        for l in range(depth):
            if do_mixer and l % 2 == 1:
                emit_mixer_mla(l)
            elif do_mixer:
                emit_mixer_ab(l)
            else:
                emit_mixer_stub(l)
            emit_moe(l)
    except _Stop:
        pass

    yv = y_out.rearrange("(t p) d -> p t d", p=128)
    for t in range(NT):
        K.dma(K.q_sync, yv[:, t, :], X[:, t, :], reads=[dX[t]])
    K.finish()
    print("SBUF peak bytes", mem.peak, "sem counts", [(e.name, e.cnt) for e in K.engs])
    return nc


def _gla_consts():
    idx = np.arange(128)
    same = (idx[:, None] // 32) == (idx[None, :] // 32)
    s, t = idx[:, None], idx[None, :]
    m = np.zeros((6, 128, 128), np.float32)
    m[0] = same & (s <= t)
    m[1] = same & (s > t)
    m[2] = same & (s >= t)
    m[3] = same & (s < t)
    m[4] = same & (s <= t)
    m[5] = same & (s >= t)
    csel = np.zeros((128, 4), np.float32)
    csel[idx, idx // 32] = 1.0
    return m, csel


def _rope_tables():
    n_rows = T // 64
    rows = np.repeat(np.arange(n_rows, dtype=np.float32), 64)
    cols = np.tile(np.arange(64, dtype=np.float32), n_rows)
    quarter = 16
    freqs = (10000.0 ** (-np.arange(quarter, dtype=np.float32) / quarter)).astype(np.float32)
    ang = np.concatenate([rows[:, None] * freqs, cols[:, None] * freqs], axis=-1).astype(np.float32)
    return np.cos(ang).astype(np.float32), np.sin(ang).astype(np.float32)


_CACHE = {}


def kernel(**inputs):
    cfg = inputs.pop("_cfg", {})
    inp = {k: np.ascontiguousarray(np.asarray(v)) for k, v in inputs.items()}
    key = tuple(sorted((k, str(v)) for k, v in cfg.items()))
    if key not in _CACHE:
        _CACHE[key] = build(cfg)
    nc = _CACHE[key]
    glam, csel = _gla_consts()
    cos, sin = _rope_tables()
    wnames = ["ada_w", "ada_b", "ln_g", "ln_b", "ab_w_in", "gla_w_gate2", "gla_b_gate2", "gla_norm_g",
              "diff_lambda", "diff_norm_g", "ab_w_out", "mla_w_in", "mla_q_norm_g", "mla_w_uq",
              "mla_kv_norm_g", "mla_w_ukv", "mla_w_out", "moe_w_rg", "moe_b_rg", "moe_w_re", "moe_b_re",
              "moe_w_gate", "moe_w_up", "moe_w_down"]
    in_maps = []
    depth = cfg.get("depth", DEPTH)
    n_exp = cfg.get("n_exp", N_EXP)
    cores = cfg.get("cores", list(range(8)))
    dl, nab, ncl, ne = max(depth, 1), max((depth + 1) // 2, 1), max(depth // 2, 1), max(n_exp, 1)
    wsl = {}
    for n in wnames:
        a = inp[n]
        if n in ("ada_w", "ada_b", "ln_g", "ln_b", "moe_w_rg", "moe_b_rg", "moe_w_re", "moe_b_re"):
            a = a[:dl]
        elif n in ("moe_w_gate", "moe_w_up", "moe_w_down"):
            a = a[:dl, :ne]
        elif n.startswith("mla_"):
            a = a[:ncl]
        else:
            a = a[:nab]
        wsl[n] = np.ascontiguousarray(a)
    for c in cores:
        m = dict(wsl)
        m["identF"] = np.eye(128, dtype=np.float32)
        m["glam"] = glam
        m["csel"] = csel
        if c < 4:
            m["x_in"] = inp["x_prompt"][4 * c:4 * c + 4].reshape(T, D)
            m["cond"] = inp["c_ctx"]
            m["st_gla"] = np.zeros((2, 2, 4, 128, 256), np.float32)
            m["c_dk"] = np.zeros((2, PAST, 1024), np.float32)
            m["c_dv"] = np.zeros((2, PAST, 1024), np.float32)
            m["c_ckv"] = np.zeros((2, PAST, 256), np.float32)
            m["c_kr"] = np.zeros((2, PAST, 64), np.float32)
            m["rope_cos"] = np.ones((T, 32), np.float32)
            m["rope_sin"] = np.zeros((T, 32), np.float32)
            m["ropeT_c"] = np.ones((64, T), np.float32)
            m["ropeT_s"] = np.zeros((64, T), np.float32)
            ss = np.zeros((4, T), np.float32)
            km = np.full((4, NKEY), NEG, np.float32)
            for s in range(4):
                ss[s, 256 * s:256 * (s + 1)] = 1.0
                km[s, PAST + 256 * s:PAST + 256 * (s + 1)] = 0.0
            m["seqsel"] = ss
            m["kmask"] = km
            m["keep"] = np.zeros((128, 1), np.float32)
        else:
            b = c - 4
            m["x_in"] = inp["x_sample"][b]
            m["cond"] = inp["c"][b]
            m["st_gla"] = inp["state_gla"][b]
            m["c_dk"] = inp["cache_diff_k"][b].reshape(2, PAST, 1024)
            m["c_dv"] = inp["cache_diff_v"][b].reshape(2, PAST, 1024)
            m["c_ckv"] = inp["cache_mla_ckv"][b]
            m["c_kr"] = inp["cache_mla_krope"][b]
            m["rope_cos"] = cos
            m["rope_sin"] = sin
            m["ropeT_c"] = np.concatenate([cos.T, cos.T], axis=0)
            m["ropeT_s"] = np.concatenate([-sin.T, sin.T], axis=0)
            ss = np.zeros((4, T), np.float32)
            ss[0] = 1.0
            m["seqsel"] = ss
            m["kmask"] = np.zeros((4, NKEY), np.float32)
            m["keep"] = np.ones((128, 1), np.float32)
        in_maps.append({k: np.ascontiguousarray(v, dtype=np.float32) for k, v in m.items()})
    res = run_bass_kernel_spmd(nc, in_maps, core_ids=list(range(len(cores))))
    if len(cores) != 8:
        return {c: res.results[i] for i, c in enumerate(cores)}
    r = res.results
    y_prompt = np.stack([r[c]["y"].reshape(4, 256, D) for c in range(4)]).reshape(16, 256, D)
    y_sample = np.stack([r[c]["y"] for c in range(4, 8)])
    new_gla = np.concatenate([np.transpose(r[c]["o_gla"], (1, 0, 2, 3, 4, 5)) for c in range(4)], axis=0)
    def tok(name, shp):
        a = np.stack([r[c][name] for c in range(4)])
        a = a.reshape(4, 2, 4, 256, -1).transpose(0, 2, 1, 3, 4).reshape(16, 2, 256, -1)
        return a.reshape((16, 2, 256) + shp)
    new_dk = tok("o_dk", (8, 2, 64))
    new_dv = tok("o_dv", (8, 128))
    new_ckv = tok("o_ckv", (256,))
    new_kr = tok("o_kr", (64,))
    return (y_prompt.astype(np.float32), y_sample.astype(np.float32), new_gla.astype(np.float32),
            new_dk.astype(np.float32), new_dv.astype(np.float32), new_ckv.astype(np.float32),
            new_kr.astype(np.float32))
```

```python
import contextlib
import math
import numpy as np
import concourse.bass as bass
import concourse.mybir as mybir
from concourse.bass_utils import run_bass_kernel_spmd

F32 = mybir.dt.float32
BF16 = mybir.dt.bfloat16
AF = mybir.ActivationFunctionType
ALU = mybir.AluOpType
AX = mybir.AxisListType

D = 2048
KC = 16
T = 1024
NT = 8
DEPTH = 4
PAST = 256
NKEY = PAST + T
NKT = NKEY // 128
N_EXP = 16
D_EXP = 512
AB_IN = 6176
C_IN = 832
ALPHA = (2.0 * DEPTH) ** 0.25
LN_EPS = 1e-5
RMS_EPS = 1e-6
NEG = -30000.0


class Dep:
    __slots__ = ("w", "r", "excl")

    def __init__(self, excl=False):
        self.w = None
        self.r = {}
        self.excl = excl


class Eng:
    def __init__(self, K, e, name, own_wait=True):
        self.e = e
        self.name = name
        self.sem = K.es.enter_context(K.nc.semaphore("s_" + name))
        self.cnt = 0
        self.waited = {}
        self.own_wait = own_wait

    def wait(self, ev):
        if ev is None:
            return
        sem, val = ev
        if sem is self.sem and not self.own_wait:
            return
        key = id(sem)
        if self.waited.get(key, 0) >= val:
            return
        self.e.wait_ge(sem, val)
        self.waited[key] = val


class DmaQ:
    def __init__(self, K, eng, nsem, name):
        self.eng = eng
        self.sems = [K.es.enter_context(K.nc.semaphore(f"d_{name}{i}")) for i in range(nsem)]
        self.vals = [0] * nsem
        self.i = 0

    def next_sem(self):
        i = self.i
        self.i = (self.i + 1) % len(self.sems)
        if self.vals[i] > 0:
            self.eng.wait((self.sems[i], self.vals[i]))
        return i


class Kern:
    def __init__(self, nc):
        self.nc = nc
        self.es = contextlib.ExitStack()
        self.pe = Eng(self, nc.tensor, "pe", own_wait=False)
        self.act = Eng(self, nc.scalar, "act")
        self.dve = Eng(self, nc.vector, "dve")
        self.pool = Eng(self, nc.gpsimd, "pool")
        self.sp = Eng(self, nc.sync, "sp")
        self.engs = [self.pe, self.act, self.dve, self.pool, self.sp]
        self.q_sync = DmaQ(self, self.sp, 28, "sy")
        self.q_pool = DmaQ(self, self.pool, 28, "po")

    def _pre(self, eng, reads, writes):
        for d in reads:
            eng.wait(d.w)
            if d.excl:
                for ev in list(d.r.values()):
                    if ev[0] is not eng.sem:
                        eng.wait(ev)
        for d in writes:
            eng.wait(d.w)
            for ev in list(d.r.values()):
                eng.wait(ev)

    def _post(self, ev, reads, writes):
        sem, v = ev
        for d in reads:
            d.r[id(sem)] = ev
        for d in writes:
            d.w = ev
            d.r = {}

    def op(self, eng, fn, reads=(), writes=()):
        self._pre(eng, reads, writes)
        ins = fn(eng.e)
        eng.cnt += 1
        ins.then_inc(eng.sem, 1)
        ev = (eng.sem, eng.cnt)
        self._post(ev, reads, writes)
        return ev

    def dma(self, q, out, in_, reads=(), writes=()):
        eng = q.eng
        self._pre(eng, reads, writes)
        i = q.next_sem()
        ins = eng.e.dma_start(out=out, in_=in_)
        ins.then_inc(q.sems[i], 16)
        q.vals[i] += 16
        ev = (q.sems[i], q.vals[i])
        self._post(ev, reads, writes)
        return ev

    def all_events(self):
        evs = [(e.sem, e.cnt) for e in self.engs if e.cnt > 0]
        for q in (self.q_sync, self.q_pool):
            for s, v in zip(q.sems, q.vals):
                if v > 0:
                    evs.append((s, v))
        return evs

    def barrier(self):
        evs = self.all_events()
        for e in self.engs:
            for ev in evs:
                e.wait(ev)

    def finish(self):
        for ev in self.all_events():
            self.sp.wait(ev)


class Mem:
    def __init__(self, big, nbytes):
        self.big = big
        self.n = nbytes
        self.top = 0
        self.peak = 0

    def alloc(self, shape, dtype=F32):
        esz = 4 if dtype == F32 else 2
        nfree = 1
        for s in shape[1:]:
            nfree *= s
        nb = nfree * esz
        off = (self.top + 63) // 64 * 64
        self.top = off + nb
        self.peak = max(self.peak, self.top)
        assert self.top <= self.n, f"SBUF overflow {self.top} > {self.n}"
        ap = self.big[:, off // 2:(off + nb) // 2]
        if dtype == F32:
            ap = ap.bitcast(F32)
        if len(shape) > 2:
            names = [f"d{i}" for i in range(len(shape) - 1)]
            kw = {n: s for n, s in zip(names[:-1], shape[1:-1])}
            ap = ap.rearrange(f"p ({' '.join(names)}) -> p {' '.join(names)}", **kw)
        if shape[0] < 128:
            ap = ap[0:shape[0]]
        return ap


class Ring:
    def __init__(self, K, mem, nslot, slot_elems):
        self.K = K
        self.slots = [mem.alloc([128, slot_elems], BF16) for _ in range(nslot)]
        self.deps = [Dep() for _ in range(nslot)]
        self.i = 0

    def load(self, srcs):
        i = self.i
        self.i = (self.i + 1) % len(self.slots)
        views = []
        off = 0
        for src, a, b in srcs:
            v = self.slots[i][:, off:off + a * b].rearrange("p (a b) -> p a b", a=a)
            self.K.dma(self.K.q_pool, v, src, writes=[self.deps[i]])
            views.append(v)
            off += a * b
        return views, self.deps[i]


def build(cfg):
    depth = cfg.get("depth", DEPTH)
    n_exp = cfg.get("n_exp", N_EXP)
    do_mixer = cfg.get("mixer", True)
    nc = bass.Bass("TRN2", target_bir_lowering=False)
    K = Kern(nc)
    es = K.es

    def din(name, shape):
        return nc.dram_tensor(name, list(shape), F32, kind="ExternalInput").ap()

    def dout(name, shape):
        return nc.dram_tensor(name, list(shape), F32, kind="ExternalOutput").ap()

    x_in = din("x_in", [T, D])
    cond = din("cond", [D])
    st_gla = din("st_gla", [2, 2, 4, 128, 256])
    c_dk = din("c_dk", [2, PAST, 1024])
    c_dv = din("c_dv", [2, PAST, 1024])
    c_ckv = din("c_ckv", [2, PAST, 256])
    c_kr = din("c_kr", [2, PAST, 64])
    rope_cos = din("rope_cos", [T, 32])
    rope_sin = din("rope_sin", [T, 32])
    ropeT_c = din("ropeT_c", [64, T])
    ropeT_s = din("ropeT_s", [64, T])
    seqsel = din("seqsel", [4, T])
    kmask = din("kmask", [4, NKEY])
    keep = din("keep", [128, 1])
    identF_d = din("identF", [128, 128])
    glam_d = din("glam", [6, 128, 128])
    csel_d = din("csel", [128, 4])
    dl = max(depth, 1)
    nab = max((depth + 1) // 2, 1)
    ncl = max(depth // 2, 1)
    ada_w = din("ada_w", [dl, D, 6 * D])
    ada_b = din("ada_b", [dl, 6 * D])
    ln_g = din("ln_g", [dl, 2, D])
    ln_b = din("ln_b", [dl, 2, D])
    ab_w_in = din("ab_w_in", [nab, D, AB_IN])
    gla_w_gate2 = din("gla_w_gate2", [nab, 2, 16, 512])
    gla_b_gate2 = din("gla_b_gate2", [nab, 2, 512])
    gla_norm_g = din("gla_norm_g", [nab, 256])
    diff_lambda = din("diff_lambda", [nab, 4, 64])
    diff_norm_g = din("diff_norm_g", [nab, 128])
    ab_w_out = din("ab_w_out", [nab, D, D])
    mla_w_in = din("mla_w_in", [ncl, D, C_IN])
    mla_q_norm_g = din("mla_q_norm_g", [ncl, 512])
    mla_w_uq = din("mla_w_uq", [ncl, 512, 3072])
    mla_kv_norm_g = din("mla_kv_norm_g", [ncl, 256])
    mla_w_ukv = din("mla_w_ukv", [ncl, 256, 4096])
    mla_w_out = din("mla_w_out", [ncl, D, D])
    moe_w_rg = din("moe_w_rg", [dl, D, 4])
    moe_b_rg = din("moe_b_rg", [dl, 4])
    moe_w_re = din("moe_w_re", [dl, D, 16])
    moe_b_re = din("moe_b_re", [dl, 16])
    ne = max(n_exp, 1)
    moe_w_gate = din("moe_w_gate", [dl, ne, D, D_EXP])
    moe_w_up = din("moe_w_up", [dl, ne, D, D_EXP])
    moe_w_down = din("moe_w_down", [dl, ne, D_EXP, D])

    y_out = dout("y", [T, D])
    o_gla = dout("o_gla", [2, 4, 2, 4, 128, 256])
    o_dk = dout("o_dk", [2, T, 1024])
    o_dv = dout("o_dv", [2, T, 1024])
    o_ckv = dout("o_ckv", [2, T, 256])
    o_kr = dout("o_kr", [2, T, 64])
    x_spill = nc.dram_tensor("x_spill", [T, D], F32, kind="Internal").ap()

    SB_BYTES = 207 * 1024
    big = es.enter_context(nc.sbuf_tensor("big", [128, SB_BYTES // 2], BF16))
    mem = Mem(big, SB_BYTES)
    ps = es.enter_context(nc.psum_tensor("ps", [128, 4096], F32))
    dps = [Dep(excl=True) for _ in range(8)]

    def bank(i, n=512, off=0):
        return ps[:, i * 512 + off:i * 512 + off + n]

    def mm_group(out_ap, pairs, reads, writes):
        def fn(e):
            n = len(pairs)
            ins = None
            for i, (l, r) in enumerate(pairs):
                ins = e.matmul(out_ap, lhsT=l, rhs=r, start=(i == 0), stop=(i == n - 1))
            return ins
        return K.op(K.pe, fn, reads, writes)

    identF = mem.alloc([128, 128], F32)
    identB = mem.alloc([128, 128], BF16)
    S_rep = mem.alloc([128, KC, 128], BF16)
    modcol = mem.alloc([128, 4, KC], F32)
    adab_col = mem.alloc([128, 96], F32)
    gate_bc = mem.alloc([128, D], F32)
    smallc = mem.alloc([128, 64], F32)
    d_identF, d_identB, d_Srep, d_modcol, d_adab, d_gate, d_small = (Dep() for _ in range(7))

    X_OFF = (mem.top + 63) // 64 * 64
    X = mem.alloc([128, NT, D], F32)
    dX = [Dep() for _ in range(NT)]
    PH_X = mem.top
    PH_NOX = X_OFF

    K.dma(K.q_sync, identF, identF_d, writes=[d_identF])
    K.op(K.dve, lambda e: e.tensor_copy(out=identB, in_=identF), reads=[d_identF], writes=[d_identB])
    xv = x_in.rearrange("(t p) d -> p t d", p=128)
    for t in range(NT):
        K.dma(K.q_sync, X[:, t, :], xv[:, t, :], writes=[dX[t]])

    mem.top = PH_X
    c16 = mem.alloc([16, 128], F32)
    scol = mem.alloc([128, KC], F32)
    d_c16, d_scol = Dep(), Dep()
    K.dma(K.q_sync, c16, cond.rearrange("(c p) -> c p", p=128), writes=[d_c16])
    K.op(K.pe, lambda e: e.transpose(out=bank(0, 16), in_=c16, identity=identF[0:16, 0:16]),
         reads=[d_c16, d_identF], writes=[dps[0]])
    K.op(K.act, lambda e: e.activation(out=scol, in_=bank(0, 16), func=AF.Silu), reads=[dps[0]], writes=[d_scol])
    K.op(K.dve, lambda e: e.tensor_copy(out=S_rep, in_=scol.unsqueeze(2).to_broadcast([128, KC, 128])),
         reads=[d_scol], writes=[d_Srep])
    K.barrier()
    mem.top = PH_X

    def emit_mod(l, half, ring):
        adaw_v = ada_w[l].rearrange("(c p) n -> p c n", p=128)
        if half == 0:
            ab96 = mem.alloc([96, 128], F32)
            d96 = Dep()
            K.dma(K.q_sync, ab96, ada_b[l].rearrange("(c p) -> c p", p=128), writes=[d96])
            K.op(K.pe, lambda e: e.transpose(out=bank(0, 96), in_=ab96, identity=identF[0:96, 0:96]),
                 reads=[d96, d_identF], writes=[dps[0]])
            K.op(K.act, lambda e: e.copy(out=adab_col, in_=bank(0, 96)), reads=[dps[0]], writes=[d_adab])
        tmp = mem.alloc([128, 256], F32)
        d_tmp = Dep()
        for si in range(3):
            seg = half * 3 + si
            if si == 2:
                K.dma(K.q_sync, gate_bc, ada_b[l, seg * D:(seg + 1) * D].partition_broadcast(128),
                      writes=[d_gate])
            for blk in range(8):
                c0 = seg * D + blk * 256
                (w,), dw = ring.load([(adaw_v[:, :, c0:c0 + 256], KC, 256)])
                pb = (blk + si * 8) % 2
                po = bank(pb, 256)
                mm_group(po, [(S_rep[:, kc, :], w[:, kc, :]) for kc in range(KC)],
                         reads=[d_Srep, dw], writes=[dps[pb]])
                if si < 2:
                    mc = half * 2 + si
                    K.op(K.dve, lambda e: e.tensor_tensor(
                        out=tmp.rearrange("p (c j) -> p c j", c=2), in0=po.rearrange("p (c j) -> p c j", c=2),
                        in1=identF.unsqueeze(1).to_broadcast([128, 2, 128]), op=ALU.mult),
                        reads=[dps[pb], d_identF], writes=[d_tmp])
                    K.op(K.dve, lambda e: e.tensor_reduce(
                        out=modcol[:, mc, blk * 2:blk * 2 + 2], in_=tmp.rearrange("p (c j) -> p c j", c=2),
                        axis=AX.X, op=ALU.add), reads=[d_tmp], writes=[d_modcol])
                else:
                    K.op(K.dve, lambda e: e.tensor_tensor(
                        out=gate_bc[:, blk * 256:(blk + 1) * 256], in0=gate_bc[:, blk * 256:(blk + 1) * 256],
                        in1=po, op=ALU.add), reads=[dps[pb], d_gate], writes=[d_gate])
            if si < 2:
                mc = half * 2 + si
                K.op(K.dve, lambda e: e.tensor_tensor(
                    out=modcol[:, mc, :], in0=modcol[:, mc, :], in1=adab_col[:, seg * KC:(seg + 1) * KC],
                    op=ALU.add), reads=[d_modcol, d_adab], writes=[d_modcol])
                if si == 1:
                    K.op(K.dve, lambda e: e.tensor_scalar(
                        out=modcol[:, mc, :], in0=modcol[:, mc, :], scalar1=1.0, scalar2=None, op0=ALU.add),
                        reads=[d_modcol], writes=[d_modcol])

    def emit_convert(half, hT, d_hT, router=None):
        sh, sc = half * 2, half * 2 + 1
        for t in range(NT):
            for g in range(4):
                pb = 2 + (t * 4 + g) % 4
                def tr(e, t=t, g=g, pb=pb):
                    ins = None
                    for j in range(4):
                        kc = g * 4 + j
                        ins = e.transpose(out=bank(pb, 128, j * 128), in_=X[:, t, kc * 128:(kc + 1) * 128],
                                          identity=identF)
                    return ins
                K.op(K.pe, tr, reads=[dX[t], d_identF], writes=[dps[pb]])
                for j in range(4):
                    kc = g * 4 + j
                    K.op(K.act, lambda e, kc=kc, j=j, pb=pb, t=t: e.activation(
                        out=hT[:, kc, t * 128:(t + 1) * 128], in_=bank(pb, 128, j * 128), func=AF.Identity,
                        scale=modcol[:, sc, kc:kc + 1], bias=modcol[:, sh, kc:kc + 1]),
                        reads=[dps[pb], d_modcol], writes=[d_hT[t]])
                    if router is not None:
                        hF, d_hF = router["hF"], router["d_hF"]
                        K.op(K.dve, lambda e, kc=kc, j=j, pb=pb: e.tensor_scalar(
                            out=hF[:, kc, :], in0=bank(pb, 128, j * 128), scalar1=modcol[:, sc, kc:kc + 1],
                            scalar2=modcol[:, sh, kc:kc + 1], op0=ALU.mult, op1=ALU.add),
                            reads=[dps[pb], d_modcol], writes=[d_hF])
            if router is not None:
                emit_route(t, router)

    def emit_route(t, R):
        hF, d_hF, wr, d_wr, comb, d_comb, rb, d_rb, rt, d_rt = (R[k] for k in (
            "hF", "d_hF", "wr", "d_wr", "comb", "d_comb", "rb", "d_rb", "rt", "d_rt"))
        mm_group(bank(1, 20), [(hF[:, kc, :], wr[:, kc, :]) for kc in range(KC)],
                 reads=[d_hF, d_wr], writes=[dps[1]])
        V = K.dve
        lg = rt[:, 0:20]
        def dv(fn, reads=(), writes=()):
            return K.op(V, fn, reads=list(reads) + [d_rt], writes=list(writes) + [d_rt])
        dv(lambda e: e.tensor_tensor(out=lg, in0=bank(1, 20), in1=rb, op=ALU.add), reads=[dps[1], d_rb])
        gl = rt[:, 0:4]
        el = rt[:, 4:20].rearrange("p (g j) -> p g j", g=4)
        gmax, gsum, m1, m2, e2, den, w1, w2 = (rt[:, 20 + i:21 + i] for i in range(8))
        ohg = rt[:, 32:36]
        tmp16 = rt[:, 36:52].rearrange("p (g j) -> p g j", g=4)
        esel = rt[:, 52:56]
        oh1 = rt[:, 56:60]
        msk = rt[:, 60:64]
        oh2 = rt[:, 64:68]
        cig = rt[:, 68:72]
        gex = rt[:, 72:76]
        dv(lambda e: e.reduce_max(out=gmax, in_=gl, axis=AX.X))
        dv(lambda e: e.tensor_scalar(out=ohg, in0=gl, scalar1=gmax, scalar2=None, op0=ALU.is_equal))
        dv(lambda e: e.tensor_scalar(out=gex, in0=gl, scalar1=gmax, scalar2=None, op0=ALU.subtract))
        K.op(K.act, lambda e: e.activation(out=gex, in_=gex, func=AF.Exp, accum_out=gsum),
             reads=[d_rt], writes=[d_rt])
        dv(lambda e: e.tensor_tensor(out=tmp16, in0=el, in1=ohg.unsqueeze(2).to_broadcast([128, 4, 4]),
                                     op=ALU.mult))
        dv(lambda e: e.tensor_reduce(out=esel, in_=tmp16.rearrange("p g j -> p j g"), axis=AX.X, op=ALU.add))
        dv(lambda e: e.reduce_max(out=m1, in_=esel, axis=AX.X))
        dv(lambda e: e.tensor_scalar(out=oh1, in0=esel, scalar1=m1, scalar2=None, op0=ALU.is_equal))
        dv(lambda e: e.scalar_tensor_tensor(out=msk, in0=oh1, scalar=-1e30, in1=esel, op0=ALU.mult, op1=ALU.add))
        dv(lambda e: e.reduce_max(out=m2, in_=msk, axis=AX.X))
        dv(lambda e: e.tensor_scalar(out=oh2, in0=msk, scalar1=m2, scalar2=None, op0=ALU.is_equal))
        dv(lambda e: e.tensor_tensor(out=e2, in0=m2, in1=m1, op=ALU.subtract))
        K.op(K.act, lambda e: e.activation(out=e2, in_=e2, func=AF.Exp), reads=[d_rt], writes=[d_rt])
        dv(lambda e: e.scalar_tensor_tensor(out=den, in0=e2, scalar=1.0, in1=gsum, op0=ALU.add, op1=ALU.mult))
        dv(lambda e: e.reciprocal(out=w1, in_=den))
        dv(lambda e: e.tensor_tensor(out=w2, in0=e2, in1=w1, op=ALU.mult))
        dv(lambda e: e.tensor_scalar(out=cig, in0=oh1, scalar1=w1, scalar2=None, op0=ALU.mult))
        dv(lambda e: e.scalar_tensor_tensor(out=cig, in0=oh2, scalar=w2, in1=cig, op0=ALU.mult, op1=ALU.add))
        K.op(V, lambda e: e.tensor_tensor(
            out=comb[:, t, :].rearrange("p (g j) -> p g j", g=4),
            in0=ohg.unsqueeze(2).to_broadcast([128, 4, 4]), in1=cig.unsqueeze(1).to_broadcast([128, 4, 4]),
            op=ALU.mult), reads=[d_rt], writes=[d_comb[t]])

    def emit_ln_tile(t, gb, d_gb, scr, d_scr):
        st, mv, cols = scr
        lnstop = cfg.get("lnstop", 99)
        if lnstop < 1:
            return
        for c in range(4):
            K.op(K.dve, lambda e, c=c: e.bn_stats(out=st[:, c, :], in_=X[:, t, c * 512:(c + 1) * 512]),
                 reads=[dX[t]], writes=[d_scr])
        K.op(K.dve, lambda e: e.bn_aggr(out=mv, in_=st), reads=[d_scr], writes=[d_scr])
        if lnstop < 2:
            return
        K.op(K.dve, lambda e: e.tensor_scalar(out=cols[:, 0:1], in0=mv[:, 1:2], scalar1=LN_EPS, scalar2=None,
                                              op0=ALU.add), reads=[d_scr], writes=[d_scr])
        K.op(K.act, lambda e: e.activation(out=cols[:, 0:1], in_=cols[:, 0:1], func=AF.Ln),
             reads=[d_scr], writes=[d_scr])
        K.op(K.act, lambda e: e.activation(out=cols[:, 0:1], in_=cols[:, 0:1], func=AF.Exp, scale=-0.5),
             reads=[d_scr], writes=[d_scr])
        K.op(K.dve, lambda e: e.scalar_tensor_tensor(out=cols[:, 1:2], in0=mv[:, 0:1], scalar=-1.0,
                                                     in1=cols[:, 0:1], op0=ALU.mult, op1=ALU.mult),
             reads=[d_scr], writes=[d_scr])
        if lnstop < 3:
            return
        K.op(K.act, lambda e: e.activation(out=X[:, t, :], in_=X[:, t, :], func=AF.Identity,
                                           scale=cols[:, 0:1], bias=cols[:, 1:2]),
             reads=[d_scr], writes=[dX[t]])
        if lnstop < 4:
            return
        K.op(K.dve, lambda e: e.tensor_tensor(out=X[:, t, :], in0=X[:, t, :], in1=gb[:, 0, :], op=ALU.mult),
             reads=[d_gb], writes=[dX[t]])
        K.op(K.dve, lambda e: e.tensor_tensor(out=X[:, t, :], in0=X[:, t, :], in1=gb[:, 1, :], op=ALU.add),
             reads=[d_gb], writes=[dX[t]])

    def load_ln(l, which):
        gb = mem.alloc([128, 2, D], F32)
        d_gb = Dep()
        K.dma(K.q_sync, gb[:, 0, :], ln_g[l, which].partition_broadcast(128), writes=[d_gb])
        K.dma(K.q_sync, gb[:, 1, :], ln_b[l, which].partition_broadcast(128), writes=[d_gb])
        st = mem.alloc([128, 4, 6], F32)
        mv = mem.alloc([128, 2], F32)
        cols = mem.alloc([128, 2], F32)
        return gb, d_gb, (st, mv, cols), Dep()

    def emit_moe(l):
        mem.top = PH_X
        ring = Ring(K, mem, 6, 4096)
        emit_mod(l, 1, ring)
        chk("mod1")
        hT = mem.alloc([128, KC, T], BF16)
        d_hT = [Dep() for _ in range(NT)]
        R = {}
        R["hF"] = mem.alloc([128, KC, 128], F32); R["d_hF"] = Dep()
        R["wr"] = mem.alloc([128, KC, 20], F32); R["d_wr"] = Dep()
        R["comb"] = mem.alloc([128, NT, 16], F32); R["d_comb"] = [Dep() for _ in range(NT)]
        R["rb"] = mem.alloc([128, 20], F32); R["d_rb"] = Dep()
        R["rt"] = mem.alloc([128, 80], F32); R["d_rt"] = Dep()
        K.dma(K.q_sync, R["wr"][:, :, 0:4], moe_w_rg[l].rearrange("(c p) n -> p c n", p=128), writes=[R["d_wr"]])
        K.dma(K.q_sync, R["wr"][:, :, 4:20], moe_w_re[l].rearrange("(c p) n -> p c n", p=128), writes=[R["d_wr"]])
        K.dma(K.q_sync, R["rb"][:, 0:4], moe_b_rg[l].partition_broadcast(128), writes=[R["d_rb"]])
        K.dma(K.q_sync, R["rb"][:, 4:20], moe_b_re[l].partition_broadcast(128), writes=[R["d_rb"]])
        emit_convert(1, hT, d_hT, router=R)
        comb, d_comb = R["comb"], R["d_comb"]
        chk("conv1")

        actT = mem.alloc([128, 4, T], BF16)
        d_act = [[Dep() for _ in range(2)] for _ in range(4)]
        sg = [mem.alloc([128, 512], BF16) for _ in range(2)]
        d_sg = [Dep(), Dep()]
        acc = [mem.alloc([128, 512], F32) for _ in range(4)]
        d_acc = [Dep() for _ in range(4)]
        dbanks = [0, 1, 6, 7]
        gb, d_gb, scr, d_scr = load_ln(l, 1)
        ui = 0
        di = 0
        for ei in range(n_exp):
            wg = moe_w_gate[l, ei].rearrange("(c p) f -> p c f", p=128)
            wu = moe_w_up[l, ei].rearrange("(c p) f -> p c f", p=128)
            wd = moe_w_down[l, ei].rearrange("(c p) d -> p c d", p=128)
            dsl = []
            for fp in range(2):
                (g_w,), d_g = ring.load([(wg[:, :, fp * 256:(fp + 1) * 256], KC, 256)])
                (u_w,), d_u = ring.load([(wu[:, :, fp * 256:(fp + 1) * 256], KC, 256)])
                for fi in range(2):
                    f = fp * 2 + fi
                    for hf in range(2):
                        pg, pu = 2 + (ui % 2) * 2, 3 + (ui % 2) * 2
                        ui += 1
                        tok = slice(hf * 512, (hf + 1) * 512)
                        rd = [d_hT[t] for t in range(hf * 4, hf * 4 + 4)]
                        mm_group(bank(pg), [(g_w[:, kc, fi * 128:(fi + 1) * 128], hT[:, kc, tok]) for kc in range(KC)],
                                 reads=rd + [d_g], writes=[dps[pg]])
                        mm_group(bank(pu), [(u_w[:, kc, fi * 128:(fi + 1) * 128], hT[:, kc, tok]) for kc in range(KC)],
                                 reads=rd + [d_u], writes=[dps[pu]])
                        s = ui % 2
                        K.op(K.act, lambda e, s=s, pg=pg: e.activation(out=sg[s], in_=bank(pg), func=AF.Silu),
                             reads=[dps[pg]], writes=[d_sg[s]])
                        K.op(K.dve, lambda e, s=s, pu=pu, f=f, tok=tok: e.tensor_tensor(
                            out=actT[:, f, tok], in0=sg[s], in1=bank(pu), op=ALU.mult),
                            reads=[d_sg[s], dps[pu]], writes=[d_act[f][hf]])
            for fp in range(2):
                (d_w,), d_d = ring.load([(wd[:, fp * 2:fp * 2 + 2, :], 2, D)])
                dsl.append((d_w, d_d))
            for t in range(NT):
                hf = t // 4
                for db in range(4):
                    pb = dbanks[di % 4]
                    di += 1
                    pairs = [(actT[:, f, t * 128:(t + 1) * 128], dsl[f // 2][0][:, f % 2, db * 512:(db + 1) * 512])
                             for f in range(4)]
                    mm_group(bank(pb), pairs, reads=[d_act[f][hf] for f in range(4)] + [dsl[0][1], dsl[1][1]],
                             writes=[dps[pb]])
                    a = di % 4
                    K.op(K.dve, lambda e, a=a, pb=pb, t=t, db=db, ei=ei: e.scalar_tensor_tensor(
                        out=acc[a], in0=bank(pb), scalar=comb[:, t, ei:ei + 1], in1=gate_bc[:, db * 512:(db + 1) * 512],
                        op0=ALU.mult, op1=ALU.mult), reads=[dps[pb], d_comb[t], d_gate], writes=[d_acc[a]])
                    xs = X[:, t, db * 512:(db + 1) * 512]
                    if ei == 0:
                        K.op(K.dve, lambda e, a=a, xs=xs: e.scalar_tensor_tensor(
                            out=xs, in0=xs, scalar=ALPHA, in1=acc[a], op0=ALU.mult, op1=ALU.add),
                            reads=[d_acc[a]] + d_hT, writes=[dX[t]])
                    else:
                        K.op(K.dve, lambda e, a=a, xs=xs: e.tensor_tensor(out=xs, in0=xs, in1=acc[a], op=ALU.add),
                             reads=[d_acc[a]], writes=[dX[t]])
        chk("moe")
        for t in range(NT):
            emit_ln_tile(t, gb, d_gb, scr, d_scr)
        K.barrier()
        mem.top = PH_X


    def rstd_from_ss(col, n, dep):
        K.op(K.dve, lambda e: e.tensor_scalar(out=col, in0=col, scalar1=1.0 / n, scalar2=RMS_EPS,
                                              op0=ALU.mult, op1=ALU.add), reads=[dep], writes=[dep])
        K.op(K.act, lambda e: e.activation(out=col, in_=col, func=AF.Ln), reads=[dep], writes=[dep])
        K.op(K.act, lambda e: e.activation(out=col, in_=col, func=AF.Exp, scale=-0.5), reads=[dep], writes=[dep])

    def emit_out_proj(l, w_out_l, oT, d_oT, ring):
        wv = w_out_l.rearrange("(c p) n -> p c n", p=128)
        tmpo = [mem.alloc([128, 256], F32) for _ in range(2)]
        d_tmpo = [Dep(), Dep()]
        gb, d_gb, scr, d_scr = load_ln(l, 0)
        n = 0
        for blk in range(8):
            (w,), dw = ring.load([(wv[:, :, blk * 256:(blk + 1) * 256], KC, 256)])
            for t in range(NT):
                pb = n % 4
                a = n % 2
                n += 1
                mm_group(bank(pb, 256), [(oT[:, kc, t * 128:(t + 1) * 128], w[:, kc, :]) for kc in range(KC)],
                         reads=[d_oT, dw], writes=[dps[pb]])
                K.op(K.dve, lambda e, a=a, pb=pb, blk=blk: e.tensor_tensor(
                    out=tmpo[a], in0=bank(pb, 256), in1=gate_bc[:, blk * 256:(blk + 1) * 256], op=ALU.mult),
                    reads=[dps[pb], d_gate], writes=[d_tmpo[a]])
                xs = X[:, t, blk * 256:(blk + 1) * 256]
                K.op(K.dve, lambda e, a=a, xs=xs: e.scalar_tensor_tensor(
                    out=xs, in0=xs, scalar=ALPHA, in1=tmpo[a], op0=ALU.mult, op1=ALU.add),
                    reads=[d_tmpo[a]], writes=[dX[t]])
        for t in range(NT):
            emit_ln_tile(t, gb, d_gb, scr, d_scr)

    def attn_scores(sb0, qparts, kparts):
        deps_r = []
        for (q, dq), (k, dk_) in zip(qparts, kparts):
            deps_r += [dq, dk_]
        for j, (c0, n) in enumerate(((0, 512), (512, 512), (1024, 256))):
            mm_group(bank(sb0 + j, n), [(q, k[:, c0:c0 + n]) for (q, _), (k, _) in zip(qparts, kparts)],
                     reads=deps_r, writes=[dps[sb0 + j]])

    def softmax_exp(sb0, scale, e_out, d_e, cols, d_cols, ci):
        sc = ps[:, sb0 * 512: sb0 * 512 + NKEY]
        rd = [dps[sb0], dps[sb0 + 1], dps[sb0 + 2]]
        K.op(K.dve, lambda e: e.reduce_max(out=cols[:, ci:ci + 1], in_=sc, axis=AX.X), reads=rd, writes=[d_cols])
        K.op(K.dve, lambda e: e.tensor_scalar(out=cols[:, ci:ci + 1], in0=cols[:, ci:ci + 1], scalar1=-scale,
                                              scalar2=None, op0=ALU.mult), reads=[d_cols], writes=[d_cols])
        K.op(K.act, lambda e: e.activation(out=e_out, in_=sc, func=AF.Exp, scale=scale, bias=cols[:, ci:ci + 1],
                                           accum_out=cols[:, ci + 1:ci + 2]), reads=rd + [d_cols], writes=[d_e, d_cols])

    def attn_pv(e_in, d_e, eT, d_eT, V, d_V, vcol0, out_ps_off):
        pT6 = bank(6).bitcast(BF16)
        pT7 = bank(7, 128).bitcast(BF16)
        def tr(e):
            ins = None
            for kt in range(NKT):
                dst = pT6[:, kt * 128:(kt + 1) * 128] if kt < 8 else pT7[:, (kt - 8) * 128:(kt - 7) * 128]
                ins = e.transpose(out=dst, in_=e_in[:, kt * 128:(kt + 1) * 128], identity=identB)
            return ins
        K.op(K.pe, tr, reads=[d_e, d_identB], writes=[dps[6], dps[7]])
        K.op(K.act, lambda e: e.copy(out=eT[:, 0:8, :], in_=pT6.rearrange("p (a b) -> p a b", a=8)),
             reads=[dps[6]], writes=[d_eT])
        K.op(K.act, lambda e: e.copy(out=eT[:, 8:10, :], in_=pT7.rearrange("p (a b) -> p a b", a=2)),
             reads=[dps[7]], writes=[d_eT])
        mm_group(bank(7, 128, out_ps_off), [(eT[:, kt, :], V[:, kt, vcol0:vcol0 + 128]) for kt in range(NKT)],
                 reads=[d_eT, d_V], writes=[dps[7]])

    def emit_mixer_mla(l):
        i = l // 2
        mem.top = PH_X
        ring = Ring(K, mem, 4, 4096)
        emit_mod(l, 0, ring)
        cqnT = mem.alloc([128, 4, T], BF16); d_cqnT = Dep()
        ckvT = mem.alloc([128, 2, NKEY], BF16); d_ckvT = Dep()
        krT = mem.alloc([68, NKEY], BF16); d_krT = Dep()
        oT = mem.alloc([128, KC, T], BF16); d_oT = Dep()
        cs_tm = mem.alloc([128, NT, 2, 32], F32); d_cs = Dep()
        gq = mem.alloc([128, 512], F32); gkv = mem.alloc([128, 256], F32); d_g = Dep()
        K.dma(K.q_sync, cs_tm[:, :, 0, :], rope_cos.rearrange("(t p) r -> p t r", p=128), writes=[d_cs])
        K.dma(K.q_sync, cs_tm[:, :, 1, :], rope_sin.rearrange("(t p) r -> p t r", p=128), writes=[d_cs])
        K.dma(K.q_sync, gq, mla_q_norm_g[i].partition_broadcast(128), writes=[d_g])
        K.dma(K.q_sync, gkv, mla_kv_norm_g[i].partition_broadcast(128), writes=[d_g])
        K.dma(K.q_pool, krT[64:68, :], kmask, writes=[d_krT])
        mark1 = mem.top
        hT = mem.alloc([128, KC, T], BF16)
        d_hT = [Dep() for _ in range(NT)]
        emit_convert(0, hT, d_hT)
        wv = mla_w_in[i].rearrange("(c p) n -> p c n", p=128)
        wblk = []
        for c0, n in ((0, 256), (256, 256), (512, 256), (768, 64)):
            (w,), dw = ring.load([(wv[:, :, c0:c0 + n], KC, n)])
            wblk.append((w, dw, n))
        cch = mem.alloc([128, 2, 256], F32); d_cch = Dep()
        kch = mem.alloc([128, 2, 64], F32); d_kch = Dep()
        K.dma(K.q_sync, cch, c_ckv[i].rearrange("(t p) f -> p t f", p=128), writes=[d_cch])
        K.dma(K.q_sync, kch, c_kr[i].rearrange("(t p) f -> p t f", p=128), writes=[d_kch])
        cqn = mem.alloc([128, 512], F32); ckvn = mem.alloc([128, 256], F32); krf = mem.alloc([128, 64], F32)
        krr = mem.alloc([128, 64], F32); rt1 = mem.alloc([128, 32], F32); rt2 = mem.alloc([128, 32], F32)
        junk = mem.alloc([128, 512], BF16); c1 = mem.alloc([128, 4], F32)
        d_cqn, d_ckvn, d_krf, d_krr, d_junk, d_c1 = (Dep() for _ in range(6))
        for tt in range(2):
            def trc(e, tt=tt):
                ins = None
                for c in range(2):
                    ins = e.transpose(out=bank(0, 128, c * 128), in_=cch[:, tt, c * 128:(c + 1) * 128], identity=identF)
                ins = e.transpose(out=bank(0, 128, 256)[0:64, :], in_=kch[:, tt, :], identity=identF)
                return ins
            K.op(K.pe, trc, reads=[d_cch, d_kch, d_identF], writes=[dps[0]])
            K.op(K.act, lambda e, tt=tt: e.copy(out=ckvT[:, :, tt * 128:(tt + 1) * 128],
                                                in_=bank(0, 256).rearrange("p (c j) -> p c j", c=2)),
                 reads=[dps[0]], writes=[d_ckvT])
            K.op(K.act, lambda e, tt=tt: e.copy(out=krT[0:64, tt * 128:(tt + 1) * 128], in_=bank(0, 128, 256)[0:64, :]),
                 reads=[dps[0]], writes=[d_krT])
        ov_ckv = o_ckv[i].rearrange("(t p) f -> p t f", p=128)
        ov_kr = o_kr[i].rearrange("(t p) f -> p t f", p=128)
        for t in range(NT):
            tk = slice(t * 128, (t + 1) * 128)
            pa, pb_ = 2 + (t % 2) * 2, 3 + (t % 2) * 2
            for j in range(2):
                mm_group(bank(pa, 256, j * 256), [(hT[:, kc, tk], wblk[j][0][:, kc, :]) for kc in range(KC)],
                         reads=[d_hT[t], wblk[j][1]], writes=[dps[pa]])
            mm_group(bank(pb_, 256), [(hT[:, kc, tk], wblk[2][0][:, kc, :]) for kc in range(KC)],
                     reads=[d_hT[t], wblk[2][1]], writes=[dps[pb_]])
            mm_group(bank(pb_, 64, 256), [(hT[:, kc, tk], wblk[3][0][:, kc, :]) for kc in range(KC)],
                     reads=[d_hT[t], wblk[3][1]], writes=[dps[pb_]])
            K.op(K.act, lambda e, pa=pa: e.activation(out=junk, in_=bank(pa), func=AF.Square, accum_out=c1[:, 0:1]),
                 reads=[dps[pa]], writes=[d_junk, d_c1])
            K.op(K.act, lambda e, pb_=pb_: e.activation(out=junk[:, 0:256], in_=bank(pb_, 256), func=AF.Square,
                                                        accum_out=c1[:, 1:2]), reads=[dps[pb_]], writes=[d_junk, d_c1])
            rstd_from_ss(c1[:, 0:1], 512.0, d_c1)
            rstd_from_ss(c1[:, 1:2], 256.0, d_c1)
            K.op(K.dve, lambda e, pa=pa: e.scalar_tensor_tensor(out=cqn, in0=bank(pa), scalar=c1[:, 0:1], in1=gq,
                                                                op0=ALU.mult, op1=ALU.mult),
                 reads=[dps[pa], d_c1, d_g], writes=[d_cqn])
            K.op(K.dve, lambda e, pb_=pb_: e.scalar_tensor_tensor(out=ckvn, in0=bank(pb_, 256), scalar=c1[:, 1:2], in1=gkv,
                                                                  op0=ALU.mult, op1=ALU.mult),
                 reads=[dps[pb_], d_c1, d_g], writes=[d_ckvn])
            K.op(K.act, lambda e, pb_=pb_: e.copy(out=krf, in_=bank(pb_, 64, 256)), reads=[dps[pb_]], writes=[d_krf])
            K.dma(K.q_sync, ov_ckv[:, t, :], ckvn, reads=[d_ckvn])
            K.dma(K.q_sync, ov_kr[:, t, :], krf, reads=[d_krf])
            cos_t, sin_t = cs_tm[:, t, 0, :], cs_tm[:, t, 1, :]
            x1, x2 = krf[:, 0:32], krf[:, 32:64]
            V_ = K.dve
            K.op(V_, lambda e: e.tensor_tensor(out=rt1, in0=x2, in1=sin_t, op=ALU.mult), reads=[d_krf, d_cs], writes=[d_krr])
            K.op(V_, lambda e: e.tensor_tensor(out=krr[:, 0:32], in0=x1, in1=cos_t, op=ALU.mult), reads=[d_krf, d_cs], writes=[d_krr])
            K.op(V_, lambda e: e.tensor_tensor(out=krr[:, 0:32], in0=krr[:, 0:32], in1=rt1, op=ALU.subtract), reads=[d_krr], writes=[d_krr])
            K.op(V_, lambda e: e.tensor_tensor(out=rt2, in0=x1, in1=sin_t, op=ALU.mult), reads=[d_krf, d_cs], writes=[d_krr])
            K.op(V_, lambda e: e.tensor_tensor(out=krr[:, 32:64], in0=x2, in1=cos_t, op=ALU.mult), reads=[d_krf, d_cs], writes=[d_krr])
            K.op(V_, lambda e: e.tensor_tensor(out=krr[:, 32:64], in0=krr[:, 32:64], in1=rt2, op=ALU.add), reads=[d_krr], writes=[d_krr])
            def tr1(e):
                ins = None
                for c in range(4):
                    ins = e.transpose(out=bank(0, 128, c * 128), in_=cqn[:, c * 128:(c + 1) * 128], identity=identF)
                return ins
            K.op(K.pe, tr1, reads=[d_cqn, d_identF], writes=[dps[0]])
            K.op(K.act, lambda e, tk=tk: e.copy(out=cqnT[:, :, tk], in_=bank(0).rearrange("p (c j) -> p c j", c=4)),
                 reads=[dps[0]], writes=[d_cqnT])
            def tr2(e):
                ins = None
                for c in range(2):
                    ins = e.transpose(out=bank(1, 128, c * 128), in_=ckvn[:, c * 128:(c + 1) * 128], identity=identF)
                ins = e.transpose(out=bank(1, 128, 256)[0:64, :], in_=krr, identity=identF)
                return ins
            K.op(K.pe, tr2, reads=[d_ckvn, d_krr, d_identF], writes=[dps[1]])
            kk = slice(PAST + t * 128, PAST + (t + 1) * 128)
            K.op(K.act, lambda e, kk=kk: e.copy(out=ckvT[:, :, kk], in_=bank(1, 256).rearrange("p (c j) -> p c j", c=2)),
                 reads=[dps[1]], writes=[d_ckvT])
            K.op(K.act, lambda e, kk=kk: e.copy(out=krT[0:64, kk], in_=bank(1, 128, 256)[0:64, :]),
                 reads=[dps[1]], writes=[d_krT])
        K.barrier()
        mem.top = mark1
        rC = mem.alloc([64, T], F32); rS = mem.alloc([64, T], F32); d_rCS = Dep()
        K.dma(K.q_sync, rC, ropeT_c, writes=[d_rCS])
        K.dma(K.q_sync, rS, ropeT_s, writes=[d_rCS])
        qTn = mem.alloc([128, 2, T], BF16); d_qTn = Dep()
        qTr = mem.alloc([68, 2, T], BF16); d_qTr = Dep()
        for hh in range(2):
            K.dma(K.q_pool, qTr[64:68, hh, :], seqsel, writes=[d_qTr])
        kTn = mem.alloc([128, 2, NKEY], BF16); d_kTn = Dep()
        Vh = mem.alloc([128, NKT, 256], BF16); d_Vh = Dep()
        wsw = mem.alloc([128, 4, 2, 64], BF16); d_wsw = Dep()
        tq = mem.alloc([64, 512], F32); d_tq = Dep()
        tq2 = mem.alloc([64, 512], F32); d_tq2 = Dep()
        eb = [mem.alloc([128, NKEY], BF16) for _ in range(2)]; d_eb = [Dep(), Dep()]
        eT = mem.alloc([128, NKT, 128], BF16); d_eT = Dep()
        on = mem.alloc([128, 128], F32); d_on = Dep()
        cols2 = [mem.alloc([128, 8], F32) for _ in range(2)]; d_cols2 = [Dep(), Dep()]
        wq_v = mla_w_uq[i].rearrange("(c p) n -> p c n", p=128)
        wkv_v = mla_w_ukv[i].rearrange("(c p) n -> p c n", p=128)
        scale = (128 + 64) ** -0.5
        it = 0
        for hp in range(8):
            (wq,), d_wq = ring.load([(wq_v[:, :, hp * 384:(hp + 1) * 384], 4, 384)])
            (wkv,), d_wkv = ring.load([(wkv_v[:, :, hp * 512:(hp + 1) * 512], 2, 512)])
            for hh in range(2):
                b0 = hh * 192 + 128
                K.op(K.dve, lambda e, hh=hh, b0=b0: e.tensor_copy(out=wsw[:, :, hh, 0:32], in_=wq[:, :, b0 + 32:b0 + 64]),
                     reads=[d_wq], writes=[d_wsw])
                K.op(K.dve, lambda e, hh=hh, b0=b0: e.tensor_copy(out=wsw[:, :, hh, 32:64], in_=wq[:, :, b0:b0 + 32]),
                     reads=[d_wq], writes=[d_wsw])
            for hh in range(2):
                for hf in range(2):
                    tok = slice(hf * 512, (hf + 1) * 512)
                    mm_group(bank(0), [(wq[:, c, hh * 192:hh * 192 + 128], cqnT[:, c, tok]) for c in range(4)],
                             reads=[d_wq, d_cqnT], writes=[dps[0]])
                    K.op(K.act, lambda e, hh=hh, tok=tok: e.copy(out=qTn[:, hh, tok], in_=bank(0)), reads=[dps[0]], writes=[d_qTn])
                    mm_group(bank(1)[0:64, :], [(wq[:, c, hh * 192 + 128:hh * 192 + 192], cqnT[:, c, tok]) for c in range(4)],
                             reads=[d_wq, d_cqnT], writes=[dps[1]])
                    mm_group(bank(2)[0:64, :], [(wsw[:, c, hh, :], cqnT[:, c, tok]) for c in range(4)],
                             reads=[d_wsw, d_cqnT], writes=[dps[2]])
                    K.op(K.dve, lambda e, tok=tok: e.tensor_tensor(out=tq, in0=bank(2)[0:64, :], in1=rS[:, tok], op=ALU.mult),
                         reads=[dps[2], d_rCS], writes=[d_tq])
                    K.op(K.dve, lambda e, tok=tok: e.tensor_tensor(out=tq2, in0=bank(1)[0:64, :], in1=rC[:, tok], op=ALU.mult),
                         reads=[dps[1], d_rCS], writes=[d_tq2])
                    K.op(K.dve, lambda e, hh=hh, tok=tok: e.tensor_tensor(out=qTr[0:64, hh, tok], in0=tq, in1=tq2, op=ALU.add),
                         reads=[d_tq, d_tq2], writes=[d_qTr])
                for j, (c0, n) in enumerate(((0, 512), (512, 512), (1024, 256))):
                    pbk = 3 + j
                    mm_group(bank(pbk, n), [(wkv[:, c, hh * 256:hh * 256 + 128], ckvT[:, c, c0:c0 + n]) for c in range(2)],
                             reads=[d_wkv, d_ckvT], writes=[dps[pbk]])
                    K.op(K.act, lambda e, hh=hh, c0=c0, n=n, pbk=pbk: e.copy(out=kTn[:, hh, c0:c0 + n], in_=bank(pbk, n)),
                         reads=[dps[pbk]], writes=[d_kTn])
            for kt in range(NKT):
                pbv = kt % 2
                mm_group(bank(pbv, 256).rearrange("p (a b) -> p a b", a=2),
                         [(ckvT[:, c, kt * 128:(kt + 1) * 128],
                           wkv[:, c, :].rearrange("p (a b) -> p a b", a=2)[:, :, 128:256]) for c in range(2)],
                         reads=[d_wkv, d_ckvT], writes=[dps[pbv]])
                K.op(K.act, lambda e, kt=kt, pbv=pbv: e.copy(out=Vh[:, kt, :], in_=bank(pbv, 256)), reads=[dps[pbv]], writes=[d_Vh])
            pending = None
            for hh in range(2):
                h = hp * 2 + hh
                for qb in range(NT):
                    qs = slice(qb * 128, (qb + 1) * 128)
                    sb0 = (it % 2) * 3
                    es = it % 2
                    it += 1
                    attn_scores(sb0, [(qTn[:, hh, qs], d_qTn), (qTr[:, hh, qs], d_qTr)],
                                [(kTn[:, hh, :], d_kTn), (krT, d_krT)])
                    if pending is not None:
                        pending[0]()
                    softmax_exp(sb0, scale, eb[es], d_eb[es], cols2[es], d_cols2[es], 0)
                    if pending is not None:
                        pending[1]()

                    def stage2a(es=es, hh=hh):
                        attn_pv(eb[es], d_eb[es], eT, d_eT, Vh, d_Vh, hh * 128, 256)

                    def stage2(es=es, hh=hh, h=h, qs=qs):
                        cols, d_cols = cols2[es], d_cols2[es]
                        K.op(K.dve, lambda e: e.reciprocal(out=cols[:, 2:3], in_=cols[:, 1:2]), reads=[d_cols], writes=[d_cols])
                        K.op(K.dve, lambda e: e.tensor_scalar(out=on, in0=bank(7, 128, 256), scalar1=cols[:, 2:3], scalar2=None,
                                                              op0=ALU.mult), reads=[dps[7], d_cols], writes=[d_on])
                        K.op(K.pe, lambda e: e.transpose(out=bank(7, 128, 384), in_=on, identity=identF),
                             reads=[d_on, d_identF], writes=[dps[7]])
                        K.op(K.act, lambda e: e.copy(out=oT[:, h, qs], in_=bank(7, 128, 384)),
                             reads=[dps[7]], writes=[d_oT])
                    pending = (stage2a, stage2)
            pending[0]()
            pending[1]()
        K.barrier()
        mem.top = mark1
        emit_out_proj(l, mla_w_out[i], oT, d_oT, ring)
        K.barrier()
        mem.top = PH_X


    def emit_mixer_ab(l):
        i = l // 2
        lam_init = 0.8 - 0.6 * math.exp(-0.3 * l)
        mem.top = PH_X
        ring = Ring(K, mem, 3, 4096)
        emit_mod(l, 0, ring)
        oT = mem.alloc([128, KC, T], BF16); d_oT = Dep()
        hT = mem.alloc([128, KC, T], BF16)
        d_hT = [Dep() for _ in range(NT)]
        emit_convert(0, hT, d_hT)
        cs_tm = mem.alloc([128, NT, 2, 32], F32); d_cs = Dep()
        K.dma(K.q_sync, cs_tm[:, :, 0, :], rope_cos.rearrange("(t p) r -> p t r", p=128), writes=[d_cs])
        K.dma(K.q_sync, cs_tm[:, :, 1, :], rope_sin.rearrange("(t p) r -> p t r", p=128), writes=[d_cs])
        wv = ab_w_in[i].rearrange("(c p) n -> p c n", p=128)
        markA = mem.top
        xsp = x_spill.rearrange("(t p) d -> p t d", p=128)
        for t in range(NT):
            K.dma(K.q_sync, xsp[:, t, :], X[:, t, :], reads=[dX[t]])
        K.barrier()
        mem.top = PH_NOX
        if not cfg.get("gla", True):
            K.op(K.dve, lambda e: e.memset(oT[:, 0:8, :], 0.0), writes=[d_oT])
        else:
            emit_gla(l, i, hT, d_hT, oT, d_oT, wv, ring)
            K.barrier()
            mem.top = PH_NOX
        lvb = mem.alloc([128, 4, 64], F32); lcol = mem.alloc([128, 8], F32); d_l = Dep()
        gdb = mem.alloc([128, 128], F32); d_gdb = Dep()
        K.dma(K.q_sync, lvb, diff_lambda[i].partition_broadcast(128), writes=[d_l])
        K.dma(K.q_sync, gdb, diff_norm_g[i].partition_broadcast(128), writes=[d_gdb])
        K.op(K.dve, lambda e: e.tensor_scalar(out=gdb, in0=gdb, scalar1=1.0 - lam_init, scalar2=None, op0=ALU.mult),
             reads=[d_gdb], writes=[d_gdb])
        lv4 = lvb.rearrange("p (a b) d -> p a b d", a=2)
        prod = mem.alloc([128, 2, 64], F32)
        K.op(K.dve, lambda e: e.tensor_tensor(out=prod, in0=lv4[:, :, 0, :], in1=lv4[:, :, 1, :], op=ALU.mult),
             reads=[d_l], writes=[d_l])
        K.op(K.dve, lambda e: e.tensor_reduce(out=lcol[:, 0:2], in_=prod, axis=AX.X, op=ALU.add), reads=[d_l], writes=[d_l])
        K.op(K.act, lambda e: e.activation(out=lcol[:, 0:2], in_=lcol[:, 0:2], func=AF.Exp), reads=[d_l], writes=[d_l])
        K.op(K.dve, lambda e: e.tensor_tensor(out=lcol[:, 2:3], in0=lcol[:, 1:2], in1=lcol[:, 0:1], op=ALU.subtract),
             reads=[d_l], writes=[d_l])
        K.op(K.dve, lambda e: e.tensor_scalar(out=lcol[:, 2:3], in0=lcol[:, 2:3], scalar1=-lam_init, scalar2=None,
                                              op0=ALU.add), reads=[d_l], writes=[d_l])
        neg_lam = lcol[:, 2:3]
        qT = mem.alloc([68, 4, T], BF16); d_qT = Dep()
        kT = mem.alloc([68, 4, NKEY], BF16); d_kT = Dep()
        Vd = mem.alloc([128, NKT, 256], BF16); d_Vd = Dep()
        for s4 in range(4):
            K.dma(K.q_pool, qT[64:68, s4, :], seqsel, writes=[d_qT])
            K.dma(K.q_pool, kT[64:68, s4, :], kmask, writes=[d_kT])
        kc_f = mem.alloc([128, 2, 256], F32); d_kcf = Dep()
        k2f = [mem.alloc([128, 256], F32) for _ in range(2)]; d_k2f = [Dep(), Dep()]
        v2f = [mem.alloc([128, 256], F32) for _ in range(2)]; d_v2f = [Dep(), Dep()]
        qr = mem.alloc([128, 4, 64], F32); kr_ = mem.alloc([128, 4, 64], F32); d_qr = Dep(); d_kr = Dep()
        r1 = mem.alloc([128, 4, 32], F32); r2 = mem.alloc([128, 4, 32], F32); d_r = Dep()
        eb4 = [[mem.alloc([128, NKEY], BF16) for _ in range(2)] for _ in range(2)]
        d_eb4 = [[Dep(), Dep()], [Dep(), Dep()]]
        wg = mem.alloc([128, NKEY], BF16); d_wg = Dep()
        eT = mem.alloc([128, NKT, 128], BF16); d_eT = Dep()
        on = mem.alloc([128, 128], F32); d_on = Dep()
        junk = mem.alloc([128, 128], BF16); d_junk = Dep()
        colsj = [[mem.alloc([128, 4], F32) for _ in range(2)] for _ in range(2)]
        d_colsj = [[Dep(), Dep()], [Dep(), Dep()]]
        cols2 = [mem.alloc([128, 8], F32) for _ in range(2)]; d_cols2 = [Dep(), Dep()]
        odk = o_dk[i].rearrange("(t p) f -> p t f", p=128)
        odv = o_dv[i].rearrange("(t p) f -> p t f", p=128)
        cdk = c_dk[i].rearrange("(t p) f -> p t f", p=128)
        cdv = c_dv[i].rearrange("(t p) f -> p t f", p=128)

        def rope_tm(dst, src, rd, t, d_dst):
            cos_b = cs_tm[:, t, 0, :].unsqueeze(1).to_broadcast([128, 4, 32])
            sin_b = cs_tm[:, t, 1, :].unsqueeze(1).to_broadcast([128, 4, 32])
            x1, x2 = src[:, :, 0:32], src[:, :, 32:64]
            V_ = K.dve
            K.op(V_, lambda e: e.tensor_tensor(out=r1, in0=x2, in1=sin_b, op=ALU.mult), reads=rd + [d_cs], writes=[d_r])
            K.op(V_, lambda e: e.tensor_tensor(out=dst[:, :, 0:32], in0=x1, in1=cos_b, op=ALU.mult), reads=rd + [d_cs], writes=[d_dst])
            K.op(V_, lambda e: e.tensor_tensor(out=dst[:, :, 0:32], in0=dst[:, :, 0:32], in1=r1, op=ALU.subtract), reads=[d_r], writes=[d_dst])
            K.op(V_, lambda e: e.tensor_tensor(out=r2, in0=x1, in1=sin_b, op=ALU.mult), reads=rd + [d_cs], writes=[d_r])
            K.op(V_, lambda e: e.tensor_tensor(out=dst[:, :, 32:64], in0=x2, in1=cos_b, op=ALU.mult), reads=rd + [d_cs], writes=[d_dst])
            K.op(V_, lambda e: e.tensor_tensor(out=dst[:, :, 32:64], in0=dst[:, :, 32:64], in1=r2, op=ALU.add), reads=[d_r], writes=[d_dst])

        def tr4(src, d_src, dstT, d_dstT, col0, pb):
            def f(e):
                ins = None
                for s4 in range(4):
                    ins = e.transpose(out=bank(pb, 128, s4 * 128)[0:64, :], in_=src[:, s4, :], identity=identF)
                return ins
            K.op(K.pe, f, reads=[d_src, d_identF], writes=[dps[pb]])
            K.op(K.act, lambda e: e.copy(out=dstT[0:64, :, col0:col0 + 128],
                                         in_=bank(pb)[0:64, :].rearrange("p (a b) -> p a b", a=4)),
                 reads=[dps[pb]], writes=[d_dstT])

        it = 0
        for hp in range(4):
            (wq,), d_wq = ring.load([(wv[:, :, 3104 + hp * 256:3104 + (hp + 1) * 256], KC, 256)])
            (wk,), d_wk = ring.load([(wv[:, :, 4128 + hp * 256:4128 + (hp + 1) * 256], KC, 256)])
            (wvv,), d_wv = ring.load([(wv[:, :, 5152 + hp * 256:5152 + (hp + 1) * 256], KC, 256)])
            K.dma(K.q_sync, kc_f, cdk[:, :, hp * 256:(hp + 1) * 256], writes=[d_kcf])
            K.dma(K.q_pool, Vd[:, 0:2, :], cdv[:, :, hp * 256:(hp + 1) * 256], writes=[d_Vd])
            for tt in range(2):
                tr4(kc_f[:, tt, :].rearrange("p (a b) -> p a b", a=4), d_kcf, kT, d_kT, tt * 128, 6)
            for t in range(NT):
                tk = slice(t * 128, (t + 1) * 128)
                a = t % 2
                pq = 0 + a * 3
                mm_group(bank(pq, 256), [(hT[:, kc, tk], wq[:, kc, :]) for kc in range(KC)], reads=[d_hT[t], d_wq], writes=[dps[pq]])
                mm_group(bank(pq + 1, 256), [(hT[:, kc, tk], wk[:, kc, :]) for kc in range(KC)], reads=[d_hT[t], d_wk], writes=[dps[pq + 1]])
                mm_group(bank(pq + 2, 256), [(hT[:, kc, tk], wvv[:, kc, :]) for kc in range(KC)], reads=[d_hT[t], d_wv], writes=[dps[pq + 2]])
                K.op(K.act, lambda e, a=a, pq=pq: e.copy(out=k2f[a], in_=bank(pq + 1, 256)), reads=[dps[pq + 1]], writes=[d_k2f[a]])
                K.op(K.act, lambda e, a=a, pq=pq: e.copy(out=v2f[a], in_=bank(pq + 2, 256)), reads=[dps[pq + 2]], writes=[d_v2f[a]])
                K.dma(K.q_sync, odk[:, t, hp * 256:(hp + 1) * 256], k2f[a], reads=[d_k2f[a]])
                K.dma(K.q_sync, odv[:, t, hp * 256:(hp + 1) * 256], v2f[a], reads=[d_v2f[a]])
                K.op(K.dve, lambda e, a=a, t=t: e.tensor_copy(out=Vd[:, 2 + t, :], in_=v2f[a]), reads=[d_v2f[a]], writes=[d_Vd])
                rope_tm(qr, bank(pq, 256).rearrange("p (a b) -> p a b", a=4), [dps[pq]], t, d_qr)
                rope_tm(kr_, k2f[a].rearrange("p (a b) -> p a b", a=4), [d_k2f[a]], t, d_kr)
                tr4(qr, d_qr, qT, d_qT, t * 128, 6)
                tr4(kr_, d_kr, kT, d_kT, PAST + t * 128, 6)
            pending = None
            for hh in range(2):
                h = hp * 2 + hh
                for qb in range(NT):
                    qs = slice(qb * 128, (qb + 1) * 128)
                    par = it % 2
                    it += 1
                    for j in range(2):
                        s4 = hh * 2 + j
                        attn_scores(j * 3, [(qT[:, s4, qs], d_qT)], [(kT[:, s4, :], d_kT)])
                    if pending is not None:
                        pending[0]()
                    for j in range(2):
                        softmax_exp(j * 3, 0.125, eb4[par][j], d_eb4[par][j], colsj[par][j], d_colsj[par][j], 0)
                    if pending is not None:
                        pending[1]()

                    def stage2a(par=par, hh=hh):
                        cols, d_cols = cols2[par], d_cols2[par]
                        eb, d_eb = eb4[par], d_eb4[par]
                        K.op(K.dve, lambda e: e.reciprocal(out=cols[:, 4:5], in_=colsj[par][0][:, 1:2]), reads=[d_colsj[par][0]], writes=[d_cols])
                        K.op(K.dve, lambda e: e.reciprocal(out=cols[:, 5:6], in_=colsj[par][1][:, 1:2]), reads=[d_colsj[par][1]], writes=[d_cols])
                        K.op(K.dve, lambda e: e.tensor_tensor(out=cols[:, 5:6], in0=cols[:, 5:6], in1=neg_lam, op=ALU.mult),
                             reads=[d_cols, d_l], writes=[d_cols])
                        K.op(K.dve, lambda e: e.tensor_scalar(out=wg, in0=eb[0], scalar1=cols[:, 4:5], scalar2=None, op0=ALU.mult),
                             reads=[d_eb[0], d_cols], writes=[d_wg])
                        K.op(K.dve, lambda e: e.scalar_tensor_tensor(out=wg, in0=eb[1], scalar=cols[:, 5:6], in1=wg,
                                                                     op0=ALU.mult, op1=ALU.add),
                             reads=[d_eb[1], d_cols], writes=[d_wg])
                        attn_pv(wg, d_wg, eT, d_eT, Vd, d_Vd, hh * 128, 256)

                    def stage2(par=par, hh=hh, h=h, qs=qs):
                        cols, d_cols = cols2[par], d_cols2[par]
                        K.op(K.act, lambda e: e.activation(out=junk, in_=bank(7, 128, 256), func=AF.Square, accum_out=cols[:, 6:7]),
                             reads=[dps[7]], writes=[d_junk, d_cols])
                        rstd_from_ss(cols[:, 6:7], 128.0, d_cols)
                        K.op(K.dve, lambda e: e.scalar_tensor_tensor(out=on, in0=bank(7, 128, 256), scalar=cols[:, 6:7], in1=gdb,
                                                                     op0=ALU.mult, op1=ALU.mult),
                             reads=[dps[7], d_cols, d_gdb], writes=[d_on])
                        K.op(K.pe, lambda e: e.transpose(out=bank(7, 128, 384), in_=on, identity=identF),
                             reads=[d_on, d_identF], writes=[dps[7]])
                        K.op(K.act, lambda e: e.copy(out=oT[:, 8 + h, qs], in_=bank(7, 128, 384)),
                             reads=[dps[7]], writes=[d_oT])
                    pending = (stage2a, stage2)
            pending[0]()
            pending[1]()
        assert mem.top <= PH_X, f"low region overflow {mem.top} > {PH_X}"
        K.barrier()
        mem.top = markA
        for t in range(NT):
            K.dma(K.q_sync, X[:, t, :], xsp[:, t, :], writes=[dX[t]])
        emit_out_proj(l, ab_w_out[i], oT, d_oT, ring)
        K.barrier()
        mem.top = PH_X

    def emit_gla(l, i, hT, d_hT, oT, d_oT, wv, ring):
        glam_sb = mem.alloc([128, 6, 128], F32); d_glam = Dep()
        for m in range(6):
            K.dma(K.q_sync, glam_sb[:, m, :], glam_d[m], writes=[d_glam])
        csel_sb = mem.alloc([128, 4], F32)
        K.dma(K.q_sync, csel_sb, csel_d, writes=[d_glam])
        keepc = mem.alloc([128, 1], F32)
        K.dma(K.q_sync, keepc, keep, writes=[d_glam])
        ggb = mem.alloc([128, 256], F32)
        K.dma(K.q_sync, ggb, gla_norm_g[i].partition_broadcast(128), writes=[d_glam])
        ggT = mem.alloc([17, 2, T], F32); d_ggT = Dep()
        K.op(K.dve, lambda e: e.memset(ggT, 1.0), writes=[d_ggT])
        wg2 = mem.alloc([17, 2, 512], F32); d_wg2 = Dep()
        for dr in range(2):
            K.dma(K.q_sync, wg2[0:16, dr, :], gla_w_gate2[i, dr], writes=[d_wg2])
            K.dma(K.q_sync, wg2[16:17, dr, :], gla_b_gate2[i, dr].rearrange("(o f) -> o f", o=1), writes=[d_wg2])
        (wgg,), d_wgg = ring.load([(wv[:, :, 3072:3104], KC, 32)])
        for dr in range(2):
            for hf in range(2):
                tok = slice(hf * 512, (hf + 1) * 512)
                mm_group(bank(0)[0:16, :], [(wgg[:, kc, dr * 16:(dr + 1) * 16], hT[:, kc, tok]) for kc in range(KC)],
                         reads=d_hT + [d_wgg], writes=[dps[0]])
                K.op(K.act, lambda e, dr=dr, tok=tok: e.copy(out=ggT[0:16, dr, tok], in_=bank(0)[0:16, :]),
                     reads=[dps[0]], writes=[d_ggT])
        q_f = mem.alloc([128, NT, 128], F32); k_f = mem.alloc([128, NT, 128], F32); v_b = mem.alloc([128, NT, 256], BF16)
        d_qkv = Dep()
        khat = mem.alloc([128, NT, 128], BF16); qtT = mem.alloc([128, T], BF16); ktT = mem.alloc([128, T], BF16)
        AT = mem.alloc([128, NT, 128], BF16); dcol = mem.alloc([128, 32], F32)
        d_khat, d_qtT, d_ktT, d_AT, d_dcol = (Dep() for _ in range(5))
        o_f = mem.alloc([128, NT, 256], BF16); d_of = Dep()
        S = mem.alloc([128, 256], F32); S_bf = mem.alloc([128, 256], BF16); d_S = Dep(); d_Sbf = Dep()
        qblk = mem.alloc([128, 4, 128], BF16); khm = mem.alloc([128, 4, 128], BF16); d_qblk = Dep(); d_khm = Dep()
        K.op(K.dve, lambda e: e.memset(qblk, 0.0), writes=[d_qblk])
        e1_2 = [mem.alloc([128, 128], F32) for _ in range(2)]; sp_2 = [mem.alloc([128, 128], F32) for _ in range(2)]
        ebt_2 = [mem.alloc([128, 3, 128], F32) for _ in range(2)]
        qt_2 = [mem.alloc([128, 128], F32) for _ in range(2)]; kt_2 = [mem.alloc([128, 128], F32) for _ in range(2)]
        d_e1_2, d_sp_2, d_ebt_2, d_qt_2, d_kt_2 = ([Dep(), Dep()] for _ in range(5))
        osum = mem.alloc([128, 256], F32); sgr = mem.alloc([128, 256], F32); on = mem.alloc([128, 256], F32)
        junk = mem.alloc([128, 256], BF16); cg = mem.alloc([128, 4], F32)
        d_osum, d_sgr, d_on, d_junk, d_cg = (Dep() for _ in range(5))
        og = o_gla[i]
        for h in range(4):
            (wq_,), d_wq = ring.load([(wv[:, :, h * 128:(h + 1) * 128], KC, 128)])
            (wk_,), d_wk = ring.load([(wv[:, :, 512 + h * 128:512 + (h + 1) * 128], KC, 128)])
            (wv_,), d_wv_ = ring.load([(wv[:, :, 1024 + h * 256:1024 + (h + 1) * 256], KC, 256)])
            for t in range(NT):
                tk = slice(t * 128, (t + 1) * 128)
                mm_group(bank(0, 128, 0), [(hT[:, kc, tk], wq_[:, kc, :]) for kc in range(KC)], reads=[d_hT[t], d_wq], writes=[dps[0]])
                mm_group(bank(0, 128, 128), [(hT[:, kc, tk], wk_[:, kc, :]) for kc in range(KC)], reads=[d_hT[t], d_wk], writes=[dps[0]])
                mm_group(bank(0, 256, 256), [(hT[:, kc, tk], wv_[:, kc, :]) for kc in range(KC)], reads=[d_hT[t], d_wv_], writes=[dps[0]])
                K.op(K.act, lambda e, t=t: e.mul(out=q_f[:, t, :], in_=bank(0, 128, 0), mul=128.0 ** -0.5), reads=[dps[0]], writes=[d_qkv])
                K.op(K.act, lambda e, t=t: e.copy(out=k_f[:, t, :], in_=bank(0, 128, 128)), reads=[dps[0]], writes=[d_qkv])
                K.op(K.act, lambda e, t=t: e.copy(out=v_b[:, t, :], in_=bank(0, 256, 256)), reads=[dps[0]], writes=[d_qkv])
            (wgr,), d_wgr = ring.load([(wv[:, :, 2048 + h * 256:2048 + (h + 1) * 256], KC, 256)])
            for dr in range(2):
                mi, ms = (0, 1) if dr == 0 else (2, 3)
                K.dma(K.q_sync, S, st_gla[i, dr, h], writes=[d_S])
                K.op(K.act, lambda e: e.copy(out=S_bf, in_=S), reads=[d_S], writes=[d_Sbf])
                for t in range(NT):
                    tk = slice(t * 128, (t + 1) * 128)
                    pp = t % 2
                    e1, sp, ebt, qt, kt = e1_2[pp], sp_2[pp], ebt_2[pp], qt_2[pp], kt_2[pp]
                    d_e1, d_sp, d_ebt, d_qt, d_kt = d_e1_2[pp], d_sp_2[pp], d_ebt_2[pp], d_qt_2[pp], d_kt_2[pp]
                    bL, bC, bT, bA = ((1, 2, 3, 4), (5, 6, 7, 0))[pp]
                    mm_group(bank(bL, 128), [(ggT[0:17, dr, tk], wg2[0:17, dr, h * 128:(h + 1) * 128])],
                             reads=[d_ggT, d_wg2], writes=[dps[bL]])
                    K.op(K.act, lambda e: e.activation(out=e1, in_=bank(bL, 128), func=AF.Exp, scale=-1.0), reads=[dps[bL]], writes=[d_e1])
                    K.op(K.act, lambda e: e.activation(out=sp, in_=e1, func=AF.Ln, bias=1.0), reads=[d_e1], writes=[d_sp])
                    mm_group(bank(bC, 128, 0), [(glam_sb[:, mi, :], sp)], reads=[d_glam, d_sp], writes=[dps[bC]])
                    mm_group(bank(bC, 128, 128), [(glam_sb[:, ms, :], sp)], reads=[d_glam, d_sp], writes=[dps[bC]])
                    mm_group(bank(bC, 4, 256), [(sp, csel_sb)], reads=[d_glam, d_sp], writes=[dps[bC]])
                    K.op(K.act, lambda e: e.activation(out=ebt[:, 0, :], in_=bank(bC, 128, 0), func=AF.Exp, scale=-1.0 / 16), reads=[dps[bC]], writes=[d_ebt])
                    K.op(K.act, lambda e: e.activation(out=ebt[:, 1, :], in_=bank(bC, 128, 0), func=AF.Exp, scale=1.0 / 16), reads=[dps[bC]], writes=[d_ebt])
                    K.op(K.act, lambda e: e.activation(out=ebt[:, 2, :], in_=bank(bC, 128, 128), func=AF.Exp, scale=-1.0 / 16), reads=[dps[bC]], writes=[d_ebt])
                    K.op(K.act, lambda e, t=t: e.activation(out=dcol[:, t * 4:(t + 1) * 4], in_=bank(bC, 4, 256), func=AF.Exp, scale=-1.0 / 16),
                         reads=[dps[bC]], writes=[d_dcol])
                    K.op(K.dve, lambda e, t=t: e.tensor_tensor(out=qt, in0=q_f[:, t, :], in1=ebt[:, 0, :], op=ALU.mult), reads=[d_qkv, d_ebt], writes=[d_qt])
                    K.op(K.dve, lambda e, t=t: e.tensor_tensor(out=kt, in0=k_f[:, t, :], in1=ebt[:, 1, :], op=ALU.mult), reads=[d_qkv, d_ebt], writes=[d_kt])
                    K.op(K.dve, lambda e, t=t: e.tensor_tensor(out=khat[:, t, :], in0=k_f[:, t, :], in1=ebt[:, 2, :], op=ALU.mult), reads=[d_qkv, d_ebt], writes=[d_khat])
                    def trqk(e):
                        e.transpose(out=bank(bT, 128, 0), in_=qt, identity=identF)
                        return e.transpose(out=bank(bT, 128, 128), in_=kt, identity=identF)
                    K.op(K.pe, trqk, reads=[d_qt, d_kt, d_identF], writes=[dps[bT]])
                    K.op(K.act, lambda e, tk=tk: e.copy(out=qtT[:, tk], in_=bank(bT, 128, 0)), reads=[dps[bT]], writes=[d_qtT])
                    K.op(K.act, lambda e, tk=tk: e.copy(out=ktT[:, tk], in_=bank(bT, 128, 128)), reads=[dps[bT]], writes=[d_ktT])
                    mm_group(bank(bA, 128), [(ktT[:, tk], qtT[:, tk])], reads=[d_qtT, d_ktT], writes=[dps[bA]])
                    K.op(K.dve, lambda e, t=t, dr=dr: e.tensor_tensor(out=AT[:, t, :], in0=bank(bA, 128), in1=glam_sb[:, 4 + dr, :], op=ALU.mult),
                         reads=[dps[bA], d_glam], writes=[d_AT])
                torder = list(range(NT)) if dr == 0 else list(range(NT - 1, -1, -1))
                corder = [0, 1, 2, 3] if dr == 0 else [3, 2, 1, 0]
                kv_i = 0
                for n_t, t in enumerate(torder):
                    tk = slice(t * 128, (t + 1) * 128)
                    if n_t > 0 and n_t % 2 == 0:
                        K.op(K.dve, lambda e: e.tensor_scalar(out=S, in0=S, scalar1=keepc[:, 0:1], scalar2=None, op0=ALU.mult),
                             reads=[d_glam], writes=[d_S])
                        K.op(K.act, lambda e: e.copy(out=S_bf, in_=S), reads=[d_S], writes=[d_Sbf])
                    for c in range(4):
                        K.op(K.act, lambda e, c=c, t=t: e.copy(out=qblk[:, c, c * 32:(c + 1) * 32],
                                                               in_=qtT[:, t * 128 + c * 32:t * 128 + (c + 1) * 32]),
                             reads=[d_qtT], writes=[d_qblk])
                        K.op(K.dve, lambda e, c=c, t=t: e.tensor_scalar(out=khm[:, c, :], in0=khat[:, t, :], scalar1=csel_sb[:, c:c + 1],
                                                                        scalar2=None, op0=ALU.mult),
                             reads=[d_khat, d_glam], writes=[d_khm])
                    K.op(K.pe, lambda e, t=t: e.matmul(bank(5, 256), lhsT=AT[:, t, :], rhs=v_b[:, t, :], start=True, stop=False),
                         reads=[d_AT, d_qkv], writes=[dps[5]])
                    for ci, c in enumerate(corder):
                        K.op(K.pe, lambda e, c=c, ci=ci: e.matmul(bank(5, 256), lhsT=qblk[:, c, :], rhs=S_bf, start=False, stop=(ci == 3)),
                             reads=[d_qblk, d_Sbf], writes=[dps[5]])
                        kvo = (kv_i % 2) * 256
                        kv_i += 1
                        mm_group(bank(6, 256, kvo), [(khm[:, c, :], v_b[:, t, :])], reads=[d_khm, d_qkv], writes=[dps[6]])
                        K.op(K.dve, lambda e, c=c, t=t, kvo=kvo: e.scalar_tensor_tensor(
                            out=S, in0=S, scalar=dcol[:, t * 4 + c:t * 4 + c + 1], in1=bank(6, 256, kvo), op0=ALU.mult, op1=ALU.add),
                            reads=[dps[6], d_dcol], writes=[d_S])
                        K.op(K.act, lambda e: e.copy(out=S_bf, in_=S), reads=[d_S], writes=[d_Sbf])
                    if n_t % 2 == 1:
                        K.dma(K.q_sync, og[t // 2, dr, h], S, reads=[d_S])
                    if dr == 0:
                        K.op(K.act, lambda e, t=t: e.copy(out=o_f[:, t, :], in_=bank(5, 256)), reads=[dps[5]], writes=[d_of])
                    else:
                        K.op(K.dve, lambda e, t=t: e.tensor_tensor(out=osum, in0=bank(5, 256), in1=o_f[:, t, :], op=ALU.add),
                             reads=[dps[5], d_of], writes=[d_osum])
                        K.op(K.act, lambda e: e.activation(out=junk, in_=osum, func=AF.Square, accum_out=cg[:, 0:1]),
                             reads=[d_osum], writes=[d_junk, d_cg])
                        rstd_from_ss(cg[:, 0:1], 256.0, d_cg)
                        mm_group(bank(1, 256, 128), [(hT[:, kc, tk], wgr[:, kc, :]) for kc in range(KC)], reads=[d_hT[t], d_wgr], writes=[dps[1]])
                        K.op(K.act, lambda e: e.activation(out=sgr, in_=bank(1, 256, 128), func=AF.Silu), reads=[dps[1]], writes=[d_sgr])
                        K.op(K.dve, lambda e: e.scalar_tensor_tensor(out=on, in0=osum, scalar=cg[:, 0:1], in1=ggb, op0=ALU.mult, op1=ALU.mult),
                             reads=[d_osum, d_cg, d_glam], writes=[d_on])
                        K.op(K.dve, lambda e: e.tensor_tensor(out=on, in0=on, in1=sgr, op=ALU.mult), reads=[d_sgr], writes=[d_on])
                        def tro(e):
                            e.transpose(out=bank(7, 128, 0), in_=on[:, 0:128], identity=identF)
                            return e.transpose(out=bank(7, 128, 128), in_=on[:, 128:256], identity=identF)
                        K.op(K.pe, tro, reads=[d_on, d_identF], writes=[dps[7]])
                        K.op(K.act, lambda e, h=h, tk=tk: e.copy(out=oT[:, h * 2:h * 2 + 2, tk],
                                                                 in_=bank(7, 256).rearrange("p (a b) -> p a b", a=2)),
                             reads=[dps[7]], writes=[d_oT])

    def emit_mixer_stub(l):
        mem.top = PH_X
        ring = Ring(K, mem, 4, 4096)
        emit_mod(l, 0, ring)
        chk("mod0")
        if cfg.get("dbg_noln"):
            gb, d_gb, scr, d_scr = None, Dep(), (None, None, None), Dep()
        else:
            gb, d_gb, scr, d_scr = load_ln(l, 0)
        for t in range(NT):
            if not cfg.get("dbg_nomul"):
                K.op(K.act, lambda e, t=t: e.mul(out=X[:, t, :], in_=X[:, t, :], mul=ALPHA), writes=[dX[t]])
            emit_ln_tile(t, gb, d_gb, scr, d_scr)
        K.barrier()
        mem.top = PH_X
        chk("ln0")

    class _Stop(Exception):
        pass
    stop = cfg.get("stop", "")
    def chk(tag):
        if stop == tag:
            K.barrier()
            raise _Stop()
    try:
        for l in range(depth):
            if do_mixer and l % 2 == 1:
                emit_mixer_mla(l)
            elif do_mixer:
                emit_mixer_ab(l)
            else:
                emit_mixer_stub(l)
            emit_moe(l)
    except _Stop:
        pass

    yv = y_out.rearrange("(t p) d -> p t d", p=128)
    for t in range(NT):
        K.dma(K.q_sync, yv[:, t, :], X[:, t, :], reads=[dX[t]])
    K.finish()
    print("SBUF peak bytes", mem.peak, "sem counts", [(e.name, e.cnt) for e in K.engs])
    return nc


def _gla_consts():
    idx = np.arange(128)
    same = (idx[:, None] // 32) == (idx[None, :] // 32)
    s, t = idx[:, None], idx[None, :]
    m = np.zeros((6, 128, 128), np.float32)
    m[0] = same & (s <= t)
    m[1] = same & (s > t)
    m[2] = same & (s >= t)
    m[3] = same & (s < t)
    m[4] = same & (s <= t)
    m[5] = same & (s >= t)
    csel = np.zeros((128, 4), np.float32)
    csel[idx, idx // 32] = 1.0
    return m, csel


def _rope_tables():
    n_rows = T // 64
    rows = np.repeat(np.arange(n_rows, dtype=np.float32), 64)
    cols = np.tile(np.arange(64, dtype=np.float32), n_rows)
    quarter = 16
    freqs = (10000.0 ** (-np.arange(quarter, dtype=np.float32) / quarter)).astype(np.float32)
    ang = np.concatenate([rows[:, None] * freqs, cols[:, None] * freqs], axis=-1).astype(np.float32)
    return np.cos(ang).astype(np.float32), np.sin(ang).astype(np.float32)


_CACHE = {}


def kernel(**inputs):
    cfg = inputs.pop("_cfg", {})
    inp = {k: np.ascontiguousarray(np.asarray(v)) for k, v in inputs.items()}
    key = tuple(sorted((k, str(v)) for k, v in cfg.items()))
    if key not in _CACHE:
        _CACHE[key] = build(cfg)
    nc = _CACHE[key]
    glam, csel = _gla_consts()
    cos, sin = _rope_tables()
    wnames = ["ada_w", "ada_b", "ln_g", "ln_b", "ab_w_in", "gla_w_gate2", "gla_b_gate2", "gla_norm_g",
              "diff_lambda", "diff_norm_g", "ab_w_out", "mla_w_in", "mla_q_norm_g", "mla_w_uq",
              "mla_kv_norm_g", "mla_w_ukv", "mla_w_out", "moe_w_rg", "moe_b_rg", "moe_w_re", "moe_b_re",
              "moe_w_gate", "moe_w_up", "moe_w_down"]
    in_maps = []
    depth = cfg.get("depth", DEPTH)
    n_exp = cfg.get("n_exp", N_EXP)
    cores = cfg.get("cores", list(range(8)))
    dl, nab, ncl, ne = max(depth, 1), max((depth + 1) // 2, 1), max(depth // 2, 1), max(n_exp, 1)
    wsl = {}
    for n in wnames:
        a = inp[n]
        if n in ("ada_w", "ada_b", "ln_g", "ln_b", "moe_w_rg", "moe_b_rg", "moe_w_re", "moe_b_re"):
            a = a[:dl]
        elif n in ("moe_w_gate", "moe_w_up", "moe_w_down"):
            a = a[:dl, :ne]
        elif n.startswith("mla_"):
            a = a[:ncl]
        else:
            a = a[:nab]
        wsl[n] = np.ascontiguousarray(a)
    for c in cores:
        m = dict(wsl)
        m["identF"] = np.eye(128, dtype=np.float32)
        m["glam"] = glam
        m["csel"] = csel
        if c < 4:
            m["x_in"] = inp["x_prompt"][4 * c:4 * c + 4].reshape(T, D)
            m["cond"] = inp["c_ctx"]
            m["st_gla"] = np.zeros((2, 2, 4, 128, 256), np.float32)
            m["c_dk"] = np.zeros((2, PAST, 1024), np.float32)
            m["c_dv"] = np.zeros((2, PAST, 1024), np.float32)
            m["c_ckv"] = np.zeros((2, PAST, 256), np.float32)
            m["c_kr"] = np.zeros((2, PAST, 64), np.float32)
            m["rope_cos"] = np.ones((T, 32), np.float32)
            m["rope_sin"] = np.zeros((T, 32), np.float32)
            m["ropeT_c"] = np.ones((64, T), np.float32)
            m["ropeT_s"] = np.zeros((64, T), np.float32)
            ss = np.zeros((4, T), np.float32)
            km = np.full((4, NKEY), NEG, np.float32)
            for s in range(4):
                ss[s, 256 * s:256 * (s + 1)] = 1.0
                km[s, PAST + 256 * s:PAST + 256 * (s + 1)] = 0.0
            m["seqsel"] = ss
            m["kmask"] = km
            m["keep"] = np.zeros((128, 1), np.float32)
        else:
            b = c - 4
            m["x_in"] = inp["x_sample"][b]
            m["cond"] = inp["c"][b]
            m["st_gla"] = inp["state_gla"][b]
            m["c_dk"] = inp["cache_diff_k"][b].reshape(2, PAST, 1024)
            m["c_dv"] = inp["cache_diff_v"][b].reshape(2, PAST, 1024)
            m["c_ckv"] = inp["cache_mla_ckv"][b]
            m["c_kr"] = inp["cache_mla_krope"][b]
            m["rope_cos"] = cos
            m["rope_sin"] = sin
            m["ropeT_c"] = np.concatenate([cos.T, cos.T], axis=0)
            m["ropeT_s"] = np.concatenate([-sin.T, sin.T], axis=0)
            ss = np.zeros((4, T), np.float32)
            ss[0] = 1.0
            m["seqsel"] = ss
            m["kmask"] = np.zeros((4, NKEY), np.float32)
            m["keep"] = np.ones((128, 1), np.float32)
        in_maps.append({k: np.ascontiguousarray(v, dtype=np.float32) for k, v in m.items()})
    res = run_bass_kernel_spmd(nc, in_maps, core_ids=list(range(len(cores))))
    if len(cores) != 8:
        return {c: res.results[i] for i, c in enumerate(cores)}
    r = res.results
    y_prompt = np.stack([r[c]["y"].reshape(4, 256, D) for c in range(4)]).reshape(16, 256, D)
    y_sample = np.stack([r[c]["y"] for c in range(4, 8)])
    new_gla = np.concatenate([np.transpose(r[c]["o_gla"], (1, 0, 2, 3, 4, 5)) for c in range(4)], axis=0)
    def tok(name, shp):
        a = np.stack([r[c][name] for c in range(4)])
        a = a.reshape(4, 2, 4, 256, -1).transpose(0, 2, 1, 3, 4).reshape(16, 2, 256, -1)
        return a.reshape((16, 2, 256) + shp)
    new_dk = tok("o_dk", (8, 2, 64))
    new_dv = tok("o_dv", (8, 128))
    new_ckv = tok("o_ckv", (256,))
    new_kr = tok("o_kr", (64,))
    return (y_prompt.astype(np.float32), y_sample.astype(np.float32), new_gla.astype(np.float32),
            new_dk.astype(np.float32), new_dv.astype(np.float32), new_ckv.astype(np.float32),
            new_kr.astype(np.float32))
```

```python
import contextlib
import math
import numpy as np
import concourse.bass as bass
import concourse.mybir as mybir
from concourse.bass_utils import run_bass_kernel_spmd

F32 = mybir.dt.float32
BF16 = mybir.dt.bfloat16
AF = mybir.ActivationFunctionType
ALU = mybir.AluOpType
AX = mybir.AxisListType

D = 2048
KC = 16
T = 1024
NT = 8
DEPTH = 4
PAST = 256
NKEY = PAST + T
NKT = NKEY // 128
N_EXP = 16
D_EXP = 512
AB_IN = 6176
C_IN = 832
ALPHA = (2.0 * DEPTH) ** 0.25
LN_EPS = 1e-5
RMS_EPS = 1e-6
NEG = -30000.0


class Dep:
    __slots__ = ("w", "r", "excl")

    def __init__(self, excl=False):
        self.w = None
        self.r = {}
        self.excl = excl


class Eng:
    def __init__(self, K, e, name, own_wait=True):
        self.e = e
        self.name = name
        self.sem = K.es.enter_context(K.nc.semaphore("s_" + name))
        self.cnt = 0
        self.waited = {}
        self.own_wait = own_wait

    def wait(self, ev):
        if ev is None:
            return
        sem, val = ev
        if sem is self.sem and not self.own_wait:
            return
        key = id(sem)
        if self.waited.get(key, 0) >= val:
            return
        self.e.wait_ge(sem, val)
        self.waited[key] = val


class DmaQ:
    def __init__(self, K, eng, nsem, name):
        self.eng = eng
        self.sems = [K.es.enter_context(K.nc.semaphore(f"d_{name}{i}")) for i in range(nsem)]
        self.vals = [0] * nsem
        self.i = 0

    def next_sem(self):
        i = self.i
        self.i = (self.i + 1) % len(self.sems)
        if self.vals[i] > 0:
            self.eng.wait((self.sems[i], self.vals[i]))
        return i


class Kern:
    def __init__(self, nc):
        self.nc = nc
        self.es = contextlib.ExitStack()
        self.pe = Eng(self, nc.tensor, "pe", own_wait=False)
        self.act = Eng(self, nc.scalar, "act")
        self.dve = Eng(self, nc.vector, "dve")
        self.pool = Eng(self, nc.gpsimd, "pool")
        self.sp = Eng(self, nc.sync, "sp")
        self.engs = [self.pe, self.act, self.dve, self.pool, self.sp]
        self.q_sync = DmaQ(self, self.sp, 28, "sy")
        self.q_pool = DmaQ(self, self.pool, 28, "po")

    def _pre(self, eng, reads, writes):
        for d in reads:
            eng.wait(d.w)
            if d.excl:
                for ev in list(d.r.values()):
                    if ev[0] is not eng.sem:
                        eng.wait(ev)
        for d in writes:
            eng.wait(d.w)
            for ev in list(d.r.values()):
                eng.wait(ev)

    def _post(self, ev, reads, writes):
        sem, v = ev
        for d in reads:
            d.r[id(sem)] = ev
        for d in writes:
            d.w = ev
            d.r = {}

    def op(self, eng, fn, reads=(), writes=()):
        self._pre(eng, reads, writes)
        ins = fn(eng.e)
        eng.cnt += 1
        ins.then_inc(eng.sem, 1)
        ev = (eng.sem, eng.cnt)
        self._post(ev, reads, writes)
        return ev

    def dma(self, q, out, in_, reads=(), writes=()):
        eng = q.eng
        self._pre(eng, reads, writes)
        i = q.next_sem()
        ins = eng.e.dma_start(out=out, in_=in_)
        ins.then_inc(q.sems[i], 16)
        q.vals[i] += 16
        ev = (q.sems[i], q.vals[i])
        self._post(ev, reads, writes)
        return ev

    def all_events(self):
        evs = [(e.sem, e.cnt) for e in self.engs if e.cnt > 0]
        for q in (self.q_sync, self.q_pool):
            for s, v in zip(q.sems, q.vals):
                if v > 0:
                    evs.append((s, v))
        return evs

    def barrier(self):
        evs = self.all_events()
        for e in self.engs:
            for ev in evs:
                e.wait(ev)

    def finish(self):
        for ev in self.all_events():
            self.sp.wait(ev)


class Mem:
    def __init__(self, big, nbytes):
        self.big = big
        self.n = nbytes
        self.top = 0
        self.peak = 0

    def alloc(self, shape, dtype=F32):
        esz = 4 if dtype == F32 else 2
        nfree = 1
        for s in shape[1:]:
            nfree *= s
        nb = nfree * esz
        off = (self.top + 63) // 64 * 64
        self.top = off + nb
        self.peak = max(self.peak, self.top)
        assert self.top <= self.n, f"SBUF overflow {self.top} > {self.n}"
        ap = self.big[:, off // 2:(off + nb) // 2]
        if dtype == F32:
            ap = ap.bitcast(F32)
        if len(shape) > 2:
            names = [f"d{i}" for i in range(len(shape) - 1)]
            kw = {n: s for n, s in zip(names[:-1], shape[1:-1])}
            ap = ap.rearrange(f"p ({' '.join(names)}) -> p {' '.join(names)}", **kw)
        if shape[0] < 128:
            ap = ap[0:shape[0]]
        return ap


class Ring:
    def __init__(self, K, mem, nslot, slot_elems):
        self.K = K
        self.slots = [mem.alloc([128, slot_elems], BF16) for _ in range(nslot)]
        self.deps = [Dep() for _ in range(nslot)]
        self.i = 0

    def load(self, srcs):
        i = self.i
        self.i = (self.i + 1) % len(self.slots)
        views = []
        off = 0
        for src, a, b in srcs:
            v = self.slots[i][:, off:off + a * b].rearrange("p (a b) -> p a b", a=a)
            self.K.dma(self.K.q_pool, v, src, writes=[self.deps[i]])
            views.append(v)
            off += a * b
        return views, self.deps[i]


def build(cfg):
    depth = cfg.get("depth", DEPTH)
    n_exp = cfg.get("n_exp", N_EXP)
    do_mixer = cfg.get("mixer", True)
    nc = bass.Bass("TRN2", target_bir_lowering=False)
    K = Kern(nc)
    es = K.es

    def din(name, shape):
        return nc.dram_tensor(name, list(shape), F32, kind="ExternalInput").ap()

    def dout(name, shape):
        return nc.dram_tensor(name, list(shape), F32, kind="ExternalOutput").ap()

    x_in = din("x_in", [T, D])
    cond = din("cond", [D])
    st_gla = din("st_gla", [2, 2, 4, 128, 256])
    c_dk = din("c_dk", [2, PAST, 1024])
    c_dv = din("c_dv", [2, PAST, 1024])
    c_ckv = din("c_ckv", [2, PAST, 256])
    c_kr = din("c_kr", [2, PAST, 64])
    rope_cos = din("rope_cos", [T, 32])
    rope_sin = din("rope_sin", [T, 32])
    ropeT_c = din("ropeT_c", [64, T])
    ropeT_s = din("ropeT_s", [64, T])
    seqsel = din("seqsel", [4, T])
    kmask = din("kmask", [4, NKEY])
    keep = din("keep", [128, 1])
    identF_d = din("identF", [128, 128])
    glam_d = din("glam", [6, 128, 128])
    csel_d = din("csel", [128, 4])
    dl = max(depth, 1)
    nab = max((depth + 1) // 2, 1)
    ncl = max(depth // 2, 1)
    ada_w = din("ada_w", [dl, D, 6 * D])
    ada_b = din("ada_b", [dl, 6 * D])
    ln_g = din("ln_g", [dl, 2, D])
    ln_b = din("ln_b", [dl, 2, D])
    ab_w_in = din("ab_w_in", [nab, D, AB_IN])
    gla_w_gate2 = din("gla_w_gate2", [nab, 2, 16, 512])
    gla_b_gate2 = din("gla_b_gate2", [nab, 2, 512])
    gla_norm_g = din("gla_norm_g", [nab, 256])
    diff_lambda = din("diff_lambda", [nab, 4, 64])
    diff_norm_g = din("diff_norm_g", [nab, 128])
    ab_w_out = din("ab_w_out", [nab, D, D])
    mla_w_in = din("mla_w_in", [ncl, D, C_IN])
    mla_q_norm_g = din("mla_q_norm_g", [ncl, 512])
    mla_w_uq = din("mla_w_uq", [ncl, 512, 3072])
    mla_kv_norm_g = din("mla_kv_norm_g", [ncl, 256])
    mla_w_ukv = din("mla_w_ukv", [ncl, 256, 4096])
    mla_w_out = din("mla_w_out", [ncl, D, D])
    moe_w_rg = din("moe_w_rg", [dl, D, 4])
    moe_b_rg = din("moe_b_rg", [dl, 4])
    moe_w_re = din("moe_w_re", [dl, D, 16])
    moe_b_re = din("moe_b_re", [dl, 16])
    ne = max(n_exp, 1)
    moe_w_gate = din("moe_w_gate", [dl, ne, D, D_EXP])
    moe_w_up = din("moe_w_up", [dl, ne, D, D_EXP])
    moe_w_down = din("moe_w_down", [dl, ne, D_EXP, D])

    y_out = dout("y", [T, D])
    o_gla = dout("o_gla", [2, 4, 2, 4, 128, 256])
    o_dk = dout("o_dk", [2, T, 1024])
    o_dv = dout("o_dv", [2, T, 1024])
    o_ckv = dout("o_ckv", [2, T, 256])
    o_kr = dout("o_kr", [2, T, 64])
    x_spill = nc.dram_tensor("x_spill", [T, D], F32, kind="Internal").ap()

    SB_BYTES = 207 * 1024
    big = es.enter_context(nc.sbuf_tensor("big", [128, SB_BYTES // 2], BF16))
    mem = Mem(big, SB_BYTES)
    ps = es.enter_context(nc.psum_tensor("ps", [128, 4096], F32))
    dps = [Dep(excl=True) for _ in range(8)]

    def bank(i, n=512, off=0):
        return ps[:, i * 512 + off:i * 512 + off + n]

    def mm_group(out_ap, pairs, reads, writes):
        def fn(e):
            n = len(pairs)
            ins = None
            for i, (l, r) in enumerate(pairs):
                ins = e.matmul(out_ap, lhsT=l, rhs=r, start=(i == 0), stop=(i == n - 1))
            return ins
        return K.op(K.pe, fn, reads, writes)

    identF = mem.alloc([128, 128], F32)
    identB = mem.alloc([128, 128], BF16)
    S_rep = mem.alloc([128, KC, 128], BF16)
    modcol = mem.alloc([128, 4, KC], F32)
    adab_col = mem.alloc([128, 96], F32)
    gate_bc = mem.alloc([128, D], F32)
    smallc = mem.alloc([128, 64], F32)
    d_identF, d_identB, d_Srep, d_modcol, d_adab, d_gate, d_small = (Dep() for _ in range(7))

    X_OFF = (mem.top + 63) // 64 * 64
    X = mem.alloc([128, NT, D], F32)
    dX = [Dep() for _ in range(NT)]
    PH_X = mem.top
    PH_NOX = X_OFF

    K.dma(K.q_sync, identF, identF_d, writes=[d_identF])
    K.op(K.dve, lambda e: e.tensor_copy(out=identB, in_=identF), reads=[d_identF], writes=[d_identB])
    xv = x_in.rearrange("(t p) d -> p t d", p=128)
    for t in range(NT):
        K.dma(K.q_sync, X[:, t, :], xv[:, t, :], writes=[dX[t]])

    mem.top = PH_X
    c16 = mem.alloc([16, 128], F32)
    scol = mem.alloc([128, KC], F32)
    d_c16, d_scol = Dep(), Dep()
    K.dma(K.q_sync, c16, cond.rearrange("(c p) -> c p", p=128), writes=[d_c16])
    K.op(K.pe, lambda e: e.transpose(out=bank(0, 16), in_=c16, identity=identF[0:16, 0:16]),
         reads=[d_c16, d_identF], writes=[dps[0]])
    K.op(K.act, lambda e: e.activation(out=scol, in_=bank(0, 16), func=AF.Silu), reads=[dps[0]], writes=[d_scol])
    K.op(K.dve, lambda e: e.tensor_copy(out=S_rep, in_=scol.unsqueeze(2).to_broadcast([128, KC, 128])),
         reads=[d_scol], writes=[d_Srep])
    K.barrier()
    mem.top = PH_X

    def emit_mod(l, half, ring, segs=(0, 1, 2)):
        adaw_v = ada_w[l].rearrange("(c p) n -> p c n", p=128)
        if half == 0 and 0 in segs:
            ab96 = mem.alloc([96, 128], F32)
            d96 = Dep()
            K.dma(K.q_sync, ab96, ada_b[l].rearrange("(c p) -> c p", p=128), writes=[d96])
            K.op(K.pe, lambda e: e.transpose(out=bank(0, 96), in_=ab96, identity=identF[0:96, 0:96]),
                 reads=[d96, d_identF], writes=[dps[0]])
            K.op(K.act, lambda e: e.copy(out=adab_col, in_=bank(0, 96)), reads=[dps[0]], writes=[d_adab])
        tmp = mem.alloc([128, 256], F32)
        d_tmp = Dep()
        for si in segs:
            seg = half * 3 + si
            if si == 2:
                K.dma(K.q_sync, gate_bc, ada_b[l, seg * D:(seg + 1) * D].partition_broadcast(128),
                      writes=[d_gate])
            for blk in range(8):
                c0 = seg * D + blk * 256
                (w,), dw = ring.load([(adaw_v[:, :, c0:c0 + 256], KC, 256)])
                pb = (blk + si * 8) % 2
                po = bank(pb, 256)
                mm_group(po, [(S_rep[:, kc, :], w[:, kc, :]) for kc in range(KC)],
                         reads=[d_Srep, dw], writes=[dps[pb]])
                if si < 2:
                    mc = half * 2 + si
                    K.op(K.dve, lambda e: e.tensor_tensor(
                        out=tmp.rearrange("p (c j) -> p c j", c=2), in0=po.rearrange("p (c j) -> p c j", c=2),
                        in1=identF.unsqueeze(1).to_broadcast([128, 2, 128]), op=ALU.mult),
                        reads=[dps[pb], d_identF], writes=[d_tmp])
                    K.op(K.dve, lambda e: e.tensor_reduce(
                        out=modcol[:, mc, blk * 2:blk * 2 + 2], in_=tmp.rearrange("p (c j) -> p c j", c=2),
                        axis=AX.X, op=ALU.add), reads=[d_tmp], writes=[d_modcol])
                else:
                    K.op(K.dve, lambda e: e.tensor_tensor(
                        out=gate_bc[:, blk * 256:(blk + 1) * 256], in0=gate_bc[:, blk * 256:(blk + 1) * 256],
                        in1=po, op=ALU.add), reads=[dps[pb], d_gate], writes=[d_gate])
            if si < 2:
                mc = half * 2 + si
                K.op(K.dve, lambda e: e.tensor_tensor(
                    out=modcol[:, mc, :], in0=modcol[:, mc, :], in1=adab_col[:, seg * KC:(seg + 1) * KC],
                    op=ALU.add), reads=[d_modcol, d_adab], writes=[d_modcol])
                if si == 1:
                    K.op(K.dve, lambda e: e.tensor_scalar(
                        out=modcol[:, mc, :], in0=modcol[:, mc, :], scalar1=1.0, scalar2=None, op0=ALU.add),
                        reads=[d_modcol], writes=[d_modcol])

    def emit_convert(half, hT, d_hT, router=None):
        sh, sc = half * 2, half * 2 + 1
        for t in range(NT):
            for g in range(4):
                pb = 2 + (t * 4 + g) % 4
                def tr(e, t=t, g=g, pb=pb):
                    ins = None
                    for j in range(4):
                        kc = g * 4 + j
                        ins = e.transpose(out=bank(pb, 128, j * 128), in_=X[:, t, kc * 128:(kc + 1) * 128],
                                          identity=identF)
                    return ins
                K.op(K.pe, tr, reads=[dX[t], d_identF], writes=[dps[pb]])
                for j in range(4):
                    kc = g * 4 + j
                    K.op(K.act, lambda e, kc=kc, j=j, pb=pb, t=t: e.activation(
                        out=hT[:, kc, t * 128:(t + 1) * 128], in_=bank(pb, 128, j * 128), func=AF.Identity,
                        scale=modcol[:, sc, kc:kc + 1], bias=modcol[:, sh, kc:kc + 1]),
                        reads=[dps[pb], d_modcol], writes=[d_hT[t]])
                    if router is not None:
                        hF, d_hF = router["hF"], router["d_hF"]
                        K.op(K.dve, lambda e, kc=kc, j=j, pb=pb: e.tensor_scalar(
                            out=hF[:, kc, :], in0=bank(pb, 128, j * 128), scalar1=modcol[:, sc, kc:kc + 1],
                            scalar2=modcol[:, sh, kc:kc + 1], op0=ALU.mult, op1=ALU.add),
                            reads=[dps[pb], d_modcol], writes=[d_hF])
            if router is not None:
                emit_route(t, router)

    def emit_route(t, R):
        hF, d_hF, wr, d_wr, comb, d_comb, rb, d_rb, rt, d_rt = (R[k] for k in (
            "hF", "d_hF", "wr", "d_wr", "comb", "d_comb", "rb", "d_rb", "rt", "d_rt"))
        mm_group(bank(1, 20), [(hF[:, kc, :], wr[:, kc, :]) for kc in range(KC)],
                 reads=[d_hF, d_wr], writes=[dps[1]])
        V = K.dve
        lg = rt[:, 0:20]
        def dv(fn, reads=(), writes=()):
            return K.op(V, fn, reads=list(reads) + [d_rt], writes=list(writes) + [d_rt])
        dv(lambda e: e.tensor_tensor(out=lg, in0=bank(1, 20), in1=rb, op=ALU.add), reads=[dps[1], d_rb])
        gl = rt[:, 0:4]
        el = rt[:, 4:20].rearrange("p (g j) -> p g j", g=4)
        gmax, gsum, m1, m2, e2, den, w1, w2 = (rt[:, 20 + i:21 + i] for i in range(8))
        ohg = rt[:, 32:36]
        tmp16 = rt[:, 36:52].rearrange("p (g j) -> p g j", g=4)
        esel = rt[:, 52:56]
        oh1 = rt[:, 56:60]
        msk = rt[:, 60:64]
        oh2 = rt[:, 64:68]
        cig = rt[:, 68:72]
        gex = rt[:, 72:76]
        dv(lambda e: e.reduce_max(out=gmax, in_=gl, axis=AX.X))
        dv(lambda e: e.tensor_scalar(out=ohg, in0=gl, scalar1=gmax, scalar2=None, op0=ALU.is_equal))
        dv(lambda e: e.tensor_scalar(out=gex, in0=gl, scalar1=gmax, scalar2=None, op0=ALU.subtract))
        K.op(K.act, lambda e: e.activation(out=gex, in_=gex, func=AF.Exp, accum_out=gsum),
             reads=[d_rt], writes=[d_rt])
        dv(lambda e: e.tensor_tensor(out=tmp16, in0=el, in1=ohg.unsqueeze(2).to_broadcast([128, 4, 4]),
                                     op=ALU.mult))
        dv(lambda e: e.tensor_reduce(out=esel, in_=tmp16.rearrange("p g j -> p j g"), axis=AX.X, op=ALU.add))
        dv(lambda e: e.reduce_max(out=m1, in_=esel, axis=AX.X))
        dv(lambda e: e.tensor_scalar(out=oh1, in0=esel, scalar1=m1, scalar2=None, op0=ALU.is_equal))
        dv(lambda e: e.scalar_tensor_tensor(out=msk, in0=oh1, scalar=-1e30, in1=esel, op0=ALU.mult, op1=ALU.add))
        dv(lambda e: e.reduce_max(out=m2, in_=msk, axis=AX.X))
        dv(lambda e: e.tensor_scalar(out=oh2, in0=msk, scalar1=m2, scalar2=None, op0=ALU.is_equal))
        dv(lambda e: e.tensor_tensor(out=e2, in0=m2, in1=m1, op=ALU.subtract))
        K.op(K.act, lambda e: e.activation(out=e2, in_=e2, func=AF.Exp), reads=[d_rt], writes=[d_rt])
        dv(lambda e: e.scalar_tensor_tensor(out=den, in0=e2, scalar=1.0, in1=gsum, op0=ALU.add, op1=ALU.mult))
        dv(lambda e: e.reciprocal(out=w1, in_=den))
        dv(lambda e: e.tensor_tensor(out=w2, in0=e2, in1=w1, op=ALU.mult))
        dv(lambda e: e.tensor_scalar(out=cig, in0=oh1, scalar1=w1, scalar2=None, op0=ALU.mult))
        dv(lambda e: e.scalar_tensor_tensor(out=cig, in0=oh2, scalar=w2, in1=cig, op0=ALU.mult, op1=ALU.add))
        K.op(V, lambda e: e.tensor_tensor(
            out=comb[:, t, :].rearrange("p (g j) -> p g j", g=4),
            in0=ohg.unsqueeze(2).to_broadcast([128, 4, 4]), in1=cig.unsqueeze(1).to_broadcast([128, 4, 4]),
            op=ALU.mult), reads=[d_rt], writes=[d_comb[t]])

    def emit_ln_tile(t, gb, d_gb, scr, d_scr):
        st, mv, cols = scr
        lnstop = cfg.get("lnstop", 99)
        if lnstop < 1:
            return
        for c in range(4):
            K.op(K.dve, lambda e, c=c: e.bn_stats(out=st[:, c, :], in_=X[:, t, c * 512:(c + 1) * 512]),
                 reads=[dX[t]], writes=[d_scr])
        K.op(K.dve, lambda e: e.bn_aggr(out=mv, in_=st), reads=[d_scr], writes=[d_scr])
        if lnstop < 2:
            return
        K.op(K.dve, lambda e: e.tensor_scalar(out=cols[:, 0:1], in0=mv[:, 1:2], scalar1=LN_EPS, scalar2=None,
                                              op0=ALU.add), reads=[d_scr], writes=[d_scr])
        K.op(K.act, lambda e: e.activation(out=cols[:, 0:1], in_=cols[:, 0:1], func=AF.Ln),
             reads=[d_scr], writes=[d_scr])
        K.op(K.act, lambda e: e.activation(out=cols[:, 0:1], in_=cols[:, 0:1], func=AF.Exp, scale=-0.5),
             reads=[d_scr], writes=[d_scr])
        K.op(K.dve, lambda e: e.scalar_tensor_tensor(out=cols[:, 1:2], in0=mv[:, 0:1], scalar=-1.0,
                                                     in1=cols[:, 0:1], op0=ALU.mult, op1=ALU.mult),
             reads=[d_scr], writes=[d_scr])
        if lnstop < 3:
            return
        K.op(K.act, lambda e: e.activation(out=X[:, t, :], in_=X[:, t, :], func=AF.Identity,
                                           scale=cols[:, 0:1], bias=cols[:, 1:2]),
             reads=[d_scr], writes=[dX[t]])
        if lnstop < 4:
            return
        K.op(K.dve, lambda e: e.tensor_tensor(out=X[:, t, :], in0=X[:, t, :], in1=gb[:, 0, :], op=ALU.mult),
             reads=[d_gb], writes=[dX[t]])
        K.op(K.dve, lambda e: e.tensor_tensor(out=X[:, t, :], in0=X[:, t, :], in1=gb[:, 1, :], op=ALU.add),
             reads=[d_gb], writes=[dX[t]])

    def load_ln(l, which):
        gb = mem.alloc([128, 2, D], F32)
        d_gb = Dep()
        K.dma(K.q_sync, gb[:, 0, :], ln_g[l, which].partition_broadcast(128), writes=[d_gb])
        K.dma(K.q_sync, gb[:, 1, :], ln_b[l, which].partition_broadcast(128), writes=[d_gb])
        st = mem.alloc([128, 4, 6], F32)
        mv = mem.alloc([128, 2], F32)
        cols = mem.alloc([128, 2], F32)
        return gb, d_gb, (st, mv, cols), Dep()

    def emit_moe(l):
        mem.top = PH_X
        ring = Ring(K, mem, 6, 4096)
        emit_mod(l, 1, ring, segs=(0, 1))
        chk("mod1")
        hT = mem.alloc([128, KC, T], BF16)
        d_hT = [Dep() for _ in range(NT)]
        R = {}
        R["hF"] = mem.alloc([128, KC, 128], F32); R["d_hF"] = Dep()
        R["wr"] = mem.alloc([128, KC, 20], F32); R["d_wr"] = Dep()
        R["comb"] = mem.alloc([128, NT, 16], F32); R["d_comb"] = [Dep() for _ in range(NT)]
        R["rb"] = mem.alloc([128, 20], F32); R["d_rb"] = Dep()
        R["rt"] = mem.alloc([128, 80], F32); R["d_rt"] = Dep()
        K.dma(K.q_sync, R["wr"][:, :, 0:4], moe_w_rg[l].rearrange("(c p) n -> p c n", p=128), writes=[R["d_wr"]])
        K.dma(K.q_sync, R["wr"][:, :, 4:20], moe_w_re[l].rearrange("(c p) n -> p c n", p=128), writes=[R["d_wr"]])
        K.dma(K.q_sync, R["rb"][:, 0:4], moe_b_rg[l].partition_broadcast(128), writes=[R["d_rb"]])
        K.dma(K.q_sync, R["rb"][:, 4:20], moe_b_re[l].partition_broadcast(128), writes=[R["d_rb"]])
        emit_convert(1, hT, d_hT, router=R)
        emit_mod(l, 1, ring, segs=(2,))
        comb, d_comb = R["comb"], R["d_comb"]
        chk("conv1")

        actT = mem.alloc([128, 4, T], BF16)
        d_act = [[Dep() for _ in range(2)] for _ in range(4)]
        sg = [mem.alloc([128, 512], BF16) for _ in range(2)]
        d_sg = [Dep(), Dep()]
        acc = [mem.alloc([128, 512], F32) for _ in range(4)]
        d_acc = [Dep() for _ in range(4)]
        dbanks = [0, 1, 6, 7]
        gb, d_gb, scr, d_scr = load_ln(l, 1)
        ui = 0
        di = 0
        for ei in range(n_exp):
            wg = moe_w_gate[l, ei].rearrange("(c p) f -> p c f", p=128)
            wu = moe_w_up[l, ei].rearrange("(c p) f -> p c f", p=128)
            wd = moe_w_down[l, ei].rearrange("(c p) d -> p c d", p=128)
            dsl = []
            for fp in range(2):
                (g_w,), d_g = ring.load([(wg[:, :, fp * 256:(fp + 1) * 256], KC, 256)])
                (u_w,), d_u = ring.load([(wu[:, :, fp * 256:(fp + 1) * 256], KC, 256)])
                for fi in range(2):
                    f = fp * 2 + fi
                    for hf in range(2):
                        pg, pu = 2 + (ui % 2) * 2, 3 + (ui % 2) * 2
                        ui += 1
                        tok = slice(hf * 512, (hf + 1) * 512)
                        rd = [d_hT[t] for t in range(hf * 4, hf * 4 + 4)]
                        mm_group(bank(pg), [(g_w[:, kc, fi * 128:(fi + 1) * 128], hT[:, kc, tok]) for kc in range(KC)],
                                 reads=rd + [d_g], writes=[dps[pg]])
                        mm_group(bank(pu), [(u_w[:, kc, fi * 128:(fi + 1) * 128], hT[:, kc, tok]) for kc in range(KC)],
                                 reads=rd + [d_u], writes=[dps[pu]])
                        s = ui % 2
                        K.op(K.act, lambda e, s=s, pg=pg: e.activation(out=sg[s], in_=bank(pg), func=AF.Silu),
                             reads=[dps[pg]], writes=[d_sg[s]])
                        K.op(K.dve, lambda e, s=s, pu=pu, f=f, tok=tok: e.tensor_tensor(
                            out=actT[:, f, tok], in0=sg[s], in1=bank(pu), op=ALU.mult),
                            reads=[d_sg[s], dps[pu]], writes=[d_act[f][hf]])
            for fp in range(2):
                (d_w,), d_d = ring.load([(wd[:, fp * 2:fp * 2 + 2, :], 2, D)])
                dsl.append((d_w, d_d))
            for t in range(NT):
                hf = t // 4
                for db in range(4):
                    pb = dbanks[di % 4]
                    di += 1
                    pairs = [(actT[:, f, t * 128:(t + 1) * 128], dsl[f // 2][0][:, f % 2, db * 512:(db + 1) * 512])
                             for f in range(4)]
                    mm_group(bank(pb), pairs, reads=[d_act[f][hf] for f in range(4)] + [dsl[0][1], dsl[1][1]],
                             writes=[dps[pb]])
                    a = di % 4
                    K.op(K.dve, lambda e, a=a, pb=pb, t=t, db=db, ei=ei: e.scalar_tensor_tensor(
                        out=acc[a], in0=bank(pb), scalar=comb[:, t, ei:ei + 1], in1=gate_bc[:, db * 512:(db + 1) * 512],
                        op0=ALU.mult, op1=ALU.mult), reads=[dps[pb], d_comb[t], d_gate], writes=[d_acc[a]])
                    xs = X[:, t, db * 512:(db + 1) * 512]
                    if ei == 0:
                        K.op(K.dve, lambda e, a=a, xs=xs: e.scalar_tensor_tensor(
                            out=xs, in0=xs, scalar=ALPHA, in1=acc[a], op0=ALU.mult, op1=ALU.add),
                            reads=[d_acc[a]] + d_hT, writes=[dX[t]])
                    else:
                        K.op(K.dve, lambda e, a=a, xs=xs: e.tensor_tensor(out=xs, in0=xs, in1=acc[a], op=ALU.add),
                             reads=[d_acc[a]], writes=[dX[t]])
        chk("moe")
        for t in range(NT):
            emit_ln_tile(t, gb, d_gb, scr, d_scr)
        K.barrier()
        mem.top = PH_X


    def rstd_from_ss(col, n, dep):
        K.op(K.dve, lambda e: e.tensor_scalar(out=col, in0=col, scalar1=1.0 / n, scalar2=RMS_EPS,
                                              op0=ALU.mult, op1=ALU.add), reads=[dep], writes=[dep])
        K.op(K.act, lambda e: e.activation(out=col, in_=col, func=AF.Ln), reads=[dep], writes=[dep])
        K.op(K.act, lambda e: e.activation(out=col, in_=col, func=AF.Exp, scale=-0.5), reads=[dep], writes=[dep])

    def emit_out_proj(l, w_out_l, oT, d_oT, ring):
        wv = w_out_l.rearrange("(c p) n -> p c n", p=128)
        tmpo = [mem.alloc([128, 256], F32) for _ in range(2)]
        d_tmpo = [Dep(), Dep()]
        gb, d_gb, scr, d_scr = load_ln(l, 0)
        n = 0
        for blk in range(8):
            (w,), dw = ring.load([(wv[:, :, blk * 256:(blk + 1) * 256], KC, 256)])
            for t in range(NT):
                pb = n % 4
                a = n % 2
                n += 1
                mm_group(bank(pb, 256), [(oT[:, kc, t * 128:(t + 1) * 128], w[:, kc, :]) for kc in range(KC)],
                         reads=[d_oT, dw], writes=[dps[pb]])
                K.op(K.dve, lambda e, a=a, pb=pb, blk=blk: e.tensor_tensor(
                    out=tmpo[a], in0=bank(pb, 256), in1=gate_bc[:, blk * 256:(blk + 1) * 256], op=ALU.mult),
                    reads=[dps[pb], d_gate], writes=[d_tmpo[a]])
                xs = X[:, t, blk * 256:(blk + 1) * 256]
                K.op(K.dve, lambda e, a=a, xs=xs: e.scalar_tensor_tensor(
                    out=xs, in0=xs, scalar=ALPHA, in1=tmpo[a], op0=ALU.mult, op1=ALU.add),
                    reads=[d_tmpo[a]], writes=[dX[t]])
        for t in range(NT):
            emit_ln_tile(t, gb, d_gb, scr, d_scr)

    def attn_scores(sb0, qparts, kparts):
        deps_r = []
        for (q, dq), (k, dk_) in zip(qparts, kparts):
            deps_r += [dq, dk_]
        for j, (c0, n) in enumerate(((0, 512), (512, 512), (1024, 256))):
            mm_group(bank(sb0 + j, n), [(q, k[:, c0:c0 + n]) for (q, _), (k, _) in zip(qparts, kparts)],
                     reads=deps_r, writes=[dps[sb0 + j]])

    def softmax_exp(sb0, scale, e_out, d_e, cols, d_cols, ci):
        sc = ps[:, sb0 * 512: sb0 * 512 + NKEY]
        rd = [dps[sb0], dps[sb0 + 1], dps[sb0 + 2]]
        K.op(K.dve, lambda e: e.reduce_max(out=cols[:, ci:ci + 1], in_=sc, axis=AX.X), reads=rd, writes=[d_cols])
        K.op(K.dve, lambda e: e.tensor_scalar(out=cols[:, ci:ci + 1], in0=cols[:, ci:ci + 1], scalar1=-scale,
                                              scalar2=None, op0=ALU.mult), reads=[d_cols], writes=[d_cols])
        K.op(K.act, lambda e: e.activation(out=e_out, in_=sc, func=AF.Exp, scale=scale, bias=cols[:, ci:ci + 1],
                                           accum_out=cols[:, ci + 1:ci + 2]), reads=rd + [d_cols], writes=[d_e, d_cols])

    def attn_pv(e_in, d_e, eT, d_eT, V, d_V, vcol0, out_ps_off):
        pT6 = bank(6).bitcast(BF16)
        pT7 = bank(7, 128).bitcast(BF16)
        def tr(e):
            ins = None
            for kt in range(NKT):
                dst = pT6[:, kt * 128:(kt + 1) * 128] if kt < 8 else pT7[:, (kt - 8) * 128:(kt - 7) * 128]
                ins = e.transpose(out=dst, in_=e_in[:, kt * 128:(kt + 1) * 128], identity=identB)
            return ins
        K.op(K.pe, tr, reads=[d_e, d_identB], writes=[dps[6], dps[7]])
        K.op(K.act, lambda e: e.copy(out=eT[:, 0:8, :], in_=pT6.rearrange("p (a b) -> p a b", a=8)),
             reads=[dps[6]], writes=[d_eT])
        K.op(K.act, lambda e: e.copy(out=eT[:, 8:10, :], in_=pT7.rearrange("p (a b) -> p a b", a=2)),
             reads=[dps[7]], writes=[d_eT])
        mm_group(bank(7, 128, out_ps_off), [(eT[:, kt, :], V[:, kt, vcol0:vcol0 + 128]) for kt in range(NKT)],
                 reads=[d_eT, d_V], writes=[dps[7]])

    def emit_mixer_mla(l):
        i = l // 2
        mem.top = PH_X
        ring = Ring(K, mem, 4, 4096)
        emit_mod(l, 0, ring, segs=(0, 1))
        cqnT = mem.alloc([128, 4, T], BF16); d_cqnT = Dep()
        ckvT = mem.alloc([128, 2, NKEY], BF16); d_ckvT = Dep()
        krT = mem.alloc([96, NKEY], BF16); d_krT = Dep()
        oT = mem.alloc([128, KC, T], BF16); d_oT = Dep()
        cs_tm = mem.alloc([128, NT, 2, 32], F32); d_cs = Dep()
        gq = mem.alloc([128, 512], F32); gkv = mem.alloc([128, 256], F32); d_g = Dep()
        K.dma(K.q_sync, cs_tm[:, :, 0, :], rope_cos.rearrange("(t p) r -> p t r", p=128), writes=[d_cs])
        K.dma(K.q_sync, cs_tm[:, :, 1, :], rope_sin.rearrange("(t p) r -> p t r", p=128), writes=[d_cs])
        K.dma(K.q_sync, gq, mla_q_norm_g[i].partition_broadcast(128), writes=[d_g])
        K.dma(K.q_sync, gkv, mla_kv_norm_g[i].partition_broadcast(128), writes=[d_g])
        K.op(K.dve, lambda e: e.memset(krT[64:96, :], 0.0), writes=[d_krT])
        K.dma(K.q_pool, krT[64:68, :], kmask, writes=[d_krT])
        mark1 = mem.top
        hT = mem.alloc([128, KC, T], BF16)
        d_hT = [Dep() for _ in range(NT)]
        emit_convert(0, hT, d_hT)
        emit_mod(l, 0, ring, segs=(2,))
        wv = mla_w_in[i].rearrange("(c p) n -> p c n", p=128)
        wblk = []
        for c0, n in ((0, 256), (256, 256), (512, 256), (768, 64)):
            (w,), dw = ring.load([(wv[:, :, c0:c0 + n], KC, n)])
            wblk.append((w, dw, n))
        cch = mem.alloc([128, 2, 256], F32); d_cch = Dep()
        kch = mem.alloc([128, 2, 64], F32); d_kch = Dep()
        K.dma(K.q_sync, cch, c_ckv[i].rearrange("(t p) f -> p t f", p=128), writes=[d_cch])
        K.dma(K.q_sync, kch, c_kr[i].rearrange("(t p) f -> p t f", p=128), writes=[d_kch])
        cqn = mem.alloc([128, 512], F32); ckvn = mem.alloc([128, 256], F32); krf = mem.alloc([128, 64], F32)
        krr = mem.alloc([128, 64], F32); rt1 = mem.alloc([128, 32], F32); rt2 = mem.alloc([128, 32], F32)
        junk = mem.alloc([128, 512], BF16); c1 = mem.alloc([128, 4], F32)
        d_cqn, d_ckvn, d_krf, d_krr, d_junk, d_c1 = (Dep() for _ in range(6))
        for tt in range(2):
            def trc(e, tt=tt):
                ins = None
                for c in range(2):
                    ins = e.transpose(out=bank(0, 128, c * 128), in_=cch[:, tt, c * 128:(c + 1) * 128], identity=identF)
                ins = e.transpose(out=bank(0, 128, 256)[0:64, :], in_=kch[:, tt, :], identity=identF)
                return ins
            K.op(K.pe, trc, reads=[d_cch, d_kch, d_identF], writes=[dps[0]])
            K.op(K.act, lambda e, tt=tt: e.copy(out=ckvT[:, :, tt * 128:(tt + 1) * 128],
                                                in_=bank(0, 256).rearrange("p (c j) -> p c j", c=2)),
                 reads=[dps[0]], writes=[d_ckvT])
            K.op(K.act, lambda e, tt=tt: e.copy(out=krT[0:64, tt * 128:(tt + 1) * 128], in_=bank(0, 128, 256)[0:64, :]),
                 reads=[dps[0]], writes=[d_krT])
        ov_ckv = o_ckv[i].rearrange("(t p) f -> p t f", p=128)
        ov_kr = o_kr[i].rearrange("(t p) f -> p t f", p=128)
        for t in range(NT):
            tk = slice(t * 128, (t + 1) * 128)
            pa, pb_ = 2 + (t % 2) * 2, 3 + (t % 2) * 2
            for j in range(2):
                mm_group(bank(pa, 256, j * 256), [(hT[:, kc, tk], wblk[j][0][:, kc, :]) for kc in range(KC)],
                         reads=[d_hT[t], wblk[j][1]], writes=[dps[pa]])
            mm_group(bank(pb_, 256), [(hT[:, kc, tk], wblk[2][0][:, kc, :]) for kc in range(KC)],
                     reads=[d_hT[t], wblk[2][1]], writes=[dps[pb_]])
            mm_group(bank(pb_, 64, 256), [(hT[:, kc, tk], wblk[3][0][:, kc, :]) for kc in range(KC)],
                     reads=[d_hT[t], wblk[3][1]], writes=[dps[pb_]])
            K.op(K.act, lambda e, pa=pa: e.activation(out=junk, in_=bank(pa), func=AF.Square, accum_out=c1[:, 0:1]),
                 reads=[dps[pa]], writes=[d_junk, d_c1])
            K.op(K.act, lambda e, pb_=pb_: e.activation(out=junk[:, 0:256], in_=bank(pb_, 256), func=AF.Square,
                                                        accum_out=c1[:, 1:2]), reads=[dps[pb_]], writes=[d_junk, d_c1])
            rstd_from_ss(c1[:, 0:1], 512.0, d_c1)
            rstd_from_ss(c1[:, 1:2], 256.0, d_c1)
            K.op(K.dve, lambda e, pa=pa: e.scalar_tensor_tensor(out=cqn, in0=bank(pa), scalar=c1[:, 0:1], in1=gq,
                                                                op0=ALU.mult, op1=ALU.mult),
                 reads=[dps[pa], d_c1, d_g], writes=[d_cqn])
            K.op(K.dve, lambda e, pb_=pb_: e.scalar_tensor_tensor(out=ckvn, in0=bank(pb_, 256), scalar=c1[:, 1:2], in1=gkv,
                                                                  op0=ALU.mult, op1=ALU.mult),
                 reads=[dps[pb_], d_c1, d_g], writes=[d_ckvn])
            K.op(K.act, lambda e, pb_=pb_: e.copy(out=krf, in_=bank(pb_, 64, 256)), reads=[dps[pb_]], writes=[d_krf])
            K.dma(K.q_sync, ov_ckv[:, t, :], ckvn, reads=[d_ckvn])
            K.dma(K.q_sync, ov_kr[:, t, :], krf, reads=[d_krf])
            cos_t, sin_t = cs_tm[:, t, 0, :], cs_tm[:, t, 1, :]
            x1, x2 = krf[:, 0:32], krf[:, 32:64]
            V_ = K.dve
            K.op(V_, lambda e: e.tensor_tensor(out=rt1, in0=x2, in1=sin_t, op=ALU.mult), reads=[d_krf, d_cs], writes=[d_krr])
            K.op(V_, lambda e: e.tensor_tensor(out=krr[:, 0:32], in0=x1, in1=cos_t, op=ALU.mult), reads=[d_krf, d_cs], writes=[d_krr])
            K.op(V_, lambda e: e.tensor_tensor(out=krr[:, 0:32], in0=krr[:, 0:32], in1=rt1, op=ALU.subtract), reads=[d_krr], writes=[d_krr])
            K.op(V_, lambda e: e.tensor_tensor(out=rt2, in0=x1, in1=sin_t, op=ALU.mult), reads=[d_krf, d_cs], writes=[d_krr])
            K.op(V_, lambda e: e.tensor_tensor(out=krr[:, 32:64], in0=x2, in1=cos_t, op=ALU.mult), reads=[d_krf, d_cs], writes=[d_krr])
            K.op(V_, lambda e: e.tensor_tensor(out=krr[:, 32:64], in0=krr[:, 32:64], in1=rt2, op=ALU.add), reads=[d_krr], writes=[d_krr])
            def tr1(e):
                ins = None
                for c in range(4):
                    ins = e.transpose(out=bank(0, 128, c * 128), in_=cqn[:, c * 128:(c + 1) * 128], identity=identF)
                return ins
            K.op(K.pe, tr1, reads=[d_cqn, d_identF], writes=[dps[0]])
            K.op(K.act, lambda e, tk=tk: e.copy(out=cqnT[:, :, tk], in_=bank(0).rearrange("p (c j) -> p c j", c=4)),
                 reads=[dps[0]], writes=[d_cqnT])
            def tr2(e):
                ins = None
                for c in range(2):
                    ins = e.transpose(out=bank(1, 128, c * 128), in_=ckvn[:, c * 128:(c + 1) * 128], identity=identF)
                ins = e.transpose(out=bank(1, 128, 256)[0:64, :], in_=krr, identity=identF)
                return ins
            K.op(K.pe, tr2, reads=[d_ckvn, d_krr, d_identF], writes=[dps[1]])
            kk = slice(PAST + t * 128, PAST + (t + 1) * 128)
            K.op(K.act, lambda e, kk=kk: e.copy(out=ckvT[:, :, kk], in_=bank(1, 256).rearrange("p (c j) -> p c j", c=2)),
                 reads=[dps[1]], writes=[d_ckvT])
            K.op(K.act, lambda e, kk=kk: e.copy(out=krT[0:64, kk], in_=bank(1, 128, 256)[0:64, :]),
                 reads=[dps[1]], writes=[d_krT])
        K.barrier()
        mem.top = mark1
        rC = mem.alloc([64, T], F32); rS = mem.alloc([64, T], F32); d_rCS = Dep()
        K.dma(K.q_sync, rC, ropeT_c, writes=[d_rCS])
        K.dma(K.q_sync, rS, ropeT_s, writes=[d_rCS])
        qTn = mem.alloc([128, 2, T], BF16); d_qTn = Dep()
        qTr = mem.alloc([96, 2, T], BF16); d_qTr = Dep()
        K.op(K.dve, lambda e: e.memset(qTr[64:96, :, :], 0.0), writes=[d_qTr])
        for hh in range(2):
            K.dma(K.q_pool, qTr[64:68, hh, :], seqsel, writes=[d_qTr])
        kTn = mem.alloc([128, 2, NKEY], BF16); d_kTn = Dep()
        Vh = mem.alloc([128, NKT, 256], BF16); d_Vh = Dep()
        wsw = mem.alloc([128, 4, 2, 64], BF16); d_wsw = Dep()
        tq = mem.alloc([64, 512], F32); d_tq = Dep()
        tq2 = mem.alloc([64, 512], F32); d_tq2 = Dep()
        eb = [mem.alloc([128, NKEY], BF16) for _ in range(2)]; d_eb = [Dep(), Dep()]
        eT = mem.alloc([128, NKT, 128], BF16); d_eT = Dep()
        on = mem.alloc([128, 128], F32); d_on = Dep()
        cols2 = [mem.alloc([128, 8], F32) for _ in range(2)]; d_cols2 = [Dep(), Dep()]
        wq_v = mla_w_uq[i].rearrange("(c p) n -> p c n", p=128)
        wkv_v = mla_w_ukv[i].rearrange("(c p) n -> p c n", p=128)
        scale = (128 + 64) ** -0.5
        it = 0
        for hp in range(8):
            (wq,), d_wq = ring.load([(wq_v[:, :, hp * 384:(hp + 1) * 384], 4, 384)])
            (wkv,), d_wkv = ring.load([(wkv_v[:, :, hp * 512:(hp + 1) * 512], 2, 512)])
            for hh in range(2):
                b0 = hh * 192 + 128
                K.op(K.dve, lambda e, hh=hh, b0=b0: e.tensor_copy(out=wsw[:, :, hh, 0:32], in_=wq[:, :, b0 + 32:b0 + 64]),
                     reads=[d_wq], writes=[d_wsw])
                K.op(K.dve, lambda e, hh=hh, b0=b0: e.tensor_copy(out=wsw[:, :, hh, 32:64], in_=wq[:, :, b0:b0 + 32]),
                     reads=[d_wq], writes=[d_wsw])
            for hh in range(2):
                for hf in range(2):
                    tok = slice(hf * 512, (hf + 1) * 512)
                    mm_group(bank(0), [(wq[:, c, hh * 192:hh * 192 + 128], cqnT[:, c, tok]) for c in range(4)],
                             reads=[d_wq, d_cqnT], writes=[dps[0]])
                    K.op(K.act, lambda e, hh=hh, tok=tok: e.copy(out=qTn[:, hh, tok], in_=bank(0)), reads=[dps[0]], writes=[d_qTn])
                    mm_group(bank(1)[0:64, :], [(wq[:, c, hh * 192 + 128:hh * 192 + 192], cqnT[:, c, tok]) for c in range(4)],
                             reads=[d_wq, d_cqnT], writes=[dps[1]])
                    mm_group(bank(2)[0:64, :], [(wsw[:, c, hh, :], cqnT[:, c, tok]) for c in range(4)],
                             reads=[d_wsw, d_cqnT], writes=[dps[2]])
                    K.op(K.dve, lambda e, tok=tok: e.tensor_tensor(out=tq, in0=bank(2)[0:64, :], in1=rS[:, tok], op=ALU.mult),
                         reads=[dps[2], d_rCS], writes=[d_tq])
                    K.op(K.dve, lambda e, tok=tok: e.tensor_tensor(out=tq2, in0=bank(1)[0:64, :], in1=rC[:, tok], op=ALU.mult),
                         reads=[dps[1], d_rCS], writes=[d_tq2])
                    K.op(K.dve, lambda e, hh=hh, tok=tok: e.tensor_tensor(out=qTr[0:64, hh, tok], in0=tq, in1=tq2, op=ALU.add),
                         reads=[d_tq, d_tq2], writes=[d_qTr])
                for j, (c0, n) in enumerate(((0, 512), (512, 512), (1024, 256))):
                    pbk = 3 + j
                    mm_group(bank(pbk, n), [(wkv[:, c, hh * 256:hh * 256 + 128], ckvT[:, c, c0:c0 + n]) for c in range(2)],
                             reads=[d_wkv, d_ckvT], writes=[dps[pbk]])
                    K.op(K.act, lambda e, hh=hh, c0=c0, n=n, pbk=pbk: e.copy(out=kTn[:, hh, c0:c0 + n], in_=bank(pbk, n)),
                         reads=[dps[pbk]], writes=[d_kTn])
            for kt in range(NKT):
                pbv = kt % 2
                mm_group(bank(pbv, 256).rearrange("p (a b) -> p a b", a=2),
                         [(ckvT[:, c, kt * 128:(kt + 1) * 128],
                           wkv[:, c, :].rearrange("p (a b) -> p a b", a=2)[:, :, 128:256]) for c in range(2)],
                         reads=[d_wkv, d_ckvT], writes=[dps[pbv]])
                K.op(K.act, lambda e, kt=kt, pbv=pbv: e.copy(out=Vh[:, kt, :], in_=bank(pbv, 256)), reads=[dps[pbv]], writes=[d_Vh])
            pending = None
            for hh in range(2):
                h = hp * 2 + hh
                for qb in range(NT):
                    qs = slice(qb * 128, (qb + 1) * 128)
                    sb0 = (it % 2) * 3
                    es = it % 2
                    it += 1
                    attn_scores(sb0, [(qTn[:, hh, qs], d_qTn), (qTr[:, hh, qs], d_qTr)],
                                [(kTn[:, hh, :], d_kTn), (krT, d_krT)])
                    if pending is not None:
                        pending[0]()
                    softmax_exp(sb0, scale, eb[es], d_eb[es], cols2[es], d_cols2[es], 0)
                    if pending is not None:
                        pending[1]()

                    def stage2a(es=es, hh=hh):
                        attn_pv(eb[es], d_eb[es], eT, d_eT, Vh, d_Vh, hh * 128, 256)

                    def stage2(es=es, hh=hh, h=h, qs=qs):
                        cols, d_cols = cols2[es], d_cols2[es]
                        K.op(K.dve, lambda e: e.reciprocal(out=cols[:, 2:3], in_=cols[:, 1:2]), reads=[d_cols], writes=[d_cols])
                        K.op(K.dve, lambda e: e.tensor_scalar(out=on, in0=bank(7, 128, 256), scalar1=cols[:, 2:3], scalar2=None,
                                                              op0=ALU.mult), reads=[dps[7], d_cols], writes=[d_on])
                        K.op(K.pe, lambda e: e.transpose(out=bank(7, 128, 384), in_=on, identity=identF),
                             reads=[d_on, d_identF], writes=[dps[7]])
                        K.op(K.act, lambda e: e.copy(out=oT[:, h, qs], in_=bank(7, 128, 384)),
                             reads=[dps[7]], writes=[d_oT])
                    pending = (stage2a, stage2)
            pending[0]()
            pending[1]()
        K.barrier()
        mem.top = mark1
        emit_out_proj(l, mla_w_out[i], oT, d_oT, ring)
        K.barrier()
        mem.top = PH_X


    def emit_mixer_ab(l):
        i = l // 2
        lam_init = 0.8 - 0.6 * math.exp(-0.3 * l)
        mem.top = PH_X
        ring = Ring(K, mem, 3, 4096)
        emit_mod(l, 0, ring, segs=(0, 1))
        oT = mem.alloc([128, KC, T], BF16); d_oT = Dep()
        hT = mem.alloc([128, KC, T], BF16)
        d_hT = [Dep() for _ in range(NT)]
        emit_convert(0, hT, d_hT)
        emit_mod(l, 0, ring, segs=(2,))
        cs_tm = mem.alloc([128, NT, 2, 32], F32); d_cs = Dep()
        K.dma(K.q_sync, cs_tm[:, :, 0, :], rope_cos.rearrange("(t p) r -> p t r", p=128), writes=[d_cs])
        K.dma(K.q_sync, cs_tm[:, :, 1, :], rope_sin.rearrange("(t p) r -> p t r", p=128), writes=[d_cs])
        wv = ab_w_in[i].rearrange("(c p) n -> p c n", p=128)
        markA = mem.top
        xsp = x_spill.rearrange("(t p) d -> p t d", p=128)
        for t in range(NT):
            K.dma(K.q_sync, xsp[:, t, :], X[:, t, :], reads=[dX[t]])
        K.barrier()
        mem.top = PH_NOX
        if not cfg.get("gla", True):
            K.op(K.dve, lambda e: e.memset(oT[:, 0:8, :], 0.0), writes=[d_oT])
        else:
            emit_gla(l, i, hT, d_hT, oT, d_oT, wv, ring)
            K.barrier()
            mem.top = PH_NOX
        lvb = mem.alloc([128, 4, 64], F32); lcol = mem.alloc([128, 8], F32); d_l = Dep()
        gdb = mem.alloc([128, 128], F32); d_gdb = Dep()
        K.dma(K.q_sync, lvb, diff_lambda[i].partition_broadcast(128), writes=[d_l])
        K.dma(K.q_sync, gdb, diff_norm_g[i].partition_broadcast(128), writes=[d_gdb])
        K.op(K.dve, lambda e: e.tensor_scalar(out=gdb, in0=gdb, scalar1=1.0 - lam_init, scalar2=None, op0=ALU.mult),
             reads=[d_gdb], writes=[d_gdb])
        lv4 = lvb.rearrange("p (a b) d -> p a b d", a=2)
        prod = mem.alloc([128, 2, 64], F32)
        K.op(K.dve, lambda e: e.tensor_tensor(out=prod, in0=lv4[:, :, 0, :], in1=lv4[:, :, 1, :], op=ALU.mult),
             reads=[d_l], writes=[d_l])
        K.op(K.dve, lambda e: e.tensor_reduce(out=lcol[:, 0:2], in_=prod, axis=AX.X, op=ALU.add), reads=[d_l], writes=[d_l])
        K.op(K.act, lambda e: e.activation(out=lcol[:, 0:2], in_=lcol[:, 0:2], func=AF.Exp), reads=[d_l], writes=[d_l])
        K.op(K.dve, lambda e: e.tensor_tensor(out=lcol[:, 2:3], in0=lcol[:, 1:2], in1=lcol[:, 0:1], op=ALU.subtract),
             reads=[d_l], writes=[d_l])
        K.op(K.dve, lambda e: e.tensor_scalar(out=lcol[:, 2:3], in0=lcol[:, 2:3], scalar1=-lam_init, scalar2=None,
                                              op0=ALU.add), reads=[d_l], writes=[d_l])
        neg_lam = lcol[:, 2:3]
        qT = mem.alloc([96, 4, T], BF16); d_qT = Dep()
        kT = mem.alloc([96, 4, NKEY], BF16); d_kT = Dep()
        K.op(K.dve, lambda e: e.memset(qT[64:96, :, :], 0.0), writes=[d_qT])
        K.op(K.dve, lambda e: e.memset(kT[64:96, :, :], 0.0), writes=[d_kT])
        Vd = mem.alloc([128, NKT, 256], BF16); d_Vd = Dep()
        for s4 in range(4):
            K.dma(K.q_pool, qT[64:68, s4, :], seqsel, writes=[d_qT])
            K.dma(K.q_pool, kT[64:68, s4, :], kmask, writes=[d_kT])
        kc_f = mem.alloc([128, 2, 256], F32); d_kcf = Dep()
        k2f = [mem.alloc([128, 256], F32) for _ in range(2)]; d_k2f = [Dep(), Dep()]
        v2f = [mem.alloc([128, 256], F32) for _ in range(2)]; d_v2f = [Dep(), Dep()]
        qr = mem.alloc([128, 4, 64], F32); kr_ = mem.alloc([128, 4, 64], F32); d_qr = Dep(); d_kr = Dep()
        r1 = mem.alloc([128, 4, 32], F32); r2 = mem.alloc([128, 4, 32], F32); d_r = Dep()
        eb4 = [[mem.alloc([128, NKEY], BF16) for _ in range(2)] for _ in range(2)]
        d_eb4 = [[Dep(), Dep()], [Dep(), Dep()]]
        wg = mem.alloc([128, NKEY], BF16); d_wg = Dep()
        eT = mem.alloc([128, NKT, 128], BF16); d_eT = Dep()
        on = mem.alloc([128, 128], F32); d_on = Dep()
        junk = mem.alloc([128, 128], BF16); d_junk = Dep()
        colsj = [[mem.alloc([128, 4], F32) for _ in range(2)] for _ in range(2)]
        d_colsj = [[Dep(), Dep()], [Dep(), Dep()]]
        cols2 = [mem.alloc([128, 8], F32) for _ in range(2)]; d_cols2 = [Dep(), Dep()]
        odk = o_dk[i].rearrange("(t p) f -> p t f", p=128)
        odv = o_dv[i].rearrange("(t p) f -> p t f", p=128)
        cdk = c_dk[i].rearrange("(t p) f -> p t f", p=128)
        cdv = c_dv[i].rearrange("(t p) f -> p t f", p=128)

        def rope_tm(dst, src, rd, t, d_dst):
            cos_b = cs_tm[:, t, 0, :].unsqueeze(1).to_broadcast([128, 4, 32])
            sin_b = cs_tm[:, t, 1, :].unsqueeze(1).to_broadcast([128, 4, 32])
            x1, x2 = src[:, :, 0:32], src[:, :, 32:64]
            V_ = K.dve
            K.op(V_, lambda e: e.tensor_tensor(out=r1, in0=x2, in1=sin_b, op=ALU.mult), reads=rd + [d_cs], writes=[d_r])
            K.op(V_, lambda e: e.tensor_tensor(out=dst[:, :, 0:32], in0=x1, in1=cos_b, op=ALU.mult), reads=rd + [d_cs], writes=[d_dst])
            K.op(V_, lambda e: e.tensor_tensor(out=dst[:, :, 0:32], in0=dst[:, :, 0:32], in1=r1, op=ALU.subtract), reads=[d_r], writes=[d_dst])
            K.op(V_, lambda e: e.tensor_tensor(out=r2, in0=x1, in1=sin_b, op=ALU.mult), reads=rd + [d_cs], writes=[d_r])
            K.op(V_, lambda e: e.tensor_tensor(out=dst[:, :, 32:64], in0=x2, in1=cos_b, op=ALU.mult), reads=rd + [d_cs], writes=[d_dst])
            K.op(V_, lambda e: e.tensor_tensor(out=dst[:, :, 32:64], in0=dst[:, :, 32:64], in1=r2, op=ALU.add), reads=[d_r], writes=[d_dst])

        def tr4(src, d_src, dstT, d_dstT, col0, pb):
            def f(e):
                ins = None
                for s4 in range(4):
                    ins = e.transpose(out=bank(pb, 128, s4 * 128)[0:64, :], in_=src[:, s4, :], identity=identF)
                return ins
            K.op(K.pe, f, reads=[d_src, d_identF], writes=[dps[pb]])
            K.op(K.act, lambda e: e.copy(out=dstT[0:64, :, col0:col0 + 128],
                                         in_=bank(pb)[0:64, :].rearrange("p (a b) -> p a b", a=4)),
                 reads=[dps[pb]], writes=[d_dstT])

        it = 0
        for hp in range(4):
            (wq,), d_wq = ring.load([(wv[:, :, 3104 + hp * 256:3104 + (hp + 1) * 256], KC, 256)])
            (wk,), d_wk = ring.load([(wv[:, :, 4128 + hp * 256:4128 + (hp + 1) * 256], KC, 256)])
            (wvv,), d_wv = ring.load([(wv[:, :, 5152 + hp * 256:5152 + (hp + 1) * 256], KC, 256)])
            K.dma(K.q_sync, kc_f, cdk[:, :, hp * 256:(hp + 1) * 256], writes=[d_kcf])
            K.dma(K.q_pool, Vd[:, 0:2, :], cdv[:, :, hp * 256:(hp + 1) * 256], writes=[d_Vd])
            for tt in range(2):
                tr4(kc_f[:, tt, :].rearrange("p (a b) -> p a b", a=4), d_kcf, kT, d_kT, tt * 128, 6)
            for t in range(NT):
                tk = slice(t * 128, (t + 1) * 128)
                a = t % 2
                pq = 0 + a * 3
                mm_group(bank(pq, 256), [(hT[:, kc, tk], wq[:, kc, :]) for kc in range(KC)], reads=[d_hT[t], d_wq], writes=[dps[pq]])
                mm_group(bank(pq + 1, 256), [(hT[:, kc, tk], wk[:, kc, :]) for kc in range(KC)], reads=[d_hT[t], d_wk], writes=[dps[pq + 1]])
                mm_group(bank(pq + 2, 256), [(hT[:, kc, tk], wvv[:, kc, :]) for kc in range(KC)], reads=[d_hT[t], d_wv], writes=[dps[pq + 2]])
                K.op(K.act, lambda e, a=a, pq=pq: e.copy(out=k2f[a], in_=bank(pq + 1, 256)), reads=[dps[pq + 1]], writes=[d_k2f[a]])
                K.op(K.act, lambda e, a=a, pq=pq: e.copy(out=v2f[a], in_=bank(pq + 2, 256)), reads=[dps[pq + 2]], writes=[d_v2f[a]])
                K.dma(K.q_sync, odk[:, t, hp * 256:(hp + 1) * 256], k2f[a], reads=[d_k2f[a]])
                K.dma(K.q_sync, odv[:, t, hp * 256:(hp + 1) * 256], v2f[a], reads=[d_v2f[a]])
                K.op(K.dve, lambda e, a=a, t=t: e.tensor_copy(out=Vd[:, 2 + t, :], in_=v2f[a]), reads=[d_v2f[a]], writes=[d_Vd])
                rope_tm(qr, bank(pq, 256).rearrange("p (a b) -> p a b", a=4), [dps[pq]], t, d_qr)
                rope_tm(kr_, k2f[a].rearrange("p (a b) -> p a b", a=4), [d_k2f[a]], t, d_kr)
                tr4(qr, d_qr, qT, d_qT, t * 128, 6)
                tr4(kr_, d_kr, kT, d_kT, PAST + t * 128, 6)
            pending = None
            for hh in range(2):
                h = hp * 2 + hh
                for qb in range(NT):
                    qs = slice(qb * 128, (qb + 1) * 128)
                    par = it % 2
                    it += 1
                    for j in range(2):
                        s4 = hh * 2 + j
                        attn_scores(j * 3, [(qT[:, s4, qs], d_qT)], [(kT[:, s4, :], d_kT)])
                    if pending is not None:
                        pending[0]()
                    for j in range(2):
                        softmax_exp(j * 3, 0.125, eb4[par][j], d_eb4[par][j], colsj[par][j], d_colsj[par][j], 0)
                    if pending is not None:
                        pending[1]()

                    def stage2a(par=par, hh=hh):
                        cols, d_cols = cols2[par], d_cols2[par]
                        eb, d_eb = eb4[par], d_eb4[par]
                        K.op(K.dve, lambda e: e.reciprocal(out=cols[:, 4:5], in_=colsj[par][0][:, 1:2]), reads=[d_colsj[par][0]], writes=[d_cols])
                        K.op(K.dve, lambda e: e.reciprocal(out=cols[:, 5:6], in_=colsj[par][1][:, 1:2]), reads=[d_colsj[par][1]], writes=[d_cols])
                        K.op(K.dve, lambda e: e.tensor_tensor(out=cols[:, 5:6], in0=cols[:, 5:6], in1=neg_lam, op=ALU.mult),
                             reads=[d_cols, d_l], writes=[d_cols])
                        K.op(K.dve, lambda e: e.tensor_scalar(out=wg, in0=eb[0], scalar1=cols[:, 4:5], scalar2=None, op0=ALU.mult),
                             reads=[d_eb[0], d_cols], writes=[d_wg])
                        K.op(K.dve, lambda e: e.scalar_tensor_tensor(out=wg, in0=eb[1], scalar=cols[:, 5:6], in1=wg,
                                                                     op0=ALU.mult, op1=ALU.add),
                             reads=[d_eb[1], d_cols], writes=[d_wg])
                        attn_pv(wg, d_wg, eT, d_eT, Vd, d_Vd, hh * 128, 256)

                    def stage2(par=par, hh=hh, h=h, qs=qs):
                        cols, d_cols = cols2[par], d_cols2[par]
                        K.op(K.act, lambda e: e.activation(out=junk, in_=bank(7, 128, 256), func=AF.Square, accum_out=cols[:, 6:7]),
                             reads=[dps[7]], writes=[d_junk, d_cols])
                        rstd_from_ss(cols[:, 6:7], 128.0, d_cols)
                        K.op(K.dve, lambda e: e.scalar_tensor_tensor(out=on, in0=bank(7, 128, 256), scalar=cols[:, 6:7], in1=gdb,
                                                                     op0=ALU.mult, op1=ALU.mult),
                             reads=[dps[7], d_cols, d_gdb], writes=[d_on])
                        K.op(K.pe, lambda e: e.transpose(out=bank(7, 128, 384), in_=on, identity=identF),
                             reads=[d_on, d_identF], writes=[dps[7]])
                        K.op(K.act, lambda e: e.copy(out=oT[:, 8 + h, qs], in_=bank(7, 128, 384)),
                             reads=[dps[7]], writes=[d_oT])
                    pending = (stage2a, stage2)
            pending[0]()
            pending[1]()
        assert mem.top <= PH_X, f"low region overflow {mem.top} > {PH_X}"
        K.barrier()
        mem.top = markA
        for t in range(NT):
            K.dma(K.q_sync, X[:, t, :], xsp[:, t, :], writes=[dX[t]])
        emit_out_proj(l, ab_w_out[i], oT, d_oT, ring)
        K.barrier()
        mem.top = PH_X

    def emit_gla(l, i, hT, d_hT, oT, d_oT, wv, ring):
        glam_sb = mem.alloc([128, 6, 128], F32); d_glam = Dep()
        for m in range(6):
            K.dma(K.q_sync, glam_sb[:, m, :], glam_d[m], writes=[d_glam])
        csel_sb = mem.alloc([128, 4], F32)
        K.dma(K.q_sync, csel_sb, csel_d, writes=[d_glam])
        keepc = mem.alloc([128, 1], F32)
        K.dma(K.q_sync, keepc, keep, writes=[d_glam])
        ggb = mem.alloc([128, 256], F32)
        K.dma(K.q_sync, ggb, gla_norm_g[i].partition_broadcast(128), writes=[d_glam])
        ggT = mem.alloc([17, 2, T], F32); d_ggT = Dep()
        K.op(K.dve, lambda e: e.memset(ggT, 1.0), writes=[d_ggT])
        wg2 = mem.alloc([17, 2, 512], F32); d_wg2 = Dep()
        for dr in range(2):
            K.dma(K.q_sync, wg2[0:16, dr, :], gla_w_gate2[i, dr], writes=[d_wg2])
            K.dma(K.q_sync, wg2[16:17, dr, :], gla_b_gate2[i, dr].rearrange("(o f) -> o f", o=1), writes=[d_wg2])
        (wgg,), d_wgg = ring.load([(wv[:, :, 3072:3104], KC, 32)])
        for dr in range(2):
            for hf in range(2):
                tok = slice(hf * 512, (hf + 1) * 512)
                mm_group(bank(0)[0:16, :], [(wgg[:, kc, dr * 16:(dr + 1) * 16], hT[:, kc, tok]) for kc in range(KC)],
                         reads=d_hT + [d_wgg], writes=[dps[0]])
                K.op(K.act, lambda e, dr=dr, tok=tok: e.copy(out=ggT[0:16, dr, tok], in_=bank(0)[0:16, :]),
                     reads=[dps[0]], writes=[d_ggT])
        q_f = mem.alloc([128, NT, 128], F32); k_f = mem.alloc([128, NT, 128], F32); v_b = mem.alloc([128, NT, 256], BF16)
        d_qkv = Dep()
        khat = mem.alloc([128, NT, 128], BF16); qtT = mem.alloc([128, T], BF16); ktT = mem.alloc([128, T], BF16)
        AT = mem.alloc([128, NT, 128], BF16); dcol = mem.alloc([128, 32], F32)
        d_khat, d_qtT, d_ktT, d_AT, d_dcol = (Dep() for _ in range(5))
        o_f = mem.alloc([128, NT, 256], BF16); d_of = Dep()
        S = mem.alloc([128, 256], F32); S_bf = mem.alloc([128, 256], BF16); d_S = Dep(); d_Sbf = Dep()
        qblk = mem.alloc([128, 4, 128], BF16); khm = mem.alloc([128, 4, 128], BF16); d_qblk = Dep(); d_khm = Dep()
        K.op(K.dve, lambda e: e.memset(qblk, 0.0), writes=[d_qblk])
        e1_2 = [mem.alloc([128, 128], F32) for _ in range(2)]; sp_2 = [mem.alloc([128, 128], F32) for _ in range(2)]
        ebt_2 = [mem.alloc([128, 3, 128], F32) for _ in range(2)]
        qt_2 = [mem.alloc([128, 128], F32) for _ in range(2)]; kt_2 = [mem.alloc([128, 128], F32) for _ in range(2)]
        d_e1_2, d_sp_2, d_ebt_2, d_qt_2, d_kt_2 = ([Dep(), Dep()] for _ in range(5))
        osum = mem.alloc([128, 256], F32); sgr = mem.alloc([128, 256], F32); on = mem.alloc([128, 256], F32)
        junk = mem.alloc([128, 256], BF16); cg = mem.alloc([128, 4], F32)
        d_osum, d_sgr, d_on, d_junk, d_cg = (Dep() for _ in range(5))
        og = o_gla[i]
        for h in range(4):
            (wq_,), d_wq = ring.load([(wv[:, :, h * 128:(h + 1) * 128], KC, 128)])
            (wk_,), d_wk = ring.load([(wv[:, :, 512 + h * 128:512 + (h + 1) * 128], KC, 128)])
            (wv_,), d_wv_ = ring.load([(wv[:, :, 1024 + h * 256:1024 + (h + 1) * 256], KC, 256)])
            for t in range(NT):
                tk = slice(t * 128, (t + 1) * 128)
                mm_group(bank(0, 128, 0), [(hT[:, kc, tk], wq_[:, kc, :]) for kc in range(KC)], reads=[d_hT[t], d_wq], writes=[dps[0]])
                mm_group(bank(0, 128, 128), [(hT[:, kc, tk], wk_[:, kc, :]) for kc in range(KC)], reads=[d_hT[t], d_wk], writes=[dps[0]])
                mm_group(bank(0, 256, 256), [(hT[:, kc, tk], wv_[:, kc, :]) for kc in range(KC)], reads=[d_hT[t], d_wv_], writes=[dps[0]])
                K.op(K.act, lambda e, t=t: e.mul(out=q_f[:, t, :], in_=bank(0, 128, 0), mul=128.0 ** -0.5), reads=[dps[0]], writes=[d_qkv])
                K.op(K.act, lambda e, t=t: e.copy(out=k_f[:, t, :], in_=bank(0, 128, 128)), reads=[dps[0]], writes=[d_qkv])
                K.op(K.act, lambda e, t=t: e.copy(out=v_b[:, t, :], in_=bank(0, 256, 256)), reads=[dps[0]], writes=[d_qkv])
            (wgr,), d_wgr = ring.load([(wv[:, :, 2048 + h * 256:2048 + (h + 1) * 256], KC, 256)])
            for dr in range(2):
                mi, ms = (0, 1) if dr == 0 else (2, 3)
                K.dma(K.q_sync, S, st_gla[i, dr, h], writes=[d_S])
                K.op(K.act, lambda e: e.copy(out=S_bf, in_=S), reads=[d_S], writes=[d_Sbf])
                for t in range(NT):
                    tk = slice(t * 128, (t + 1) * 128)
                    pp = t % 2
                    e1, sp, ebt, qt, kt = e1_2[pp], sp_2[pp], ebt_2[pp], qt_2[pp], kt_2[pp]
                    d_e1, d_sp, d_ebt, d_qt, d_kt = d_e1_2[pp], d_sp_2[pp], d_ebt_2[pp], d_qt_2[pp], d_kt_2[pp]
                    bL, bC, bT, bA = ((1, 2, 3, 4), (5, 6, 7, 0))[pp]
                    mm_group(bank(bL, 128), [(ggT[0:17, dr, tk], wg2[0:17, dr, h * 128:(h + 1) * 128])],
                             reads=[d_ggT, d_wg2], writes=[dps[bL]])
                    K.op(K.act, lambda e: e.activation(out=e1, in_=bank(bL, 128), func=AF.Exp, scale=-1.0), reads=[dps[bL]], writes=[d_e1])
                    K.op(K.act, lambda e: e.activation(out=sp, in_=e1, func=AF.Ln, bias=1.0), reads=[d_e1], writes=[d_sp])
                    mm_group(bank(bC, 128, 0), [(glam_sb[:, mi, :], sp)], reads=[d_glam, d_sp], writes=[dps[bC]])
                    mm_group(bank(bC, 128, 128), [(glam_sb[:, ms, :], sp)], reads=[d_glam, d_sp], writes=[dps[bC]])
                    mm_group(bank(bC, 4, 256), [(sp, csel_sb)], reads=[d_glam, d_sp], writes=[dps[bC]])
                    K.op(K.act, lambda e: e.activation(out=ebt[:, 0, :], in_=bank(bC, 128, 0), func=AF.Exp, scale=-1.0 / 16), reads=[dps[bC]], writes=[d_ebt])
                    K.op(K.act, lambda e: e.activation(out=ebt[:, 1, :], in_=bank(bC, 128, 0), func=AF.Exp, scale=1.0 / 16), reads=[dps[bC]], writes=[d_ebt])
                    K.op(K.act, lambda e: e.activation(out=ebt[:, 2, :], in_=bank(bC, 128, 128), func=AF.Exp, scale=-1.0 / 16), reads=[dps[bC]], writes=[d_ebt])
                    K.op(K.act, lambda e, t=t: e.activation(out=dcol[:, t * 4:(t + 1) * 4], in_=bank(bC, 4, 256), func=AF.Exp, scale=-1.0 / 16),
                         reads=[dps[bC]], writes=[d_dcol])
                    K.op(K.dve, lambda e, t=t: e.tensor_tensor(out=qt, in0=q_f[:, t, :], in1=ebt[:, 0, :], op=ALU.mult), reads=[d_qkv, d_ebt], writes=[d_qt])
                    K.op(K.dve, lambda e, t=t: e.tensor_tensor(out=kt, in0=k_f[:, t, :], in1=ebt[:, 1, :], op=ALU.mult), reads=[d_qkv, d_ebt], writes=[d_kt])
                    K.op(K.dve, lambda e, t=t: e.tensor_tensor(out=khat[:, t, :], in0=k_f[:, t, :], in1=ebt[:, 2, :], op=ALU.mult), reads=[d_qkv, d_ebt], writes=[d_khat])
                    def trqk(e):
                        e.transpose(out=bank(bT, 128, 0), in_=qt, identity=identF)
                        return e.transpose(out=bank(bT, 128, 128), in_=kt, identity=identF)
                    K.op(K.pe, trqk, reads=[d_qt, d_kt, d_identF], writes=[dps[bT]])
                    K.op(K.act, lambda e, tk=tk: e.copy(out=qtT[:, tk], in_=bank(bT, 128, 0)), reads=[dps[bT]], writes=[d_qtT])
                    K.op(K.act, lambda e, tk=tk: e.copy(out=ktT[:, tk], in_=bank(bT, 128, 128)), reads=[dps[bT]], writes=[d_ktT])
                    mm_group(bank(bA, 128), [(ktT[:, tk], qtT[:, tk])], reads=[d_qtT, d_ktT], writes=[dps[bA]])
                    K.op(K.dve, lambda e, t=t, dr=dr: e.tensor_tensor(out=AT[:, t, :], in0=bank(bA, 128), in1=glam_sb[:, 4 + dr, :], op=ALU.mult),
                         reads=[dps[bA], d_glam], writes=[d_AT])
                torder = list(range(NT)) if dr == 0 else list(range(NT - 1, -1, -1))
                corder = [0, 1, 2, 3] if dr == 0 else [3, 2, 1, 0]
                kv_i = 0
                for n_t, t in enumerate(torder):
                    tk = slice(t * 128, (t + 1) * 128)
                    if n_t > 0 and n_t % 2 == 0:
                        K.op(K.dve, lambda e: e.tensor_scalar(out=S, in0=S, scalar1=keepc[:, 0:1], scalar2=None, op0=ALU.mult),
                             reads=[d_glam], writes=[d_S])
                        K.op(K.act, lambda e: e.copy(out=S_bf, in_=S), reads=[d_S], writes=[d_Sbf])
                    for c in range(4):
                        K.op(K.act, lambda e, c=c, t=t: e.copy(out=qblk[:, c, c * 32:(c + 1) * 32],
                                                               in_=qtT[:, t * 128 + c * 32:t * 128 + (c + 1) * 32]),
                             reads=[d_qtT], writes=[d_qblk])
                        K.op(K.dve, lambda e, c=c, t=t: e.tensor_scalar(out=khm[:, c, :], in0=khat[:, t, :], scalar1=csel_sb[:, c:c + 1],
                                                                        scalar2=None, op0=ALU.mult),
                             reads=[d_khat, d_glam], writes=[d_khm])
                    K.op(K.pe, lambda e, t=t: e.matmul(bank(5, 256), lhsT=AT[:, t, :], rhs=v_b[:, t, :], start=True, stop=False),
                         reads=[d_AT, d_qkv], writes=[dps[5]])
                    for ci, c in enumerate(corder):
                        K.op(K.pe, lambda e, c=c, ci=ci: e.matmul(bank(5, 256), lhsT=qblk[:, c, :], rhs=S_bf, start=False, stop=(ci == 3)),
                             reads=[d_qblk, d_Sbf], writes=[dps[5]])
                        kvo = (kv_i % 2) * 256
                        kv_i += 1
                        mm_group(bank(6, 256, kvo), [(khm[:, c, :], v_b[:, t, :])], reads=[d_khm, d_qkv], writes=[dps[6]])
                        K.op(K.dve, lambda e, c=c, t=t, kvo=kvo: e.scalar_tensor_tensor(
                            out=S, in0=S, scalar=dcol[:, t * 4 + c:t * 4 + c + 1], in1=bank(6, 256, kvo), op0=ALU.mult, op1=ALU.add),
                            reads=[dps[6], d_dcol], writes=[d_S])
                        K.op(K.act, lambda e: e.copy(out=S_bf, in_=S), reads=[d_S], writes=[d_Sbf])
                    if n_t % 2 == 1:
                        K.dma(K.q_sync, og[t // 2, dr, h], S, reads=[d_S])
                    if dr == 0:
                        K.op(K.act, lambda e, t=t: e.copy(out=o_f[:, t, :], in_=bank(5, 256)), reads=[dps[5]], writes=[d_of])
                    else:
                        K.op(K.dve, lambda e, t=t: e.tensor_tensor(out=osum, in0=bank(5, 256), in1=o_f[:, t, :], op=ALU.add),
                             reads=[dps[5], d_of], writes=[d_osum])
                        K.op(K.act, lambda e: e.activation(out=junk, in_=osum, func=AF.Square, accum_out=cg[:, 0:1]),
                             reads=[d_osum], writes=[d_junk, d_cg])
                        rstd_from_ss(cg[:, 0:1], 256.0, d_cg)
                        mm_group(bank(1, 256, 128), [(hT[:, kc, tk], wgr[:, kc, :]) for kc in range(KC)], reads=[d_hT[t], d_wgr], writes=[dps[1]])
                        K.op(K.act, lambda e: e.activation(out=sgr, in_=bank(1, 256, 128), func=AF.Silu), reads=[dps[1]], writes=[d_sgr])
                        K.op(K.dve, lambda e: e.scalar_tensor_tensor(out=on, in0=osum, scalar=cg[:, 0:1], in1=ggb, op0=ALU.mult, op1=ALU.mult),
                             reads=[d_osum, d_cg, d_glam], writes=[d_on])
                        K.op(K.dve, lambda e: e.tensor_tensor(out=on, in0=on, in1=sgr, op=ALU.mult), reads=[d_sgr], writes=[d_on])
                        def tro(e):
                            e.transpose(out=bank(7, 128, 0), in_=on[:, 0:128], identity=identF)
                            return e.transpose(out=bank(7, 128, 128), in_=on[:, 128:256], identity=identF)
                        K.op(K.pe, tro, reads=[d_on, d_identF], writes=[dps[7]])
                        K.op(K.act, lambda e, h=h, tk=tk: e.copy(out=oT[:, h * 2:h * 2 + 2, tk],
                                                                 in_=bank(7, 256).rearrange("p (a b) -> p a b", a=2)),
                             reads=[dps[7]], writes=[d_oT])

    def emit_mixer_stub(l):
        mem.top = PH_X
        ring = Ring(K, mem, 4, 4096)
        emit_mod(l, 0, ring)
        chk("mod0")
        if cfg.get("dbg_noln"):
            gb, d_gb, scr, d_scr = None, Dep(), (None, None, None), Dep()
        else:
            gb, d_gb, scr, d_scr = load_ln(l, 0)
        for t in range(NT):
            if not cfg.get("dbg_nomul"):
                K.op(K.act, lambda e, t=t: e.mul(out=X[:, t, :], in_=X[:, t, :], mul=ALPHA), writes=[dX[t]])
            emit_ln_tile(t, gb, d_gb, scr, d_scr)
        K.barrier()
        mem.top = PH_X
        chk("ln0")

    class _Stop(Exception):
        pass
    stop = cfg.get("stop", "")
    def chk(tag):
        if stop == tag:
            K.barrier()
            raise _Stop()
    try:
        for l in range(depth):
            if do_mixer and l % 2 == 1:
                emit_mixer_mla(l)
            elif do_mixer:
                emit_mixer_ab(l)
            else:
                emit_mixer_stub(l)
            emit_moe(l)
    except _Stop:
        pass

    yv = y_out.rearrange("(t p) d -> p t d", p=128)
    for t in range(NT):
        K.dma(K.q_sync, yv[:, t, :], X[:, t, :], reads=[dX[t]])
    K.finish()
    print("SBUF peak bytes", mem.peak, "sem counts", [(e.name, e.cnt) for e in K.engs])
    return nc


def _gla_consts():
    idx = np.arange(128)
    same = (idx[:, None] // 32) == (idx[None, :] // 32)
    s, t = idx[:, None], idx[None, :]
    m = np.zeros((6, 128, 128), np.float32)
    m[0] = same & (s <= t)
    m[1] = same & (s > t)
    m[2] = same & (s >= t)
    m[3] = same & (s < t)
    m[4] = same & (s <= t)
    m[5] = same & (s >= t)
    csel = np.zeros((128, 4), np.float32)
    csel[idx, idx // 32] = 1.0
    return m, csel


def _rope_tables():
    n_rows = T // 64
    rows = np.repeat(np.arange(n_rows, dtype=np.float32), 64)
    cols = np.tile(np.arange(64, dtype=np.float32), n_rows)
    quarter = 16
    freqs = (10000.0 ** (-np.arange(quarter, dtype=np.float32) / quarter)).astype(np.float32)
    ang = np.concatenate([rows[:, None] * freqs, cols[:, None] * freqs], axis=-1).astype(np.float32)
    return np.cos(ang).astype(np.float32), np.sin(ang).astype(np.float32)


_CACHE = {}


def kernel(**inputs):
    cfg = inputs.pop("_cfg", {})
    inp = {k: np.ascontiguousarray(np.asarray(v)) for k, v in inputs.items()}
    key = tuple(sorted((k, str(v)) for k, v in cfg.items()))
    if key not in _CACHE:
        _CACHE[key] = build(cfg)
    nc = _CACHE[key]
    glam, csel = _gla_consts()
    cos, sin = _rope_tables()
    wnames = ["ada_w", "ada_b", "ln_g", "ln_b", "ab_w_in", "gla_w_gate2", "gla_b_gate2", "gla_norm_g",
              "diff_lambda", "diff_norm_g", "ab_w_out", "mla_w_in", "mla_q_norm_g", "mla_w_uq",
              "mla_kv_norm_g", "mla_w_ukv", "mla_w_out", "moe_w_rg", "moe_b_rg", "moe_w_re", "moe_b_re",
              "moe_w_gate", "moe_w_up", "moe_w_down"]
    in_maps = []
    depth = cfg.get("depth", DEPTH)
    n_exp = cfg.get("n_exp", N_EXP)
    cores = cfg.get("cores", list(range(8)))
    dl, nab, ncl, ne = max(depth, 1), max((depth + 1) // 2, 1), max(depth // 2, 1), max(n_exp, 1)
    wsl = {}
    for n in wnames:
        a = inp[n]
        if n in ("ada_w", "ada_b", "ln_g", "ln_b", "moe_w_rg", "moe_b_rg", "moe_w_re", "moe_b_re"):
            a = a[:dl]
        elif n in ("moe_w_gate", "moe_w_up", "moe_w_down"):
            a = a[:dl, :ne]
        elif n.startswith("mla_"):
            a = a[:ncl]
        else:
            a = a[:nab]
        wsl[n] = np.ascontiguousarray(a)
    for c in cores:
        m = dict(wsl)
        m["identF"] = np.eye(128, dtype=np.float32)
        m["glam"] = glam
        m["csel"] = csel
        if c < 4:
            m["x_in"] = inp["x_prompt"][4 * c:4 * c + 4].reshape(T, D)
            m["cond"] = inp["c_ctx"]
            m["st_gla"] = np.zeros((2, 2, 4, 128, 256), np.float32)
            m["c_dk"] = np.zeros((2, PAST, 1024), np.float32)
            m["c_dv"] = np.zeros((2, PAST, 1024), np.float32)
            m["c_ckv"] = np.zeros((2, PAST, 256), np.float32)
            m["c_kr"] = np.zeros((2, PAST, 64), np.float32)
            m["rope_cos"] = np.ones((T, 32), np.float32)
            m["rope_sin"] = np.zeros((T, 32), np.float32)
            m["ropeT_c"] = np.ones((64, T), np.float32)
            m["ropeT_s"] = np.zeros((64, T), np.float32)
            ss = np.zeros((4, T), np.float32)
            km = np.full((4, NKEY), NEG, np.float32)
            for s in range(4):
                ss[s, 256 * s:256 * (s + 1)] = 1.0
                km[s, PAST + 256 * s:PAST + 256 * (s + 1)] = 0.0
            m["seqsel"] = ss
            m["kmask"] = km
            m["keep"] = np.zeros((128, 1), np.float32)
        else:
            b = c - 4
            m["x_in"] = inp["x_sample"][b]
            m["cond"] = inp["c"][b]
            m["st_gla"] = inp["state_gla"][b]
            m["c_dk"] = inp["cache_diff_k"][b].reshape(2, PAST, 1024)
            m["c_dv"] = inp["cache_diff_v"][b].reshape(2, PAST, 1024)
            m["c_ckv"] = inp["cache_mla_ckv"][b]
            m["c_kr"] = inp["cache_mla_krope"][b]
            m["rope_cos"] = cos
            m["rope_sin"] = sin
            m["ropeT_c"] = np.concatenate([cos.T, cos.T], axis=0)
            m["ropeT_s"] = np.concatenate([-sin.T, sin.T], axis=0)
            ss = np.zeros((4, T), np.float32)
            ss[0] = 1.0
            m["seqsel"] = ss
            m["kmask"] = np.zeros((4, NKEY), np.float32)
            m["keep"] = np.ones((128, 1), np.float32)
        in_maps.append({k: np.ascontiguousarray(v, dtype=np.float32) for k, v in m.items()})
    res = run_bass_kernel_spmd(nc, in_maps, core_ids=list(range(len(cores))))
    if len(cores) != 8:
        return {c: res.results[i] for i, c in enumerate(cores)}
    r = res.results
    y_prompt = np.stack([r[c]["y"].reshape(4, 256, D) for c in range(4)]).reshape(16, 256, D)
    y_sample = np.stack([r[c]["y"] for c in range(4, 8)])
    new_gla = np.concatenate([np.transpose(r[c]["o_gla"], (1, 0, 2, 3, 4, 5)) for c in range(4)], axis=0)
    def tok(name, shp):
        a = np.stack([r[c][name] for c in range(4)])
        a = a.reshape(4, 2, 4, 256, -1).transpose(0, 2, 1, 3, 4).reshape(16, 2, 256, -1)
        return a.reshape((16, 2, 256) + shp)
    new_dk = tok("o_dk", (8, 2, 64))
    new_dv = tok("o_dv", (8, 128))
    new_ckv = tok("o_ckv", (256,))
    new_kr = tok("o_kr", (64,))
    return (y_prompt.astype(np.float32), y_sample.astype(np.float32), new_gla.astype(np.float32),
            new_dk.astype(np.float32), new_dv.astype(np.float32), new_ckv.astype(np.float32),
            new_kr.astype(np.float32))
```
